# Optimizing a Trainium2 kernel written in Bass

```python
import math
import jax
import jax.numpy as jnp
from jax import lax
import numpy as np

D_MODEL = 1024
BATCH = 8
SEQ = 2048
DEPTH = 4

N_MIXERS = 4
QBLOCK = 128
NORM_EPS = 1e-6
NEG_BIG = -1e30
F32 = jnp.float32
DA_HEADS = 8
DA_HEAD_DIM = 64
DA_IN_WIDTH = 3 * DA_HEADS * 2 * DA_HEAD_DIM
NSA_HEADS = 16
NSA_KV_GROUPS = 2
NSA_HEAD_DIM = 64
NSA_QBLOCK = 64
CMP_BLOCK = 32
CMP_STRIDE = 16
CMP_HIDDEN = 256
SEL_BLOCK = 64
N_SELECT = 8
WINDOW = 512
NSA_IN_WIDTH = NSA_HEADS * NSA_HEAD_DIM + 6 * NSA_KV_GROUPS * NSA_HEAD_DIM + 3 * NSA_HEADS
GDN_HEADS = 8
GDN_HEAD_DIM = 128
GDN_CHUNK = 64
GDN_IN_WIDTH = 4 * GDN_HEADS * GDN_HEAD_DIM + 2 * GDN_HEADS
CONV_WIDTH = 4
LRU_WIDTH = D_MODEL
LRU_BLOCKS = 4
LRU_C = 8.0
N_GROUPS = 4
EXPERTS_PER_GROUP = 4
N_EXPERTS = N_GROUPS * EXPERTS_PER_GROUP
TOP_K_IN_GROUP = 2
D_EXPERT = 512

kernel_name = 'hybrid_interleaved_diffattn_nsa_gdn_rglru_hmoe'


def rmsnorm(x, gain):
    xf = x.astype(F32)
    y = xf * lax.rsqrt(jnp.mean(xf * xf, -1, keepdims=True) + NORM_EPS)
    return (y * gain.astype(F32)).astype(x.dtype)


def l2norm(x):
    xf = x.astype(F32)
    return xf * lax.rsqrt(jnp.sum(xf * xf, -1, keepdims=True) + NORM_EPS)


def alibi_slopes(n):
    return jnp.asarray(2.0 ** (-8.0 * np.arange(1, n + 1) / n), dtype=F32)


def causal_depthwise_conv(x, w):
    k = w.shape[0]
    xp = jnp.pad(x, ((0, 0), (k - 1, 0), (0, 0)))
    return lax.conv_general_dilated(xp, w[:, None, :].astype(x.dtype), window_strides=(1,), padding='VALID',
                                    dimension_numbers=('NWC', 'WIO', 'NWC'), feature_group_count=x.shape[-1])


def diff_attention(x, w_in, q_gain, k_gain, lq1, lk1, lq2, lk2, subln, w_out, lambda_init):
    B, T, _ = x.shape
    H, dh = DA_HEADS, DA_HEAD_DIM
    q, k, v = jnp.split(x @ w_in, [2 * H * dh, 4 * H * dh], axis=-1)
    q = rmsnorm(q.reshape(B, T, H, 2, dh), q_gain) * (dh ** -0.5)
    k = rmsnorm(k.reshape(B, T, H, 2, dh), k_gain)
    v = v.reshape(B, T, H, 2 * dh)
    lam = (jnp.exp(jnp.sum(lq1.astype(F32) * lk1.astype(F32)))
           - jnp.exp(jnp.sum(lq2.astype(F32) * lk2.astype(F32))) + lambda_init)
    slopes = alibi_slopes(H)
    outs = []
    for s in range(0, T, QBLOCK):
        e = s + QBLOCK
        sc = jnp.einsum('bqhcd,bkhcd->bhcqk', q[:, s:e], k[:, :e]).astype(F32)
        dist = (jnp.arange(s, e)[:, None] - jnp.arange(e)[None, :]).astype(F32)
        sc = jnp.where(dist >= 0, sc - slopes[:, None, None, None] * dist, -jnp.inf)
        p = jax.nn.softmax(sc, axis=-1)
        pd = p[:, :, 0] - lam * p[:, :, 1]
        outs.append(jnp.einsum('bhqk,bkhe->bqhe', pd.astype(v.dtype), v[:, :e]))
    o = rmsnorm(jnp.concatenate(outs, axis=1), subln) * (1.0 - lambda_init)
    return o.reshape(B, T, H * 2 * dh) @ w_out


def nsa_attention(x, w_in, q_gain, k_gains, cmp_pos, cmp_w1, cmp_w2, w_out):
    B, T, _ = x.shape
    H, G, dh = NSA_HEADS, NSA_KV_GROUPS, NSA_HEAD_DIM
    P = H // G
    kvw = G * dh
    cuts = np.cumsum([H * dh] + [kvw] * 6).tolist()
    q, kc, vc, ks, vs, kw, vw, gl = jnp.split(x @ w_in, cuts, axis=-1)
    q = (rmsnorm(q.reshape(B, T, H, dh), q_gain) * (dh ** -0.5)).reshape(B, T, G, P, dh)
    gates = jax.nn.sigmoid(gl.reshape(B, T, G, P, 3))
    n_cmp = (T - CMP_BLOCK) // CMP_STRIDE + 1
    cidx = np.arange(n_cmp)[:, None] * CMP_STRIDE + np.arange(CMP_BLOCK)[None, :]

    def compress(t, c):
        tb = t.reshape(B, T, G, dh)[:, cidx] + cmp_pos[c][:, None, :]
        tb = tb.transpose(0, 1, 3, 2, 4).reshape(B, n_cmp, G, CMP_BLOCK * dh)
        return jax.nn.silu(tb @ cmp_w1[c]) @ cmp_w2[c]

    kcmp = rmsnorm(compress(kc, 0), k_gains[0])
    vcmp = compress(vc, 1)
    cmp_end = jnp.asarray(cidx[:, -1])
    n_sel = T // SEL_BLOCK
    k_top = min(N_SELECT, n_sel)

    def to_blocks(t):
        return t.reshape(B, n_sel, SEL_BLOCK, G, dh).transpose(0, 3, 1, 2, 4)

    ksb = to_blocks(rmsnorm(ks.reshape(B, T, G, dh), k_gains[1]))
    vsb = to_blocks(vs)
    cs = np.arange(n_cmp) * CMP_STRIDE
    ss0 = np.arange(n_sel) * SEL_BLOCK
    overlap = jnp.asarray(((cs[:, None] < ss0[None, :] + SEL_BLOCK)
                           & (cs[:, None] + CMP_BLOCK > ss0[None, :])).astype(np.float32))
    kwin = rmsnorm(kw.reshape(B, T, G, dh), k_gains[2])
    vwin = vw.reshape(B, T, G, dh)
    slopes = alibi_slopes(H).reshape(G, P)
    gather = jax.vmap(jax.vmap(lambda blocks, idx: blocks[idx]))
    blk_ids = jnp.arange(n_sel)
    outs = []
    for s in range(0, T, NSA_QBLOCK):
        e = s + NSA_QBLOCK
        qb = q[:, s:e]
        tpos = jnp.arange(s, e)
        dist_c = (tpos[:, None] - cmp_end[None, :]).astype(F32)
        valid_c = dist_c >= 0
        sc = jnp.einsum('bqgpd,bcgd->bgpqc', qb, kcmp).astype(F32) - slopes[:, :, None, None] * dist_c
        p_c = jax.nn.softmax(jnp.where(valid_c, sc, NEG_BIG), -1) * valid_c
        o_cmp = jnp.einsum('bgpqc,bcgd->bqgpd', p_c.astype(vcmp.dtype), vcmp)
        imp = jnp.einsum('bgpqc,cn->bgqn', p_c, overlap)
        qblk = tpos // SEL_BLOCK
        forced = (blk_ids[None, :] == 0) | (blk_ids[None, :] == qblk[:, None])
        future = blk_ids[None, :] > qblk[:, None]
        score = jnp.where(forced, 1e4, jnp.where(future, -1.0, imp))
        _, sel = lax.top_k(score, k_top)
        kg = gather(ksb, sel)
        vg = gather(vsb, sel)
        dist_s = (tpos[None, None, :, None, None]
                  - (sel[..., None] * SEL_BLOCK + jnp.arange(SEL_BLOCK))).astype(F32)
        ssc = (jnp.einsum('bqgpd,bgqksd->bgpqks', qb, kg).astype(F32)
               - slopes[None, :, :, None, None, None] * dist_s[:, :, None])
        ssc = jnp.where(dist_s[:, :, None] >= 0, ssc, -jnp.inf)
        p_s = jax.nn.softmax(ssc.reshape(B, G, P, NSA_QBLOCK, k_top * SEL_BLOCK), -1)
        o_sel = jnp.einsum('bgpqn,bgqnd->bqgpd', p_s.astype(vg.dtype),
                           vg.reshape(B, G, NSA_QBLOCK, k_top * SEL_BLOCK, dh))
        ws = max(0, s - WINDOW + 1)
        dist_w = (tpos[:, None] - jnp.arange(ws, e)[None, :]).astype(F32)
        valid_w = (dist_w >= 0) & (dist_w < WINDOW)
        swc = jnp.einsum('bqgpd,bkgd->bgpqk', qb, kwin[:, ws:e]).astype(F32) - slopes[:, :, None, None] * dist_w
        p_w = jax.nn.softmax(jnp.where(valid_w, swc, -jnp.inf), -1)
        o_win = jnp.einsum('bgpqk,bkgd->bqgpd', p_w.astype(vwin.dtype), vwin[:, ws:e])
        gb = gates[:, s:e]
        outs.append(gb[..., 0:1] * o_cmp + gb[..., 1:2] * o_sel + gb[..., 2:3] * o_win)
    o = jnp.concatenate(outs, axis=1).reshape(B, T, H * dh)
    return o @ w_out


def gated_deltanet(x, w_in, conv_w, a_log, dt_bias, out_gain, w_out):
    B, T, _ = x.shape
    H, dk = GDN_HEADS, GDN_HEAD_DIM
    C = GDN_CHUNK
    n = T // C
    W = H * dk
    qkv, z, b, a = jnp.split(x @ w_in, [3 * W, 4 * W, 4 * W + H], axis=-1)
    qkv = jax.nn.silu(causal_depthwise_conv(qkv, conv_w))
    q, k, v = jnp.split(qkv, 3, axis=-1)
    q = l2norm(q.reshape(B, T, H, dk)) * (dk ** -0.5)
    k = l2norm(k.reshape(B, T, H, dk))
    v = v.reshape(B, T, H, dk).astype(F32)
    beta = jax.nn.sigmoid(b.astype(F32))
    g = -jnp.exp(a_log.astype(F32)) * jax.nn.softplus(a.astype(F32) + dt_bias.astype(F32))

    def chunks(t):
        return jnp.moveaxis(t.reshape(B, n, C, H, *t.shape[3:]), 3, 1)

    q, k, v, beta, g = chunks(q), chunks(k), chunks(v), chunks(beta), chunks(g)
    gc = jnp.cumsum(g, axis=-1)
    tril = jnp.tril(jnp.ones((C, C), bool))
    decay = jnp.exp(jnp.where(tril, gc[..., :, None] - gc[..., None, :], -jnp.inf))
    kb = k * beta[..., None]
    lmat = jnp.einsum('bhnid,bhnjd->bhnij', kb, k) * decay * jnp.tril(jnp.ones((C, C), F32), -1)
    ia = lmat + jnp.eye(C, dtype=F32)
    u = lax.linalg.triangular_solve(ia, v * beta[..., None], left_side=True, lower=True, unit_diagonal=True)
    w = lax.linalg.triangular_solve(ia, kb * jnp.exp(gc)[..., None], left_side=True, lower=True, unit_diagonal=True)
    aqk = jnp.einsum('bhnid,bhnjd->bhnij', q, k) * decay

    def step(S, inp):
        qc, kc, uc, wc, gcc, aq = inp
        v_new = uc - jnp.einsum('bhck,bhkv->bhcv', wc, S)
        o = (jnp.einsum('bhck,bhkv->bhcv', qc * jnp.exp(gcc)[..., None], S)
             + jnp.einsum('bhij,bhjv->bhiv', aq, v_new))
        glast = gcc[..., -1:]
        S = S * jnp.exp(glast)[..., None] + jnp.einsum('bhck,bhcv->bhkv', kc * jnp.exp(glast - gcc)[..., None], v_new)
        return S, o

    xs = (jnp.moveaxis(q, 2, 0), jnp.moveaxis(k, 2, 0), jnp.moveaxis(u, 2, 0),
          jnp.moveaxis(w, 2, 0), jnp.moveaxis(gc, 2, 0), jnp.moveaxis(aqk, 2, 0))
    _, o = lax.scan(step, jnp.zeros((B, H, dk, dk), F32), xs)
    o = o.transpose(1, 0, 3, 2, 4).reshape(B, T, H, dk)
    o = rmsnorm(o, out_gain) * jax.nn.silu(z.reshape(B, T, H, dk).astype(F32))
    return o.astype(x.dtype).reshape(B, T, W) @ w_out


def rg_lru_block(x, w_in, conv_w, conv_b, w_a, b_a, w_x, b_x, lam, w_out):
    B, T, _ = x.shape
    gate, rec = jnp.split(x @ w_in, 2, axis=-1)
    gate = jax.nn.gelu(gate, approximate=True)
    xr = causal_depthwise_conv(rec, conv_w) + conv_b
    xb = xr.reshape(B, T, LRU_BLOCKS, -1)
    r = jax.nn.sigmoid((jnp.einsum('btnc,ncd->btnd', xb, w_a).reshape(B, T, -1) + b_a).astype(F32))
    i = jax.nn.sigmoid((jnp.einsum('btnc,ncd->btnd', xb, w_x).reshape(B, T, -1) + b_x).astype(F32))
    log_a = -LRU_C * r * jax.nn.softplus(-lam.astype(F32))
    a = jnp.exp(log_a)
    bterm = jnp.sqrt(1.0 - jnp.exp(2.0 * log_a)) * (i * xr.astype(F32))
    _, h = lax.associative_scan(lambda c1, c2: (c1[0] * c2[0], c2[0] * c1[1] + c2[1]), (a, bterm), axis=1)
    return (h.astype(x.dtype) * gate) @ w_out


def hier_moe(x, w_group, b_group, w_router, b_router, w_gate, w_up, w_down):
    B, T, D = x.shape
    n_tok = B * T
    xt = x.reshape(n_tok, D)
    logit_g = (xt @ w_group + b_group).astype(F32)
    prob_g = jax.nn.softmax(logit_g, -1)
    g_sel = jnp.argmax(logit_g, -1)
    gate_g = jnp.take_along_axis(prob_g, g_sel[:, None], -1)
    logit_e = (xt @ w_router + b_router).astype(F32).reshape(n_tok, N_GROUPS, EXPERTS_PER_GROUP)
    logit_e = jnp.take_along_axis(logit_e, g_sel[:, None, None], 1)[:, 0]
    top_v, top_i = lax.top_k(jax.nn.softmax(logit_e, -1), TOP_K_IN_GROUP)
    top_v = top_v / jnp.sum(top_v, -1, keepdims=True)
    w_in_group = jnp.sum(jax.nn.one_hot(top_i, EXPERTS_PER_GROUP, dtype=F32) * top_v[..., None], 1) * gate_g
    combine = jax.nn.one_hot(g_sel, N_GROUPS, dtype=F32)[:, :, None] * w_in_group[:, None, :]
    y = jnp.zeros_like(xt)
    for g in range(N_GROUPS):
        e = slice(g * EXPERTS_PER_GROUP, (g + 1) * EXPERTS_PER_GROUP)
        h = jax.nn.silu(jnp.einsum('nd,edf->nef', xt, w_gate[e])) * jnp.einsum('nd,edf->nef', xt, w_up[e])
        h = h * combine[:, g, :, None].astype(h.dtype)
        y = y + jnp.einsum('nef,efd->nd', h, w_down[e])
    return y.reshape(B, T, D)


def setup_inputs(seed: int = 0) -> dict:
    key = jax.random.key(seed)
    keys = iter(jax.random.split(key, 48))

    def normal(shape, scale):
        return jax.random.normal(next(keys), shape, F32) * scale

    def gain(shape):
        return 1.0 + normal(shape, 0.02)

    def uniform(shape, lo, hi):
        return jax.random.uniform(next(keys), shape, F32, minval=lo, maxval=hi)

    nA, nB, nC, nD = [len(range(m, DEPTH, N_MIXERS)) for m in range(N_MIXERS)]
    D = D_MODEL
    rs = (2 * DEPTH) ** -0.5
    x = normal((BATCH, SEQ, D), 1.0)
    norm_mix = gain((DEPTH, D))
    norm_ffn = gain((DEPTH, D))
    da_w_in = normal((nA, D, DA_IN_WIDTH), D ** -0.5)
    da_q_norm = gain((nA, DA_HEAD_DIM))
    da_k_norm = gain((nA, DA_HEAD_DIM))
    da_lambda_q1 = normal((nA, DA_HEAD_DIM), 0.1)
    da_lambda_k1 = normal((nA, DA_HEAD_DIM), 0.1)
    da_lambda_q2 = normal((nA, DA_HEAD_DIM), 0.1)
    da_lambda_k2 = normal((nA, DA_HEAD_DIM), 0.1)
    da_subln = gain((nA, 2 * DA_HEAD_DIM))
    da_w_out = normal((nA, DA_HEADS * 2 * DA_HEAD_DIM, D), (DA_HEADS * 2 * DA_HEAD_DIM) ** -0.5 * rs)
    nsa_w_in = normal((nB, D, NSA_IN_WIDTH), D ** -0.5)
    nsa_q_norm = gain((nB, NSA_HEAD_DIM))
    nsa_k_norm = gain((nB, 3, NSA_HEAD_DIM))
    nsa_cmp_pos = normal((nB, 2, CMP_BLOCK, NSA_HEAD_DIM), 0.1)
    nsa_cmp_w1 = normal((nB, 2, CMP_BLOCK * NSA_HEAD_DIM, CMP_HIDDEN), (CMP_BLOCK * NSA_HEAD_DIM) ** -0.5)
    nsa_cmp_w2 = normal((nB, 2, CMP_HIDDEN, NSA_HEAD_DIM), CMP_HIDDEN ** -0.5)
    nsa_w_out = normal((nB, NSA_HEADS * NSA_HEAD_DIM, D), (NSA_HEADS * NSA_HEAD_DIM) ** -0.5 * rs)
    gdn_w_in = normal((nC, D, GDN_IN_WIDTH), D ** -0.5)
    gdn_conv_w = normal((nC, CONV_WIDTH, 3 * GDN_HEADS * GDN_HEAD_DIM), CONV_WIDTH ** -0.5)
    gdn_a_log = jnp.log(uniform((nC, GDN_HEADS), 1.0, 16.0))
    dt = jnp.exp(uniform((nC, GDN_HEADS), math.log(1e-3), math.log(0.1)))
    gdn_dt_bias = dt + jnp.log(-jnp.expm1(-dt))
    gdn_out_norm = gain((nC, GDN_HEAD_DIM))
    gdn_w_out = normal((nC, GDN_HEADS * GDN_HEAD_DIM, D), (GDN_HEADS * GDN_HEAD_DIM) ** -0.5 * rs)
    bw = LRU_WIDTH // LRU_BLOCKS
    lru_w_in = normal((nD, D, 2 * LRU_WIDTH), D ** -0.5)
    lru_conv_w = normal((nD, CONV_WIDTH, LRU_WIDTH), CONV_WIDTH ** -0.5)
    lru_conv_b = normal((nD, LRU_WIDTH), 0.1)
    lru_w_a = normal((nD, LRU_BLOCKS, bw, bw), bw ** -0.5)
    lru_b_a = normal((nD, LRU_WIDTH), 0.1)
    lru_w_x = normal((nD, LRU_BLOCKS, bw, bw), bw ** -0.5)
    lru_b_x = normal((nD, LRU_WIDTH), 0.1)
    a_c = uniform((nD, LRU_WIDTH), 0.9, 0.999)
    a0 = a_c ** (1.0 / LRU_C)
    lru_lambda = jnp.log(a0) - jnp.log1p(-a0)
    lru_w_out = normal((nD, LRU_WIDTH, D), LRU_WIDTH ** -0.5 * rs)
    moe_w_group = normal((DEPTH, D, N_GROUPS), D ** -0.5)
    moe_b_group = normal((DEPTH, N_GROUPS), 0.01)
    moe_w_router = normal((DEPTH, D, N_EXPERTS), D ** -0.5)
    moe_b_router = normal((DEPTH, N_EXPERTS), 0.01)
    moe_w_gate = normal((DEPTH, N_EXPERTS, D, D_EXPERT), D ** -0.5)
    moe_w_up = normal((DEPTH, N_EXPERTS, D, D_EXPERT), D ** -0.5)
    moe_w_down = normal((DEPTH, N_EXPERTS, D_EXPERT, D), D_EXPERT ** -0.5 * rs)
    return {'x': x, 'norm_mix': norm_mix, 'norm_ffn': norm_ffn,
            'da_w_in': da_w_in, 'da_q_norm': da_q_norm, 'da_k_norm': da_k_norm,
            'da_lambda_q1': da_lambda_q1, 'da_lambda_k1': da_lambda_k1,
            'da_lambda_q2': da_lambda_q2, 'da_lambda_k2': da_lambda_k2,
            'da_subln': da_subln, 'da_w_out': da_w_out,
            'nsa_w_in': nsa_w_in, 'nsa_q_norm': nsa_q_norm, 'nsa_k_norm': nsa_k_norm,
            'nsa_cmp_pos': nsa_cmp_pos, 'nsa_cmp_w1': nsa_cmp_w1, 'nsa_cmp_w2': nsa_cmp_w2,
            'nsa_w_out': nsa_w_out,
            'gdn_w_in': gdn_w_in, 'gdn_conv_w': gdn_conv_w, 'gdn_a_log': gdn_a_log,
            'gdn_dt_bias': gdn_dt_bias, 'gdn_out_norm': gdn_out_norm, 'gdn_w_out': gdn_w_out,
            'lru_w_in': lru_w_in, 'lru_conv_w': lru_conv_w, 'lru_conv_b': lru_conv_b,
            'lru_w_a': lru_w_a, 'lru_b_a': lru_b_a, 'lru_w_x': lru_w_x, 'lru_b_x': lru_b_x,
            'lru_lambda': lru_lambda, 'lru_w_out': lru_w_out,
            'moe_w_group': moe_w_group, 'moe_b_group': moe_b_group,
            'moe_w_router': moe_w_router, 'moe_b_router': moe_b_router,
            'moe_w_gate': moe_w_gate, 'moe_w_up': moe_w_up, 'moe_w_down': moe_w_down}


def reference(x, norm_mix, norm_ffn,
              da_w_in, da_q_norm, da_k_norm, da_lambda_q1, da_lambda_k1, da_lambda_q2, da_lambda_k2,
              da_subln, da_w_out,
              nsa_w_in, nsa_q_norm, nsa_k_norm, nsa_cmp_pos, nsa_cmp_w1, nsa_cmp_w2, nsa_w_out,
              gdn_w_in, gdn_conv_w, gdn_a_log, gdn_dt_bias, gdn_out_norm, gdn_w_out,
              lru_w_in, lru_conv_w, lru_conv_b, lru_w_a, lru_b_a, lru_w_x, lru_b_x, lru_lambda, lru_w_out,
              moe_w_group, moe_b_group, moe_w_router, moe_b_router, moe_w_gate, moe_w_up, moe_w_down):
    for i in range(DEPTH):
        m, j = i % N_MIXERS, i // N_MIXERS
        h = rmsnorm(x, norm_mix[i])
        if m == 0:
            y = diff_attention(h, da_w_in[j], da_q_norm[j], da_k_norm[j], da_lambda_q1[j], da_lambda_k1[j],
                               da_lambda_q2[j], da_lambda_k2[j], da_subln[j], da_w_out[j],
                               0.8 - 0.6 * math.exp(-0.3 * i))
        elif m == 1:
            y = nsa_attention(h, nsa_w_in[j], nsa_q_norm[j], nsa_k_norm[j], nsa_cmp_pos[j],
                              nsa_cmp_w1[j], nsa_cmp_w2[j], nsa_w_out[j])
        elif m == 2:
            y = gated_deltanet(h, gdn_w_in[j], gdn_conv_w[j], gdn_a_log[j], gdn_dt_bias[j],
                               gdn_out_norm[j], gdn_w_out[j])
        else:
            y = rg_lru_block(h, lru_w_in[j], lru_conv_w[j], lru_conv_b[j], lru_w_a[j], lru_b_a[j],
                             lru_w_x[j], lru_b_x[j], lru_lambda[j], lru_w_out[j])
        x = x + y
        x = x + hier_moe(rmsnorm(x, norm_ffn[i]), moe_w_group[i], moe_b_group[i], moe_w_router[i],
                         moe_b_router[i], moe_w_gate[i], moe_w_up[i], moe_w_down[i])
    return x
```

```python
import contextlib
import math
import numpy as np
import concourse.bass as bass
import concourse.mybir as mybir
from concourse.bass_utils import run_bass_kernel_spmd

F32 = mybir.dt.float32
BF16 = mybir.dt.bfloat16
AF = mybir.ActivationFunctionType
ALU = mybir.AluOpType
AX = mybir.AxisListType

T = 2048
D = 1024
NT = 16
KC = 8
EPS = 1e-6
NEXP = 16
DEXP = 512


class Buf:
    __slots__ = ("name", "w", "r", "excl")

    def __init__(self, name="", excl=False):
        self.name = name
        self.excl = excl
        self.w = None
        self.r = None


class _Rec:
    def __init__(self):
        self.call = None

    def __getattr__(self, name):
        def f(*args, **kwargs):
            self.call = (name, args, kwargs)
            return self
        return f


class _Op:
    __slots__ = ("idx", "eng", "call", "deps", "dur", "is_dma", "slow", "start", "tok", "nsucc", "tcls")


def _free_size(ap):
    sh = ap.shape
    n = 1
    for d in sh[1:]:
        n *= int(d)
    return n


class Sched:
    WINDOW = 48

    def __init__(self, nc, es, ndma=28):
        self.nc = nc
        self.engs = {"pe": nc.tensor, "dve": nc.vector, "act": nc.scalar, "pool": nc.gpsimd, "sp": nc.sync}
        self.sem = {k: es.enter_context(nc.semaphore("c_" + k)) for k in self.engs}
        self.cnt = {k: 0 for k in self.engs}
        self.dsem = [es.enter_context(nc.semaphore("d%d" % i)) for i in range(ndma)]
        self.dcnt = [0] * ndma
        self.dnext = 0
        self.dnext_sw = 0
        self.waited = {k: {} for k in self.engs}
        self.ninst = 0
        self.muted = False
        self.ops = []
        self.seg = 0
        import os
        self.reorder = os.environ.get("KREORDER", "1") == "1"

    def _record(self, e, call, reads, writes, is_dma=False, slow=False):
        op = _Op()
        op.idx = len(self.ops)
        op.eng = e
        op.call = call
        op.is_dma = is_dma
        op.slow = slow
        deps = set()
        seg = self.seg
        xr = [b for b in reads if b.excl and b not in writes]
        if xr:
            writes = writes + xr
            reads = [b for b in reads if not b.excl]
        for b in reads:
            if b.w is not None and b.w[0] == seg:
                deps.add(b.w[1])
        for b in writes:
            if b.w is not None and b.w[0] == seg:
                deps.add(b.w[1])
            if b.r and b.r[0] == seg:
                deps.update(b.r[1])
        op.deps = deps
        for b in reads:
            if not b.r or b.r[0] != seg:
                b.r = (seg, [])
            b.r[1].append(op.idx)
        for b in writes:
            b.w = (seg, op.idx)
            b.r = (seg, [])
        name, args, kwargs = call
        if is_dma:
            out = kwargs["out"]
            nbytes = _free_size(out) * int(out.shape[0]) * 4
            op.dur = 2.0 + nbytes / 150e3
        elif e == "pe":
            if name == "transpose":
                op.dur = 0.12
            else:
                op.dur = 0.06 + _free_size(args[2]) / 2400.0
        else:
            o = kwargs.get("out", args[0] if args else None)
            op.dur = 0.22 + (_free_size(o) / 1000.0 if o is not None else 0.1)
        op.tcls = None
        if e == "act" and name == "activation":
            f = kwargs.get("func")
            if f in (AF.Exp, AF.Ln):
                op.tcls = "E"
            elif f in (AF.Copy, AF.Square, AF.Identity):
                op.tcls = None
            else:
                op.tcls = str(f)
        self.ops.append(op)

    def op(self, e, fn, reads=(), writes=()):
        if self.muted:
            return
        r = _Rec()
        fn(r)
        self._record(e, r.call, list(reads), list(writes))

    def dma(self, q, out, in_, reads=(), writes=(), slow=False):
        if self.muted:
            return
        self._record(q, ("dma_start", (), {"out": out, "in_": in_}), list(reads), list(writes), is_dma=True, slow=slow)

    def _schedule(self):
        import bisect
        ops = self.ops
        n = len(ops)
        if not self.reorder:
            for i, op in enumerate(ops):
                op.start = float(i)
            return
        succ = [[] for _ in range(n)]
        indeg = [0] * n
        for op in ops:
            indeg[op.idx] = len(op.deps)
            for d in op.deps:
                succ[d].append(op.idx)
        ready_t = [0.0] * n
        finish = [0.0] * n
        avail = {k: [] for k in self.engs}
        for op in ops:
            if indeg[op.idx] == 0:
                avail[op.eng].append(op.idx)
        free = {k: 0.0 for k in self.engs}
        dmafree = {k: 0.0 for k in self.engs}
        placed = 0
        act_cls = getattr(self, "_act_cls", None)
        W = self.WINDOW
        maxdma = getattr(self, 'MAXDMA', 10**9)
        while placed < n:
            best = None
            for e, lst in avail.items():
                if not lst:
                    continue
                fe = free[e]
                for i in lst[:W]:
                    st_ = ready_t[i]
                    if st_ < fe:
                        st_ = fe
                    if e == "act":
                        tc = ops[i].tcls
                        if tc is not None and tc != act_cls:
                            st_ += 1.3
                    key = (st_, i)
                    if best is None or key < best[0]:
                        best = (key, e, i)
            (st_, i), e, _ = best
            op = ops[i]
            if e == "act" and op.tcls is not None:
                act_cls = op.tcls
            avail[e].remove(i)
            op.start = st_
            if op.is_dma:
                free[e] = st_ + (1.0 if e == "pool" else 0.1)
                t0 = max(st_, dmafree[e])
                fin = t0 + op.dur
                dmafree[e] = t0 + (op.dur - 2.0)
            else:
                fin = st_ + op.dur
                free[e] = fin
            finish[i] = fin
            placed += 1
            for s_ in succ[i]:
                lat = fin + (0.0 if (ops[s_].eng == e and not op.is_dma) else 0.25)
                if lat > ready_t[s_]:
                    ready_t[s_] = lat
                indeg[s_] -= 1
                if indeg[s_] == 0:
                    bisect.insort(avail[ops[s_].eng], s_)

    def _wait(self, e, tok):
        kind, key, val = tok
        if kind == "c" and key == e and e == "pe":
            return
        sk = (kind, key)
        if self.waited[e].get(sk, 0) >= val:
            return
        sem = self.sem[key] if kind == "c" else self.dsem[key]
        self.engs[e].wait_ge(sem, val)
        self.waited[e][sk] = val

    def flush(self):
        if not self.ops:
            self.seg += 1
            return
        self._schedule()
        ops = self.ops
        order = sorted(ops, key=lambda o: (o.start, o.idx))
        for op in order:
            e = op.eng
            name, args, kwargs = op.call
            if op.is_dma:
                half = len(self.dsem) // 2
                if e == "pool":
                    i = half + self.dnext_sw
                    self.dnext_sw = (self.dnext_sw + 1) % (len(self.dsem) - half)
                else:
                    i = self.dnext
                    self.dnext = (self.dnext + 1) % half
                if self.dcnt[i] > 0:
                    self._wait(e, ("d", i, self.dcnt[i]))
            for d in sorted(op.deps):
                self._wait(e, ops[d].tok)
            if op.is_dma:
                self.dcnt[i] += 16
                kw = dict(kwargs)
                if op.slow:
                    kw["allow_slow_non_contiguous"] = True
                self.engs[e].dma_start(**kw).then_inc(self.dsem[i], 16)
                op.tok = ("d", i, self.dcnt[i])
            else:
                inst = getattr(self.engs[e], name)(*args, **kwargs)
                self.cnt[e] += 1
                inst.then_inc(self.sem[e], 1)
                op.tok = ("c", e, self.cnt[e])
            self.ninst += 1
        self.ops = []
        self.seg += 1

    def barrier(self, engines=("pe", "dve", "act", "pool", "sp")):
        if self.muted:
            return
        self.flush()
        for e in engines:
            for k in self.engs:
                if k != e and self.cnt[k] > 0:
                    self._wait(e, ("c", k, self.cnt[k]))
            for i, c in enumerate(self.dcnt):
                if c > 0:
                    self._wait(e, ("d", i, c))


class Rot:
    def __init__(self, items):
        self.items = items
        self.i = 0

    def next(self):
        it = self.items[self.i]
        self.i = (self.i + 1) % len(self.items)
        return it


SHAPES = {
    "x": (T, D), "norm_mix": (4, D), "norm_ffn": (4, D),
    "da_w_in": (1, D, 3072), "da_q_norm": (1, 64), "da_k_norm": (1, 64),
    "da_lambda_q1": (1, 64), "da_lambda_k1": (1, 64), "da_lambda_q2": (1, 64), "da_lambda_k2": (1, 64),
    "da_subln": (1, 128), "da_w_out": (1, 1024, D),
    "nsa_w_in": (1, D, 1840), "nsa_q_norm": (1, 64), "nsa_k_norm": (1, 3, 64), "nsa_cmp_pos": (1, 2, 32, 64),
    "nsa_cmp_w1": (1, 2, 2048, 256), "nsa_cmp_w2": (1, 2, 256, 64), "nsa_w_out": (1, 1024, D),
    "gdn_w_in": (1, D, 4112), "gdn_conv_w": (1, 4, 3072), "gdn_a_log": (1, 8), "gdn_dt_bias": (1, 8),
    "gdn_out_norm": (1, 128), "gdn_w_out": (1, 1024, D),
    "lru_w_in": (1, D, 2048), "lru_conv_w": (1, 4, 1024), "lru_conv_b": (1, 1024), "lru_w_a": (1, 4, 256, 256),
    "lru_b_a": (1, 1024), "lru_w_x": (1, 4, 256, 256), "lru_b_x": (1, 1024), "lru_lambda": (1, 1024),
    "lru_w_out": (1, 1024, D),
    "moe_w_group": (4, D, 4), "moe_b_group": (4, 4), "moe_w_router": (4, D, 16), "moe_b_router": (4, 16),
    "moe_w_gate": (4, 16, D, DEXP), "moe_w_up": (4, 16, D, DEXP), "moe_w_down": (4, 16, DEXP, D),
    "c_ident": (128, 128), "c_maskneg": (128, 128), "c_relpos": (128, 16),
    "c_e64": (64, 2048), "c_keep": (128, 512), "c_add": (128, 512), "c_dcbase": (128, 127), "c_overlap": (128, 32),
    "c_maskw": (128, 128),
    "c_mup": (128, 128), "c_mfull": (128, 128), "c_mbu": (128, 128), "c_mbls": (128, 128), "c_half": (128, 2),
}


def host_consts():
    p = np.arange(128)
    c = {}
    c["c_ident"] = np.eye(128, dtype=np.float32)
    c["c_maskneg"] = np.where(p[:, None] > p[None, :], -30000.0, 0.0).astype(np.float32)
    c["c_relpos"] = (p[:, None] - 128.0 * np.arange(16)[None, :]).astype(np.float32)
    same = (p[:, None] // 64) == (p[None, :] // 64)
    c["c_mup"] = (same & (p[:, None] <= p[None, :])).astype(np.float32)
    c["c_mfull"] = same.astype(np.float32)
    c["c_mbu"] = np.where(same & (p[:, None] <= p[None, :]), 0.0, -1e4).astype(np.float32)
    c["c_mbls"] = np.where(same & (p[None, :] < p[:, None]), 0.0, -1e4).astype(np.float32)
    c["c_half"] = np.stack([(p < 64), (p >= 64)], axis=1).astype(np.float32)
    key = np.arange(2048)
    e64 = np.zeros((64, 2048), np.float32)
    e64[key // 64, key] = 1.0
    c["c_e64"] = e64
    tpos = (np.arange(16)[None, :, None] * 128 + p[:, None, None])
    blk = np.arange(32)[None, None, :]
    qblk = tpos // 64
    forced = (blk == 0) | (blk == qblk)
    future = blk > qblk
    c["c_keep"] = (~forced & ~future).astype(np.float32).reshape(128, 512)
    c["c_add"] = (1e4 * forced - 1.0 * (future & ~forced)).astype(np.float32).reshape(128, 512)
    n = np.arange(127)
    c["c_dcbase"] = (p[:, None] - 16.0 * n[None, :] - 31.0).astype(np.float32)
    cs = n * 16
    ss0 = np.arange(32) * 64
    ov = ((cs[:, None] < ss0[None, :] + 64) & (cs[:, None] + 32 > ss0[None, :])).astype(np.float32)
    c["c_overlap"] = np.concatenate([ov, np.zeros((1, 32), np.float32)], axis=0)
    c["c_maskw"] = (p[:, None] > p[None, :]).astype(np.float32)
    return c


class Ctx:
    pass


class StopBuild(Exception):
    pass


def ckpt(C, n):
    import os
    if int(os.environ.get("KSTOP", "0")) == n:
        C.S.barrier()
        C.S.muted = True


def build(nlayers=4, only_layer=None):
    nc = bass.Bass("TRN2", target_bir_lowering=False)
    class LazyDram(dict):
        def __missing__(self, name):
            base = name.split("@")[0]
            shape = SHAPES[base] if "@" not in name else SHAPES[base][1:]
            self[name] = nc.dram_tensor(name.replace("@", "_L"), list(shape), F32, kind="ExternalInput").ap()
            return self[name]

    dr = LazyDram()
    x_d = dr["x"]
    out_d = nc.dram_tensor("out", [T, D], F32, kind="ExternalOutput").ap()

    with contextlib.ExitStack() as es:
        S = Sched(nc, es)
        C = Ctx()
        C.nc, C.S, C.dr = nc, S, dr

        uid = [0]

        def sb(name, shape, dt, stack=es):
            uid[0] += 1
            return stack.enter_context(nc.sbuf_tensor("%s_u%d" % (name, uid[0]), list(shape), dt))

        C.sb = sb
        C.X = sb("X", (128, NT, D), F32)
        C.Xb = [Buf("X%d" % i) for i in range(NT)]
        C.xnT = sb("xnT", (128, KC, T), BF16)
        C.xnTb = [Buf("xnT%d" % i) for i in range(NT)]
        C.ident = sb("ident", (128, 128), F32); C.identb = Buf("ident")
        C.identh = sb("identh", (128, 128), BF16); C.identhb = Buf("identh")
        C.maskneg = sb("maskneg", (128, 128), BF16); C.masknegb = Buf("maskneg")
        C.relpos = sb("relpos", (128, 16), F32); C.relposb = Buf("relpos")
        C.ps = [es.enter_context(nc.psum_tensor("ps%d" % i, [128, 512], F32)) for i in range(8)]
        C.psb = [Buf("ps%d" % i, excl=True) for i in range(8)]

        S.dma("sp", C.ident[:], dr["c_ident"], writes=[C.identb])
        S.dma("pool", C.identh[:], dr["c_ident"], writes=[C.identhb])
        S.dma("pool", C.maskneg[:], dr["c_maskneg"], writes=[C.masknegb])
        S.dma("sp", C.relpos[:], dr["c_relpos"], writes=[C.relposb])
        xv = x_d.rearrange("(t p) d -> p t d", p=128)
        for i in range(0, NT, 2):
            S.dma("sp", C.X[:, i:i + 2, :], xv[:, i:i + 2, :], writes=C.Xb[i:i + 2])

        for l in range(nlayers):
          try:
            m = l % 4
            if only_layer is not None and l != only_layer:
                continue
            import os
            parts = os.environ.get("KPARTS", "mix,moe")
            if m == 0 and "mix" in parts:
                da_layer(C, l)
            if m == 3 and "mix" in parts:
                lru_layer(C, l)
            if m == 2 and "mix" in parts:
                S.barrier()
                keep_r = S.reorder
                S.reorder = keep_r and os.environ.get("KREORDER_GDN", "1") == "1"
                keep_w = S.WINDOW
                S.WINDOW = int(os.environ.get("KWINDOW_GDN", str(keep_w)))
                gdn_layer(C, l)
                S.barrier()
                S.reorder = keep_r
                S.WINDOW = keep_w
            if m == 1 and "mix" in parts:
                nsa_layer(C, l)
            if "moe" in parts:
                moe_layer(C, l)
          except StopBuild:
            break

        S.muted = False
        ov = out_d.rearrange("(t p) d -> p t d", p=128)
        outb = Buf("out")
        for i in range(0, NT, 2):
            S.dma("sp", ov[:, i:i + 2, :], C.X[:, i:i + 2, :], reads=C.Xb[i:i + 2], writes=[outb])
        S.barrier()
    nc._in_names = list(dr.keys())
    print('inputs', nc._in_names)
    print("instructions:", S.ninst, "counts", S.cnt)
    return nc


def small_rstd(C, tmp, tmpb, ss, ssb, n_over, shape_p=128):
    S = C.S
    S.op("dve", lambda e: e.tensor_scalar(out=ss, in0=ss, scalar1=1.0 / n_over, scalar2=EPS,
                                          op0=ALU.mult, op1=ALU.add), reads=[ssb], writes=[ssb])
    S.op("act", lambda e: e.activation(out=ss, in_=ss, func=AF.Ln), reads=[ssb], writes=[ssb])
    S.op("act", lambda e: e.activation(out=ss, in_=ss, func=AF.Exp, scale=-0.5), reads=[ssb], writes=[ssb])


def norm_phase(C, gain_row_ap, st, router=None, keep=False):
    S, nc = C.S, C.nc
    outer_st = st
    if not keep:
        st = contextlib.ExitStack()
    C.gain = C.sb("gain", (128, D), F32, st); C.gainb = Buf("gain")
    C.junk = C.sb("junk", (128, D), BF16, st); C.junkb = Buf("junk")
    S.dma("sp", C.gain[:], gain_row_ap.partition_broadcast(128), writes=[C.gainb])
    xn32 = [C.sb("xn32_%d" % i, (128, D), F32, st) for i in range(2)]
    xn32b = [Buf() for _ in range(2)]
    ssq = [C.sb("ssq_%d" % i, (128, 1), F32, st) for i in range(2)]
    ssqb = [Buf() for _ in range(2)]
    if router is not None:
        xT32 = [C.sb("xT32_%d" % i, (128, KC, 128), BF16, st) for i in range(2)]
        xT32b = [Buf() for _ in range(2)]
    for t in range(NT):
        s = t % 2
        Xt = C.X[:, t, :]
        S.op("act", lambda e: e.activation(out=C.junk[:], in_=Xt, func=AF.Square, accum_out=ssq[s][:]),
             reads=[C.Xb[t]], writes=[C.junkb, ssqb[s]])
        small_rstd(C, None, None, ssq[s][:], ssqb[s], float(D))
        S.op("dve", lambda e: e.scalar_tensor_tensor(out=xn32[s][:], in0=Xt, scalar=ssq[s][:, 0:1], in1=C.gain[:],
                                                     op0=ALU.mult, op1=ALU.mult),
             reads=[C.Xb[t], ssqb[s], C.gainb], writes=[xn32b[s]])
        for b in range(2):
            pi = C.psrot.next()
            ps, psb = C.ps[pi], C.psb[pi]
            for k4 in range(4):
                k = b * 4 + k4
                S.op("pe", lambda e: e.transpose(ps[:, k4 * 128:(k4 + 1) * 128], xn32[s][:, k * 128:(k + 1) * 128],
                                                 C.ident[:]),
                     reads=[xn32b[s], C.identb], writes=[psb])
            psv = ps[:].rearrange("p (a b) -> p a b", a=4)
            S.op("act", lambda e: e.activation(out=C.xnT[:, b * 4:(b + 1) * 4, t * 128:(t + 1) * 128], in_=psv,
                                               func=AF.Copy),
                 reads=[psb], writes=[C.xnTb[t]])
            if router is not None:
                S.op("dve", lambda e: e.tensor_tensor(out=xT32[s][:, b * 4:(b + 1) * 4, :], in0=psv,
                                                      in1=C.xnT[:, b * 4:(b + 1) * 4, t * 128:(t + 1) * 128], op=ALU.subtract),
                     reads=[psb, C.xnTb[t]], writes=[xT32b[s]])
        if router is not None:
            router(t, xT32[s], xT32b[s])
    if not keep:
        S.barrier()
        st.close()


def moe_layer(C, l):
    S, nc, dr = C.S, C.nc, C.dr
    C.psrot = Rot(list(range(8)))
    with contextlib.ExitStack() as st:
        sb = lambda n, sh, dt: C.sb("moe%d_%s" % (l, n), sh, dt, st)
        wr = sb("wr", (128, KC, 20), F32); wrb = Buf()
        S.dma("sp", wr[:, :, 0:4], dr["moe_w_group"][l].rearrange("(k p) n -> p k n", p=128), writes=[wrb])
        S.dma("sp", wr[:, :, 4:20], dr["moe_w_router"][l].rearrange("(k p) n -> p k n", p=128), writes=[wrb])
        wrh = sb("wrh", (128, KC, 20), BF16); wrl = sb("wrl", (128, KC, 20), BF16)
        S.op("dve", lambda e: e.tensor_copy(out=wrh[:], in_=wr[:]), reads=[wrb], writes=[wrb])
        S.op("dve", lambda e: e.tensor_tensor(out=wrl[:], in0=wr[:], in1=wrh[:], op=ALU.subtract), reads=[wrb], writes=[wrb])
        rb = sb("rb", (128, 20), F32); rbb = Buf()
        S.dma("sp", rb[:, 0:4], dr["moe_b_group"][l:l + 1, :].partition_broadcast(128), writes=[rbb])
        S.dma("sp", rb[:, 4:20], dr["moe_b_router"][l:l + 1, :].partition_broadcast(128), writes=[rbb])
        comb = sb("comb", (128, NT, 16), F32); combb = [Buf() for _ in range(NT)]
        L = [sb("L%d" % i, (128, 20), F32) for i in range(2)]
        sm = [sb("sm%d" % i, (128, 64), F32) for i in range(2)]
        Lb = [Buf() for _ in range(2)]

        wg = [sb("wg%d" % i, (128, KC, DEXP), BF16) for i in range(2)]
        wu = [sb("wu%d" % i, (128, KC, DEXP), BF16) for i in range(2)]
        wd = [sb("wd%d" % i, (128, 4, D), BF16) for i in range(2)]
        wgb = [Buf() for _ in range(2)]; wub = [Buf() for _ in range(2)]; wdb = [Buf() for _ in range(2)]

        def load_expert(e):
            s = e % 2
            S.dma("pool", wg[s][:], dr["moe_w_gate@%d" % l][e].rearrange("(k p) f -> p k f", p=128), writes=[wgb[s]])
            S.dma("pool", wu[s][:], dr["moe_w_up@%d" % l][e].rearrange("(k p) f -> p k f", p=128), writes=[wub[s]])
            S.dma("pool", wd[s][:], dr["moe_w_down@%d" % l][e].rearrange("(k p) f -> p k f", p=128), writes=[wdb[s]])

        load_expert(0)
        load_expert(1)

        def router(t, xT, xTb):
            s = t % 2
            pi = C.psrot.next()
            ps, psb = C.ps[pi], C.psb[pi]
            for k in range(KC):
                xh = C.xnT[:, k, t * 128:(t + 1) * 128]
                S.op("pe", lambda e: e.matmul(ps[:, 0:20], xh, wrh[:, k, :], start=(k == 0), stop=False),
                     reads=[C.xnTb[t], wrb], writes=[psb])
                S.op("pe", lambda e: e.matmul(ps[:, 0:20], xT[:, k, :], wrh[:, k, :], start=False, stop=False),
                     reads=[xTb, wrb], writes=[psb])
                S.op("pe", lambda e: e.matmul(ps[:, 0:20], xh, wrl[:, k, :], start=False, stop=(k == KC - 1)),
                     reads=[C.xnTb[t], wrb], writes=[psb])
            Lt, m = L[s], sm[s]
            S.op("dve", lambda e: e.tensor_tensor(out=Lt[:], in0=ps[:, 0:20], in1=rb[:], op=ALU.add),
                 reads=[psb, rbb], writes=[Lb[s]])
            B = [Lb[s]]
            S.op("dve", lambda e: e.tensor_reduce(out=m[:, 0:1], in_=Lt[:, 0:4], axis=AX.X, op=ALU.max, negate=True),
                 reads=B, writes=B)
            S.op("act", lambda e: e.activation(out=m[:, 2:6], in_=Lt[:, 0:4], func=AF.Exp, bias=m[:, 0:1], scale=1.0,
                                               accum_out=m[:, 1:2]), reads=B, writes=B)
            S.op("dve", lambda e: e.tensor_scalar(out=m[:, 6:10], in0=Lt[:, 0:4], scalar1=m[:, 0:1], scalar2=0.0,
                                                  op0=ALU.add, op1=ALU.is_ge), reads=B, writes=B)
            S.op("dve", lambda e: e.tensor_scalar(out=m[:, 6:10], in0=m[:, 6:10], scalar1=-1.0, scalar2=1e30,
                                                  op0=ALU.add, op1=ALU.mult), reads=B, writes=B)
            S.op("dve", lambda e: e.tensor_tensor(
                out=m[:, 10:26].rearrange("p (g j) -> p g j", g=4),
                in0=Lt[:, 4:20].rearrange("p (g j) -> p g j", g=4),
                in1=m[:, 6:10].unsqueeze(2).to_broadcast([128, 4, 4]), op=ALU.add), reads=B, writes=B)
            S.op("dve", lambda e: e.max(out=m[:, 26:34], in_=m[:, 10:26]), reads=B, writes=B)
            S.op("dve", lambda e: e.tensor_scalar(out=m[:, 34:35], in0=m[:, 26:27], scalar1=-1.0, scalar2=None,
                                                  op0=ALU.mult), reads=B, writes=B)
            S.op("act", lambda e: e.activation(out=m[:, 40:56], in_=m[:, 10:26], func=AF.Exp, bias=m[:, 34:35],
                                               scale=1.0), reads=B, writes=B)
            S.op("dve", lambda e: e.tensor_scalar(out=m[:, 10:26], in0=m[:, 10:26], scalar1=m[:, 27:28], scalar2=None,
                                                  op0=ALU.is_ge), reads=B, writes=B)
            S.op("dve", lambda e: e.tensor_tensor(out=m[:, 40:56], in0=m[:, 40:56], in1=m[:, 10:26], op=ALU.mult),
                 reads=B, writes=B)
            S.op("dve", lambda e: e.tensor_reduce(out=m[:, 35:36], in_=m[:, 40:56], axis=AX.X, op=ALU.add),
                 reads=B, writes=B)
            S.op("dve", lambda e: e.tensor_tensor(out=m[:, 36:37], in0=m[:, 35:36], in1=m[:, 1:2], op=ALU.mult),
                 reads=B, writes=B)
            S.op("dve", lambda e: e.reciprocal(out=m[:, 36:37], in_=m[:, 36:37]), reads=B, writes=B)
            S.op("dve", lambda e: e.tensor_scalar(out=comb[:, t, :], in0=m[:, 40:56], scalar1=m[:, 36:37], scalar2=None,
                                                  op0=ALU.mult), reads=B, writes=[combb[t]])

        norm_phase(C, dr["norm_ffn"][l:l + 1, :], st, router=router, keep=True)
        ckpt(C, 11)

        sg = [sb("sg%d" % i, (128, 512), BF16) for i in range(3)]
        sgr = Rot([(sg[i], Buf()) for i in range(3)])
        hT = [sb("hT%d" % i, (128, 4, 512), BF16) for i in range(2)]
        hTb = [[Buf() for _ in range(4)] for _ in range(2)]
        hi = 0
        for e_ in range(NEXP):
            s = e_ % 2
            for tb in range(4):
                hs = hi % 2
                hi += 1
                for fc in range(4):
                    pg = C.psrot.next(); pu = C.psrot.next()
                    for k in range(KC):
                        S.op("pe", lambda e: e.matmul(C.ps[pg][:], wg[s][:, k, fc * 128:(fc + 1) * 128],
                                                      C.xnT[:, k, tb * 512:(tb + 1) * 512],
                                                      start=(k == 0), stop=(k == KC - 1)),
                             reads=[wgb[s]] + C.xnTb[tb * 4:tb * 4 + 4], writes=[C.psb[pg]])
                    for k in range(KC):
                        S.op("pe", lambda e: e.matmul(C.ps[pu][:], wu[s][:, k, fc * 128:(fc + 1) * 128],
                                                      C.xnT[:, k, tb * 512:(tb + 1) * 512],
                                                      start=(k == 0), stop=(k == KC - 1)),
                             reads=[wub[s]] + C.xnTb[tb * 4:tb * 4 + 4], writes=[C.psb[pu]])
                    sgt, sgb = sgr.next()
                    S.op("act", lambda e: e.activation(out=sgt[:], in_=C.ps[pg][:], func=AF.Silu),
                         reads=[C.psb[pg]], writes=[sgb])
                    S.op("dve", lambda e: e.tensor_tensor(out=hT[hs][:, fc, :], in0=C.ps[pu][:], in1=sgt[:], op=ALU.mult),
                         reads=[C.psb[pu], sgb], writes=[hTb[hs][fc]])
                for tt in range(4):
                    t = tb * 4 + tt
                    for dh in range(2):
                        py = C.psrot.next()
                        for fc in range(4):
                            S.op("pe", lambda e: e.matmul(C.ps[py][:], hT[hs][:, fc, tt * 128:(tt + 1) * 128],
                                                          wd[s][:, fc, dh * 512:(dh + 1) * 512],
                                                          start=(fc == 0), stop=(fc == 3)),
                                 reads=[hTb[hs][fc], wdb[s]], writes=[C.psb[py]])
                        Xs = C.X[:, t, dh * 512:(dh + 1) * 512]
                        S.op("dve", lambda e: e.scalar_tensor_tensor(out=Xs, in0=C.ps[py][:], scalar=comb[:, t, e_:e_ + 1],
                                                                     in1=Xs, op0=ALU.mult, op1=ALU.add),
                             reads=[C.psb[py], combb[t], C.Xb[t]], writes=[C.Xb[t]])
            if e_ + 2 < NEXP:
                load_expert(e_ + 2)
            if e_ == 0:
                ckpt(C, 12)
        S.barrier()


def da_layer(C, l):
    S, nc, dr = C.S, C.nc, C.dr
    j = l // 4
    lam_init = 0.8 - 0.6 * math.exp(-0.3 * l)
    H = 8
    C.psrot = Rot([0, 1])
    with contextlib.ExitStack() as st:
        sb = lambda n, sh, dt: C.sb("da%d_%s" % (l, n), sh, dt, st)
        norm_phase(C, dr["norm_mix"][l:l + 1, :], st)
        ckpt(C, 1)
        gv = sb("gv", (128, 256), F32); gvb = Buf()
        for c in range(2):
            S.dma("sp", gv[:, c * 64:(c + 1) * 64], dr["da_q_norm"][j:j + 1, :].partition_broadcast(128), writes=[gvb])
            S.dma("sp", gv[:, 128 + c * 64:128 + (c + 1) * 64], dr["da_k_norm"][j:j + 1, :].partition_broadcast(128),
                  writes=[gvb])
        S.op("dve", lambda e: e.tensor_scalar(out=gv[:, 0:128], in0=gv[:, 0:128], scalar1=0.125, scalar2=None, op0=ALU.mult),
             reads=[gvb], writes=[gvb])
        lm = sb("lm", (128, 4, 64), F32); lmb = Buf()
        for i, n in enumerate(("da_lambda_q1", "da_lambda_k1", "da_lambda_q2", "da_lambda_k2")):
            S.dma("sp", lm[:, i, :], dr[n][j:j + 1, :].partition_broadcast(128), writes=[lmb])
        lv = sb("lv", (128, 8), F32); lvb = Buf()
        S.op("dve", lambda e: e.tensor_tensor(out=lm[:, 0, :], in0=lm[:, 0, :], in1=lm[:, 1, :], op=ALU.mult),
             reads=[lmb], writes=[lmb])
        S.op("dve", lambda e: e.tensor_tensor(out=lm[:, 2, :], in0=lm[:, 2, :], in1=lm[:, 3, :], op=ALU.mult),
             reads=[lmb], writes=[lmb])
        S.op("dve", lambda e: e.tensor_reduce(out=lv[:, 0:1], in_=lm[:, 0, :], axis=AX.X, op=ALU.add), reads=[lmb], writes=[lvb])
        S.op("dve", lambda e: e.tensor_reduce(out=lv[:, 1:2], in_=lm[:, 2, :], axis=AX.X, op=ALU.add), reads=[lmb], writes=[lvb])
        S.op("act", lambda e: e.activation(out=lv[:, 2:4], in_=lv[:, 0:2], func=AF.Exp), reads=[lvb], writes=[lvb])
        S.op("dve", lambda e: e.tensor_tensor(out=lv[:, 4:5], in0=lv[:, 3:4], in1=lv[:, 2:3], op=ALU.subtract),
             reads=[lvb], writes=[lvb])
        S.op("dve", lambda e: e.tensor_scalar(out=lv[:, 4:5], in0=lv[:, 4:5], scalar1=-lam_init, scalar2=None, op0=ALU.add),
             reads=[lvb], writes=[lvb])
        sl = sb("sl", (128, 128), F32); slb = Buf()
        S.dma("sp", sl[:], dr["da_subln"][j:j + 1, :].partition_broadcast(128), writes=[slb])
        S.op("dve", lambda e: e.tensor_scalar(out=sl[:], in0=sl[:], scalar1=1.0 - lam_init, scalar2=None, op0=ALU.mult),
             reads=[slb], writes=[slb])
        eb = sb("eb", (128, H, 16), F32); ebb = Buf()
        for h in range(H):
            slope = 2.0 ** (-(h + 1))
            S.op("dve", lambda e: e.tensor_scalar(out=eb[:, h, :], in0=C.relpos[:], scalar1=slope, scalar2=None, op0=ALU.mult),
                 reads=[C.relposb], writes=[ebb])
        wo = sb("wo", (128, H, D), BF16); wob = Buf()
        S.dma("pool", wo[:], dr["da_w_out"][j].rearrange("(h e) d -> e h d", e=128), writes=[wob])
        ONT = sb("ONT", (128, H, T), BF16); ONTb = [[Buf() for _ in range(NT)] for _ in range(H)]
        wh = [sb("wh%d" % i, (128, KC, 384), BF16) for i in range(2)]; whb = [Buf() for _ in range(2)]
        QK = sb("QK", (64, 4, T), BF16)
        qTb = [Buf() for _ in range(NT)]; kTb = qTb
        mask01 = sb("mask01", (128, 128), BF16); mask01b = Buf()
        S.op("dve", lambda e: e.tensor_scalar(out=mask01[:], in0=C.maskneg[:], scalar1=-1.0, scalar2=None, op0=ALU.is_ge),
             reads=[C.masknegb], writes=[mask01b])
        vx = sb("vx", (128, NT, 130), BF16); vxb = [Buf() for _ in range(NT)]
        onesb = Buf()
        S.op("dve", lambda e: e.memset(vx[:, :, 128:130], 1.0), writes=vxb)
        qk = [sb("qk%d" % i, (128, 256), F32) for i in range(2)]; qkb = [Buf() for _ in range(2)]
        sq = [sb("sq%d" % i, (128, 256), F32) for i in range(2)]; sqb = [Buf() for _ in range(2)]
        qkn = [sb("qkn%d" % i, (128, 256), BF16) for i in range(2)]; qknb = [Buf() for _ in range(2)]
        ss4 = [sb("ss4%d" % i, (128, 4), F32) for i in range(2)]; ss4b = [Buf() for _ in range(2)]
        PT = [sb("PT%d" % i, (128, 128), BF16) for i in range(4)]
        PTr = Rot([(PT[i], Buf()) for i in range(4)])
        oc = [sb("oc%d" % i, (128, 2, 128), F32) for i in range(2)]; ocb = [Buf() for _ in range(2)]
        rs = [sb("rs%d" % i, (128, 4), F32) for i in range(2)]; rsb = [Buf() for _ in range(2)]
        od = [sb("od%d" % i, (128, 128), F32) for i in range(2)]; odb = [Buf() for _ in range(2)]
        on = [sb("on%d" % i, (128, 128), BF16) for i in range(2)]; onb = [Buf() for _ in range(2)]
        C.junk = sb("dajunk", (128, 128), BF16); C.junkb = Buf()

        def load_head(h):
            s = h % 2
            for part in range(3):
                S.dma("pool", wh[s][:, :, part * 128:(part + 1) * 128],
                      dr["da_w_in"][j][:, part * 1024 + h * 128: part * 1024 + (h + 1) * 128].rearrange("(k p) n -> p k n", p=128),
                      writes=[whb[s]])

        load_head(0)
        ckpt(C, 2)
        srot = Rot([4, 5, 6, 7])
        orot = Rot([2, 3])
        for h in range(H):
            s = h % 2
            if h + 1 < H:
                load_head(h + 1)
            for t in range(NT):
                u = t % 2
                pj = srot.next()
                ps, psb = C.ps[pj], C.psb[pj]
                for k in range(KC):
                    S.op("pe", lambda e: e.matmul(ps[:, 0:384], C.xnT[:, k, t * 128:(t + 1) * 128], wh[s][:, k, :],
                                                  start=(k == 0), stop=(k == KC - 1)),
                         reads=[C.xnTb[t], whb[s]], writes=[psb])
                S.op("act", lambda e: e.activation(out=qk[u][:], in_=ps[:, 0:256], func=AF.Copy), reads=[psb], writes=[qkb[u]])
                S.op("act", lambda e: e.activation(out=vx[:, t, 0:128], in_=ps[:, 256:384], func=AF.Copy),
                     reads=[psb], writes=[vxb[t]])
                S.op("pool", lambda e: e.tensor_tensor(out=sq[u][:], in0=qk[u][:], in1=qk[u][:], op=ALU.mult),
                     reads=[qkb[u]], writes=[sqb[u]])
                S.op("dve", lambda e: e.tensor_reduce(out=ss4[u][:], in_=sq[u][:].rearrange("p (g d) -> p g d", g=4),
                                                      axis=AX.X, op=ALU.add), reads=[sqb[u]], writes=[ss4b[u]])
                small_rstd(C, None, None, ss4[u][:], ss4b[u], 64.0)
                S.op("dve", lambda e: e.tensor_tensor(out=sq[u][:].rearrange("p (g d) -> p g d", g=4),
                                                      in0=qk[u][:].rearrange("p (g d) -> p g d", g=4),
                                                      in1=ss4[u][:].unsqueeze(2).to_broadcast([128, 4, 64]), op=ALU.mult),
                     reads=[qkb[u], ss4b[u]], writes=[sqb[u]])
                S.op("pool", lambda e: e.tensor_tensor(out=qkn[u][:], in0=sq[u][:], in1=gv[:], op=ALU.mult),
                     reads=[sqb[u], gvb], writes=[qknb[u]])
                pt, ptb = C.ps[1], C.psb[1]
                ptv = pt[:].bitcast(BF16)
                for jj in range(4):
                    S.op("pe", lambda e: e.transpose(ptv[0:64, jj * 128:(jj + 1) * 128], qkn[u][:, jj * 64:(jj + 1) * 64], C.identh[:]),
                         reads=[qknb[u], C.identhb], writes=[ptb])
                S.op("act", lambda e: e.activation(out=QK[:, :, t * 128:(t + 1) * 128],
                                                   in_=ptv[0:64, 0:512].rearrange("p (a b) -> p a b", a=4), func=AF.Copy),
                     reads=[ptb], writes=[qTb[t]])
            ckpt(C, 3)
            for jq in range(NT):
                if jq == 1:
                    ckpt(C, 4)
                u = jq % 2
                for c in range(2):
                    po = orot.next()
                    for i in range(jq + 1):
                        pS = srot.next()
                        diag = (i == jq)
                        S.op("pe", lambda e: e.matmul(C.ps[pS][:, 0:128], QK[:, 2 + c, i * 128:(i + 1) * 128],
                                                      QK[:, c, jq * 128:(jq + 1) * 128],
                                                      start=True, stop=True),
                             reads=[kTb[i], qTb[jq]], writes=[C.psb[pS]])
                        P, Pb = PTr.next()
                        S.op("act", lambda e: e.activation(out=P[:], in_=C.ps[pS][:, 0:128], func=AF.Exp,
                                                           bias=eb[:, h, jq - i:jq - i + 1], scale=1.0),
                             reads=[C.psb[pS], ebb], writes=[Pb])
                        if diag:
                            S.op("pool", lambda e: e.tensor_tensor(out=P[:], in0=P[:], in1=mask01[:], op=ALU.mult),
                                 reads=[Pb, mask01b], writes=[Pb])
                        S.op("pe", lambda e: e.matmul(C.ps[po][:, 0:129], P[:], vx[:, i, 0:129], start=(i == 0), stop=diag),
                             reads=[Pb, vxb[i]], writes=[C.psb[po]])
                    S.op("dve", lambda e: e.reciprocal(out=rs[u][:, c:c + 1], in_=C.ps[po][:, 128:129]),
                         reads=[C.psb[po]], writes=[rsb[u]])
                    S.op("dve", lambda e: e.tensor_scalar(out=oc[u][:, c, :], in0=C.ps[po][:, 0:128], scalar1=rs[u][:, c:c + 1],
                                                          scalar2=None, op0=ALU.mult),
                         reads=[C.psb[po], rsb[u]], writes=[ocb[u]])
                S.op("dve", lambda e: e.scalar_tensor_tensor(out=od[u][:], in0=oc[u][:, 1, :], scalar=lv[:, 4:5], in1=oc[u][:, 0, :],
                                                             op0=ALU.mult, op1=ALU.add),
                     reads=[ocb[u], lvb], writes=[odb[u]])
                S.op("act", lambda e: e.activation(out=C.junk[:, 0:128], in_=od[u][:], func=AF.Square, accum_out=rs[u][:, 2:3]),
                     reads=[odb[u]], writes=[C.junkb, rsb[u]])
                small_rstd(C, None, None, rs[u][:, 2:3], rsb[u], 128.0)
                S.op("dve", lambda e: e.scalar_tensor_tensor(out=on[u][:], in0=od[u][:], scalar=rs[u][:, 2:3], in1=sl[:],
                                                             op0=ALU.mult, op1=ALU.mult),
                     reads=[odb[u], rsb[u], slb], writes=[onb[u]])
                pt, ptb = C.ps[1], C.psb[1]
                ptv = pt[:].bitcast(BF16)
                S.op("pe", lambda e: e.transpose(ptv[:, 256:384], on[u][:], C.identh[:]), reads=[onb[u], C.identhb], writes=[ptb])
                S.op("act", lambda e: e.activation(out=ONT[:, h, jq * 128:(jq + 1) * 128], in_=ptv[:, 256:384], func=AF.Copy),
                     reads=[ptb], writes=[ONTb[h][jq]])
            ckpt(C, 5)
        yrot = Rot([4, 5, 6, 7])
        for t in range(NT):
            for dh in range(2):
                py = yrot.next()
                for h in range(H):
                    S.op("pe", lambda e: e.matmul(C.ps[py][:], ONT[:, h, t * 128:(t + 1) * 128], wo[:, h, dh * 512:(dh + 1) * 512],
                                                  start=(h == 0), stop=(h == H - 1)),
                         reads=[ONTb[h][t], wob], writes=[C.psb[py]])
                Xs = C.X[:, t, dh * 512:(dh + 1) * 512]
                S.op("dve", lambda e: e.tensor_tensor(out=Xs, in0=C.ps[py][:], in1=Xs, op=ALU.add),
                     reads=[C.psb[py], C.Xb[t]], writes=[C.Xb[t]])
        S.barrier()


def lru_layer(C, l):
    S, nc, dr = C.S, C.nc, C.dr
    j = l // 4
    C.psrot = Rot([0, 1])
    with contextlib.ExitStack() as st:
        sb = lambda n, sh, dt: C.sb("lru%d_%s" % (l, n), sh, dt, st)
        norm_phase(C, dr["norm_mix"][l:l + 1, :], st)
        pv = sb("pv", (128, 10, 8), F32); pvb = Buf()
        for jj in range(4):
            S.dma("sp", pv[:, jj, :], dr["lru_conv_w"][j, jj].rearrange("(c p) -> p c", p=128), writes=[pvb], slow=True)
        for idx, n in ((4, "lru_conv_b"), (5, "lru_b_a"), (6, "lru_b_x"), (7, "lru_lambda")):
            S.dma("sp", pv[:, idx, :], dr[n][j].rearrange("(c p) -> p c", p=128), writes=[pvb], slow=True)
        S.op("act", lambda e: e.activation(out=pv[:, 8, :], in_=pv[:, 7, :], func=AF.Exp, scale=-1.0), reads=[pvb], writes=[pvb])
        S.op("act", lambda e: e.activation(out=pv[:, 8, :], in_=pv[:, 8, :], func=AF.Ln, bias=1.0, scale=1.0), reads=[pvb], writes=[pvb])
        S.op("dve", lambda e: e.tensor_scalar(out=pv[:, 7, :], in0=pv[:, 8, :], scalar1=-8.0, scalar2=None, op0=ALU.mult),
             reads=[pvb], writes=[pvb])
        wa = sb("wa", (128, 4, 2, 256), BF16); wx = sb("wx", (128, 4, 2, 256), BF16); wab = Buf()
        S.dma("pool", wa[:], dr["lru_w_a"][j].rearrange("n (h p) d -> p n h d", p=128), writes=[wab])
        S.dma("pool", wx[:], dr["lru_w_x"][j].rearrange("n (h p) d -> p n h d", p=128), writes=[wab])
        win = sb("win", (128, KC, 512), BF16); winb = Buf()
        wout = [sb("wout%d" % i, (128, D), BF16) for i in range(2)]; woutb = [Buf() for _ in range(2)]
        rec = sb("rec", (128, 2, T), F32); recb = [Buf() for _ in range(2)]
        xr = sb("xr", (128, 2, T), F32); xrb = [Buf() for _ in range(2)]
        xrh = sb("xrh", (128, 2, T), BF16); xrhb = [Buf() for _ in range(2)]
        hg = [sb("hg%d" % i, (128, T), BF16) for i in range(2)]; hgb = [Buf() for _ in range(2)]
        hcar = sb("hcar", (128, 1), F32); hcarb = Buf()
        tmp = [sb("tmp%d" % i, (128, 512), F32) for i in range(10)]
        tmpr = Rot([(tmp[i], Buf()) for i in range(10)])
        prot = Rot([2, 3, 4, 5, 6, 7])
        TB = 4
        for n in range(4):
            S.dma("pool", win[:, :, 0:256], dr["lru_w_in"][j][:, n * 256:(n + 1) * 256].rearrange("(k p) c -> p k c", p=128),
                  writes=[winb])
            S.dma("pool", win[:, :, 256:512], dr["lru_w_in"][j][:, 1024 + n * 256:1024 + (n + 1) * 256].rearrange("(k p) c -> p k c", p=128),
                  writes=[winb])
            for h2 in range(2):
                cc = 2 * n + h2
                for tb in range(TB):
                    pr = prot.next()
                    for k in range(KC):
                        S.op("pe", lambda e: e.matmul(C.ps[pr][:], win[:, k, 256 + h2 * 128:256 + (h2 + 1) * 128],
                                                      C.xnT[:, k, tb * 512:(tb + 1) * 512], start=(k == 0), stop=(k == KC - 1)),
                             reads=[winb] + C.xnTb[tb * 4:tb * 4 + 4], writes=[C.psb[pr]])
                    S.op("act", lambda e: e.activation(out=rec[:, h2, tb * 512:(tb + 1) * 512], in_=C.ps[pr][:], func=AF.Copy),
                         reads=[C.psb[pr]], writes=[recb[h2]])
                S.op("dve", lambda e: e.tensor_scalar(out=xr[:, h2, :], in0=rec[:, h2, :], scalar1=pv[:, 3, cc:cc + 1],
                                                      scalar2=pv[:, 4, cc:cc + 1], op0=ALU.mult, op1=ALU.add),
                     reads=[recb[h2], pvb], writes=[xrb[h2]])
                for sh in (1, 2, 3):
                    S.op("dve", lambda e: e.scalar_tensor_tensor(out=xr[:, h2, sh:T], in0=rec[:, h2, 0:T - sh],
                                                                 scalar=pv[:, 3 - sh, cc:cc + 1], in1=xr[:, h2, sh:T],
                                                                 op0=ALU.mult, op1=ALU.add),
                         reads=[recb[h2], pvb, xrb[h2]], writes=[xrb[h2]])
                S.op("pool", lambda e: e.tensor_copy(out=xrh[:, h2, :], in_=xr[:, h2, :]), reads=[xrb[h2]], writes=[xrhb[h2]])
            for h2 in range(2):
                cc = 2 * n + h2
                ws = cc % 2
                S.dma("pool", wout[ws][:], dr["lru_w_out"][j][cc * 128:(cc + 1) * 128, :], writes=[woutb[ws]])
                for tb in range(TB):
                    sl_ = slice(tb * 512, (tb + 1) * 512)
                    pa = prot.next(); px = prot.next(); pg = prot.next()
                    for ic in range(2):
                        S.op("pe", lambda e: e.matmul(C.ps[pa][:], wa[:, n, ic, h2 * 128:(h2 + 1) * 128], xrh[:, ic, sl_],
                                                      start=(ic == 0), stop=(ic == 1)),
                             reads=[wab, xrhb[ic]], writes=[C.psb[pa]])
                    for ic in range(2):
                        S.op("pe", lambda e: e.matmul(C.ps[px][:], wx[:, n, ic, h2 * 128:(h2 + 1) * 128], xrh[:, ic, sl_],
                                                      start=(ic == 0), stop=(ic == 1)),
                             reads=[wab, xrhb[ic]], writes=[C.psb[px]])
                    for k in range(KC):
                        S.op("pe", lambda e: e.matmul(C.ps[pg][:], win[:, k, h2 * 128:(h2 + 1) * 128],
                                                      C.xnT[:, k, sl_], start=(k == 0), stop=(k == KC - 1)),
                             reads=[winb] + C.xnTb[tb * 4:tb * 4 + 4], writes=[C.psb[pg]])
                    r_, rb_ = tmpr.next(); i_, ib_ = tmpr.next(); a_, ab_ = tmpr.next(); t_, tb_ = tmpr.next()
                    g_, gb_ = tmpr.next(); u_, ub_ = tmpr.next()
                    S.op("act", lambda e: e.activation(out=r_[:], in_=C.ps[pa][:], func=AF.Sigmoid, bias=pv[:, 5, cc:cc + 1], scale=1.0),
                         reads=[C.psb[pa], pvb], writes=[rb_])
                    S.op("act", lambda e: e.activation(out=i_[:], in_=C.ps[px][:], func=AF.Sigmoid, bias=pv[:, 6, cc:cc + 1], scale=1.0),
                         reads=[C.psb[px], pvb], writes=[ib_])
                    S.op("act", lambda e: e.activation(out=a_[:], in_=r_[:], func=AF.Exp, scale=pv[:, 7, cc:cc + 1]),
                         reads=[rb_, pvb], writes=[ab_])
                    S.op("pool", lambda e: e.tensor_tensor(out=t_[:], in0=a_[:], in1=a_[:], op=ALU.mult), reads=[ab_], writes=[tb_])
                    S.op("dve", lambda e: e.tensor_scalar(out=t_[:], in0=t_[:], scalar1=-1.0, scalar2=1.0, op0=ALU.mult, op1=ALU.add),
                         reads=[tb_], writes=[tb_])
                    S.op("act", lambda e: e.activation(out=t_[:], in_=t_[:], func=AF.Sqrt), reads=[tb_], writes=[tb_])
                    S.op("pool", lambda e: e.tensor_tensor(out=i_[:], in0=i_[:], in1=xr[:, h2, sl_], op=ALU.mult),
                         reads=[ib_, xrb[h2]], writes=[ib_])
                    S.op("pool", lambda e: e.tensor_tensor(out=t_[:], in0=t_[:], in1=i_[:], op=ALU.mult), reads=[tb_, ib_], writes=[tb_])
                    if tb == 0:
                        S.op("dve", lambda e: e.tensor_tensor_scan(out=r_[:], data0=a_[:], data1=t_[:], initial=0.0,
                                                                   op0=ALU.mult, op1=ALU.add),
                             reads=[ab_, tb_], writes=[rb_])
                    else:
                        S.op("dve", lambda e: e.tensor_tensor_scan(out=r_[:], data0=a_[:], data1=t_[:], initial=hcar[:, 0:1],
                                                                   op0=ALU.mult, op1=ALU.add),
                             reads=[ab_, tb_, hcarb], writes=[rb_])
                    S.op("dve", lambda e: e.tensor_copy(out=hcar[:, 0:1], in_=r_[:, 511:512]), reads=[rb_], writes=[hcarb])
                    S.op("act", lambda e: e.activation(out=g_[:], in_=C.ps[pg][:], func=AF.Copy), reads=[C.psb[pg]], writes=[gb_])
                    S.op("pool", lambda e: e.tensor_tensor(out=u_[:], in0=g_[:], in1=g_[:], op=ALU.mult), reads=[gb_], writes=[ub_])
                    S.op("dve", lambda e: e.tensor_scalar(out=u_[:], in0=u_[:], scalar1=0.044715, scalar2=1.0, op0=ALU.mult, op1=ALU.add),
                         reads=[ub_], writes=[ub_])
                    S.op("pool", lambda e: e.tensor_tensor(out=u_[:], in0=u_[:], in1=g_[:], op=ALU.mult), reads=[ub_, gb_], writes=[ub_])
                    S.op("act", lambda e: e.activation(out=u_[:], in_=u_[:], func=AF.Sigmoid, scale=1.5957691216057308),
                         reads=[ub_], writes=[ub_])
                    S.op("pool", lambda e: e.tensor_tensor(out=u_[:], in0=u_[:], in1=g_[:], op=ALU.mult), reads=[ub_, gb_], writes=[ub_])
                    S.op("dve", lambda e: e.tensor_tensor(out=hg[ws][:, sl_], in0=u_[:], in1=r_[:], op=ALU.mult),
                         reads=[ub_, rb_], writes=[hgb[ws]])
                for t in range(NT):
                    for dh in range(2):
                        py = prot.next()
                        S.op("pe", lambda e: e.matmul(C.ps[py][:], hg[ws][:, t * 128:(t + 1) * 128], wout[ws][:, dh * 512:(dh + 1) * 512],
                                                      start=True, stop=True),
                             reads=[hgb[ws], woutb[ws]], writes=[C.psb[py]])
                        Xs = C.X[:, t, dh * 512:(dh + 1) * 512]
                        S.op("dve", lambda e: e.tensor_tensor(out=Xs, in0=C.ps[py][:], in1=Xs, op=ALU.add),
                             reads=[C.psb[py], C.Xb[t]], writes=[C.Xb[t]])
        S.barrier()


def gdn_layer(C, l):
    S, nc, dr = C.S, C.nc, C.dr
    j = l // 4
    H = 8
    C.psrot = Rot([0, 1])
    with contextlib.ExitStack() as st:
        sb = lambda n, sh, dt: C.sb("gdn%d_%s" % (l, n), sh, dt, st)
        norm_phase(C, dr["norm_mix"][l:l + 1, :], st)
        prot = Rot(list(range(8)))
        W = dr["gdn_w_in"][j]
        def cload(name, dt):
            t_ = sb(name, (128, 128), dt); b_ = Buf()
            S.dma("pool", t_[:], dr["c_" + name], writes=[b_])
            return t_, b_
        mup, mupb = cload("mup", BF16)
        mfull, mfullb = cload("mfull", BF16)
        mbu, mbub = cload("mbu", F32)
        mbls, mblsb = cload("mbls", F32)
        half = sb("half", (128, 2), F32); halfb = Buf()
        S.dma("sp", half[:], dr["c_half"], writes=[halfb])
        ones = sb("ones", (128, 128), BF16); onesb = Buf()
        S.op("dve", lambda e: e.memset(ones[:], 1.0), writes=[onesb])
        cw = sb("cw", (128, 4, 24), F32); cwb = Buf()
        for jj in range(4):
            S.dma("sp", cw[:, jj, :], dr["gdn_conv_w"][j, jj].rearrange("(c p) -> p c", p=128), writes=[cwb], slow=True)
        hp = sb("hp", (128, 3, 8), F32); hpb = Buf()
        S.dma("sp", hp[:, 0, :], dr["gdn_a_log"][j:j + 1, :].partition_broadcast(128), writes=[hpb])
        S.dma("sp", hp[:, 1, :], dr["gdn_dt_bias"][j:j + 1, :].partition_broadcast(128), writes=[hpb])
        S.op("act", lambda e: e.activation(out=hp[:, 0, :], in_=hp[:, 0, :], func=AF.Exp), reads=[hpb], writes=[hpb])
        S.op("dve", lambda e: e.tensor_scalar(out=hp[:, 0, :], in0=hp[:, 0, :], scalar1=-1.0, scalar2=None, op0=ALU.mult),
             reads=[hpb], writes=[hpb])
        og_ = sb("ogain", (128, 128), F32); ogb = Buf()
        S.dma("sp", og_[:], dr["gdn_out_norm"][j:j + 1, :].partition_broadcast(128), writes=[ogb])
        wba = sb("wba", (128, KC, 16), BF16); wbab = Buf()
        S.dma("pool", wba[:], W[:, 4096:4112].rearrange("(k p) n -> p k n", p=128), writes=[wbab])
        BETA = sb("BETA", (128, NT, 8), F32); G = sb("G", (128, NT, 8), F32); gb_ = Buf()
        for t in range(NT):
            pi = prot.next()
            for k in range(KC):
                S.op("pe", lambda e: e.matmul(C.ps[pi][:, 0:16], C.xnT[:, k, t * 128:(t + 1) * 128], wba[:, k, :],
                                              start=(k == 0), stop=(k == KC - 1)),
                     reads=[C.xnTb[t], wbab], writes=[C.psb[pi]])
            S.op("act", lambda e: e.activation(out=BETA[:, t, :], in_=C.ps[pi][:, 0:8], func=AF.Sigmoid), reads=[C.psb[pi]], writes=[gb_])
            S.op("dve", lambda e: e.tensor_tensor(out=G[:, t, :], in0=C.ps[pi][:, 8:16], in1=hp[:, 1, :], op=ALU.add),
                 reads=[C.psb[pi], hpb], writes=[gb_])
        S.op("act", lambda e: e.activation(out=G[:], in_=G[:], func=AF.Exp), reads=[gb_], writes=[gb_])
        S.op("act", lambda e: e.activation(out=G[:], in_=G[:], func=AF.Ln, bias=1.0, scale=1.0), reads=[gb_], writes=[gb_])
        S.op("dve", lambda e: e.tensor_tensor(out=G[:], in0=G[:], in1=hp[:, 0, :].unsqueeze(1).to_broadcast([128, NT, 8]), op=ALU.mult),
             reads=[gb_, hpb], writes=[gb_])
        Gh = sb("Gh", (128, NT, 8), BF16); Gl = sb("Gl", (128, NT, 8), BF16)
        S.op("dve", lambda e: e.tensor_copy(out=Gh[:], in_=G[:]), reads=[gb_], writes=[gb_])
        S.op("dve", lambda e: e.tensor_tensor(out=Gl[:], in0=G[:], in1=Gh[:], op=ALU.subtract), reads=[gb_], writes=[gb_])
        GC = sb("GC", (128, NT, 8), F32); GL = sb("GL", (128, NT, 8), F32)
        for t in range(NT):
            pi = prot.next()
            S.op("pe", lambda e: e.matmul(C.ps[pi][:, 0:8], mup[:], Gh[:, t, :], start=True, stop=False), reads=[mupb, gb_], writes=[C.psb[pi]])
            S.op("pe", lambda e: e.matmul(C.ps[pi][:, 0:8], mup[:], Gl[:, t, :], start=False, stop=True), reads=[mupb, gb_], writes=[C.psb[pi]])
            S.op("act", lambda e: e.activation(out=GC[:, t, :], in_=C.ps[pi][:, 0:8], func=AF.Copy), reads=[C.psb[pi]], writes=[gb_])
            pi = prot.next()
            S.op("pe", lambda e: e.matmul(C.ps[pi][:, 0:8], mfull[:], Gh[:, t, :], start=True, stop=False), reads=[mfullb, gb_], writes=[C.psb[pi]])
            S.op("pe", lambda e: e.matmul(C.ps[pi][:, 0:8], mfull[:], Gl[:, t, :], start=False, stop=True), reads=[mfullb, gb_], writes=[C.psb[pi]])
            S.op("act", lambda e: e.activation(out=GL[:, t, :], in_=C.ps[pi][:, 0:8], func=AF.Copy), reads=[C.psb[pi]], writes=[gb_])
        NGC = sb("NGC", (128, NT, 8), F32); BEG = sb("BEG", (128, NT, 8), F32)
        DC0 = sb("DC0", (128, NT, 8), F32)
        S.op("dve", lambda e: e.tensor_scalar(out=NGC[:], in0=GC[:], scalar1=-1.0, scalar2=None, op0=ALU.mult), reads=[gb_], writes=[gb_])
        S.op("act", lambda e: e.activation(out=BEG[:], in_=GC[:], func=AF.Exp), reads=[gb_], writes=[gb_])
        S.op("dve", lambda e: e.tensor_tensor(out=BEG[:], in0=BEG[:], in1=BETA[:], op=ALU.mult), reads=[gb_], writes=[gb_])
        S.op("dve", lambda e: e.tensor_tensor(out=DC0[:], in0=GL[:], in1=GC[:], op=ALU.subtract), reads=[gb_], writes=[gb_])
        S.op("act", lambda e: e.activation(out=DC0[:], in_=DC0[:], func=AF.Exp), reads=[gb_], writes=[gb_])
        wh = sb("wh", (128, KC, 512), BF16); whb = Buf()
        pre = sb("pre", (128, T), F32); preb = Buf()
        post = sb("post", (128, T), F32); postb = Buf()
        sqs = [sb("sqb%d" % i, (128, 512), BF16) for i in range(2)]
        sqr = Rot([(sqs[i], Buf()) for i in range(2)])
        qT = sb("qT", (128, T), BF16); kT = sb("kT", (128, T), BF16); vT = sb("vT", (128, T), BF16)
        qTb = Buf(); kTb = Buf(); vTb = Buf()
        ktok = sb("ktok", (128, NT, 128), BF16); vtok = sb("vtok", (128, NT, 128), BF16); tokb = [Buf() for _ in range(NT)]
        U_ = sb("U", (128, NT, 128), F32); wT = sb("wT", (128, NT, 128), BF16); qgT = sb("qgT", (128, NT, 128), BF16)
        aqT = sb("aqT", (128, NT, 128), BF16); kd0 = sb("kd0", (128, NT, 128), BF16)
        intb = [Buf() for _ in range(NT)]
        egl = sb("egl", (128, NT, 2), F32); eglb = [Buf() for _ in range(NT)]
        otokb = [preb for _ in range(NT)]
        otok = lambda rows, t: pre[rows, t * 128:(t + 1) * 128]
        OGT = sb("OGT", (128, T), BF16); OGTb = [Buf() for _ in range(NT)]
        wo = [sb("wo%d" % i, (128, D), BF16) for i in range(2)]; wob = [Buf() for _ in range(2)]
        Sf = sb("Sf", (128, 128), F32); Sfb = Buf()
        Sb = [sb("Sb%d" % i, (128, 128), BF16) for i in range(2)]; Sbb = [Buf() for _ in range(2)]
        NTMP = 8
        tf = [sb("tf%d" % i, (128, 128), F32) for i in range(NTMP)]
        tfr = Rot([(tf[i], Buf()) for i in range(NTMP)])
        th = [sb("th%d" % i, (128, 128), BF16) for i in range(20)]
        thr = Rot([(th[i], Buf()) for i in range(20)])
        bigf = [sb("bigf%d" % i, (128, 512), F32) for i in range(2)]
        bigr = Rot([(bigf[i], Buf()) for i in range(2)])

        for h in range(H):
            for part in range(4):
                col = part * 1024 + h * 128
                S.dma("pool", wh[:, :, part * 128:(part + 1) * 128], W[:, col:col + 128].rearrange("(k p) n -> p k n", p=128),
                      writes=[whb])
            for part, (dst, dstb) in enumerate(((qT, qTb), (kT, kTb), (vT, vTb))):
                cc = part * 8 + h
                for tb in range(4):
                    pi = prot.next()
                    for k in range(KC):
                        S.op("pe", lambda e: e.matmul(C.ps[pi][:], wh[:, k, part * 128:(part + 1) * 128], C.xnT[:, k, tb * 512:(tb + 1) * 512],
                                                      start=(k == 0), stop=(k == KC - 1)),
                             reads=[whb] + C.xnTb[tb * 4:tb * 4 + 4], writes=[C.psb[pi]])
                    S.op("act", lambda e: e.activation(out=pre[:, tb * 512:(tb + 1) * 512], in_=C.ps[pi][:], func=AF.Copy),
                         reads=[C.psb[pi]], writes=[preb])
                S.op("dve", lambda e: e.tensor_scalar(out=post[:], in0=pre[:], scalar1=cw[:, 3, cc:cc + 1], scalar2=None, op0=ALU.mult),
                     reads=[preb, cwb], writes=[postb])
                for sh in (1, 2, 3):
                    S.op("dve", lambda e: e.scalar_tensor_tensor(out=post[:, sh:T], in0=pre[:, 0:T - sh], scalar=cw[:, 3 - sh, cc:cc + 1],
                                                                 in1=post[:, sh:T], op0=ALU.mult, op1=ALU.add),
                         reads=[preb, cwb, postb], writes=[postb])
                if part == 2:
                    S.op("act", lambda e: e.activation(out=dst[:], in_=post[:], func=AF.Silu), reads=[postb], writes=[dstb])
                else:
                    S.op("act", lambda e: e.activation(out=post[:], in_=post[:], func=AF.Silu), reads=[postb], writes=[postb])
                    for tb in range(4):
                        sl_ = slice(tb * 512, (tb + 1) * 512)
                        pi = prot.next()
                        sq_, sqbb = sqr.next()
                        S.op("pool", lambda e: e.tensor_tensor(out=sq_[:], in0=post[:, sl_], in1=post[:, sl_], op=ALU.mult), reads=[postb], writes=[sqbb])
                        S.op("pe", lambda e: e.matmul(C.ps[pi][:], ones[:], sq_[:], start=True, stop=True),
                             reads=[onesb, sqbb], writes=[C.psb[pi]])
                        r_, rb_ = bigr.next()
                        S.op("dve", lambda e: e.tensor_scalar(out=r_[:], in0=C.ps[pi][:], scalar1=EPS, scalar2=None, op0=ALU.add),
                             reads=[C.psb[pi]], writes=[rb_])
                        S.op("act", lambda e: e.activation(out=r_[:], in_=r_[:], func=AF.Ln), reads=[rb_], writes=[rb_])
                        S.op("act", lambda e: e.activation(out=r_[:], in_=r_[:], func=AF.Exp, scale=-0.5), reads=[rb_], writes=[rb_])
                        qs = (128.0 ** -0.5) if part == 0 else 1.0
                        S.op("dve", lambda e: e.scalar_tensor_tensor(out=dst[:, sl_], in0=post[:, sl_], scalar=qs, in1=r_[:],
                                                                     op0=ALU.mult, op1=ALU.mult),
                             reads=[postb, rb_], writes=[dstb])
            for t in range(NT):
                pi = prot.next()
                pv_ = C.ps[pi][:].bitcast(BF16)
                S.op("pe", lambda e: e.transpose(pv_[:, 0:128], kT[:, t * 128:(t + 1) * 128], C.identh[:]), reads=[kTb, C.identhb], writes=[C.psb[pi]])
                S.op("pe", lambda e: e.transpose(pv_[:, 128:256], vT[:, t * 128:(t + 1) * 128], C.identh[:]), reads=[vTb, C.identhb], writes=[C.psb[pi]])
                S.op("act", lambda e: e.activation(out=ktok[:, t, :], in_=pv_[:, 0:128], func=AF.Copy), reads=[C.psb[pi]], writes=[tokb[t]])
                S.op("dve", lambda e: e.tensor_copy(out=vtok[:, t, :], in_=pv_[:, 128:256]), reads=[C.psb[pi]], writes=[tokb[t]])
            for t in range(NT):
                ts_ = slice(t * 128, (t + 1) * 128)
                hh = slice(h, h + 1)
                gh_, ghb_ = thr.next(); gl_, glb_ = thr.next()
                S.op("pool", lambda e: e.tensor_copy(out=gh_[:], in_=Gh[:, t, hh].to_broadcast([128, 128])), reads=[gb_], writes=[ghb_])
                S.op("pool", lambda e: e.tensor_copy(out=gl_[:], in_=Gl[:, t, hh].to_broadcast([128, 128])), reads=[gb_], writes=[glb_])
                pbc = prot.next()
                S.op("pe", lambda e: e.matmul(C.ps[pbc][:, 0:128], gh_[:], mup[:], start=True, stop=False), reads=[ghb_, mupb], writes=[C.psb[pbc]])
                S.op("pe", lambda e: e.matmul(C.ps[pbc][:, 0:128], gl_[:], mup[:], start=False, stop=True), reads=[glb_, mupb], writes=[C.psb[pbc]])
                pgl = prot.next()
                S.op("pe", lambda e: e.matmul(C.ps[pgl][:, 0:128], gh_[:], mfull[:], start=True, stop=False), reads=[ghb_, mfullb], writes=[C.psb[pgl]])
                S.op("pe", lambda e: e.matmul(C.ps[pgl][:, 0:128], gl_[:], mfull[:], start=False, stop=True), reads=[glb_, mfullb], writes=[C.psb[pgl]])
                S.op("act", lambda e: e.activation(out=egl[:, t, 0:1], in_=C.ps[pgl][:, 0:1], func=AF.Exp), reads=[C.psb[pgl]], writes=[eglb[t]])
                S.op("act", lambda e: e.activation(out=egl[:, t, 1:2], in_=C.ps[pgl][:, 64:65], func=AF.Exp), reads=[C.psb[pgl]], writes=[eglb[t]])
                e2, e2b = tfr.next(); e1, e1b = tfr.next(); eg, egb = tfr.next()
                S.op("dve", lambda e: e.tensor_tensor(out=e2[:], in0=C.ps[pbc][:, 0:128], in1=mbu[:], op=ALU.add), reads=[C.psb[pbc], mbub], writes=[e2b])
                S.op("act", lambda e: e.activation(out=e2[:], in_=e2[:], func=AF.Exp, bias=NGC[:, t, hh], scale=1.0), reads=[e2b, gb_], writes=[e2b])
                S.op("dve", lambda e: e.scalar_tensor_tensor(out=e1[:], in0=C.ps[pbc][:, 0:128], scalar=-1.0, in1=mbls[:], op0=ALU.mult, op1=ALU.add),
                     reads=[C.psb[pbc], mblsb], writes=[e1b])
                S.op("act", lambda e: e.activation(out=e1[:], in_=e1[:], func=AF.Exp, bias=GC[:, t, hh], scale=1.0), reads=[e1b, gb_], writes=[e1b])
                S.op("act", lambda e: e.activation(out=eg[:], in_=C.ps[pbc][:, 0:128], func=AF.Exp), reads=[C.psb[pbc]], writes=[egb])
                S.op("pool", lambda e: e.tensor_tensor(out=qgT[:, t, :], in0=qT[:, ts_], in1=eg[:], op=ALU.mult), reads=[qTb, egb], writes=[intb[t]])
                pk = prot.next()
                S.op("pe", lambda e: e.matmul(C.ps[pk][:, 0:128], kT[:, ts_], kT[:, ts_], start=True, stop=True), reads=[kTb], writes=[C.psb[pk]])
                Lb, Lbb = thr.next(); Ub, Ubb = thr.next(); Pb, Pbb = thr.next(); Qb, Qbb = thr.next()
                S.op("dve", lambda e: e.scalar_tensor_tensor(out=Lb[:], in0=C.ps[pk][:, 0:128], scalar=BETA[:, t, hh], in1=e1[:],
                                                             op0=ALU.mult, op1=ALU.mult), reads=[C.psb[pk], gb_, e1b], writes=[Lbb])
                pt_ = prot.next()
                ptv = C.ps[pt_][:].bitcast(BF16)
                S.op("pe", lambda e: e.transpose(ptv[:, 0:128], Lb[:], C.identh[:]), reads=[Lbb, C.identhb], writes=[C.psb[pt_]])
                S.op("act", lambda e: e.activation(out=Ub[:], in_=ptv[:, 0:128], func=AF.Copy), reads=[C.psb[pt_]], writes=[Ubb])
                S.op("pool", lambda e: e.tensor_tensor(out=Qb[:], in0=C.identh[:], in1=Lb[:], op=ALU.subtract), reads=[C.identhb, Lbb], writes=[Qbb])
                S.op("pool", lambda e: e.tensor_tensor(out=Pb[:], in0=C.identh[:], in1=Ub[:], op=ALU.subtract), reads=[C.identhb, Ubb], writes=[Pbb])
                for step in range(5):
                    L2, L2b = thr.next(); U2, U2b = thr.next()
                    p1 = prot.next(); p2 = prot.next()
                    S.op("pe", lambda e: e.matmul(C.ps[p1][:, 0:128], Ub[:], Lb[:], start=True, stop=True), reads=[Ubb, Lbb], writes=[C.psb[p1]])
                    S.op("pe", lambda e: e.matmul(C.ps[p2][:, 0:128], Lb[:], Ub[:], start=True, stop=True), reads=[Ubb, Lbb], writes=[C.psb[p2]])
                    S.op("act", lambda e: e.activation(out=L2[:], in_=C.ps[p1][:, 0:128], func=AF.Copy), reads=[C.psb[p1]], writes=[L2b])
                    S.op("dve", lambda e: e.tensor_copy(out=U2[:], in_=C.ps[p2][:, 0:128]), reads=[C.psb[p2]], writes=[U2b])
                    Pn, Pnb = thr.next()
                    p3 = prot.next()
                    S.op("pe", lambda e: e.matmul(C.ps[p3][:, 0:128], Qb[:], U2[:], start=True, stop=True), reads=[Qbb, U2b], writes=[C.psb[p3]])
                    S.op("dve", lambda e: e.tensor_tensor(out=Pn[:], in0=C.ps[p3][:, 0:128], in1=Pb[:], op=ALU.add), reads=[C.psb[p3], Pbb], writes=[Pnb])
                    if step < 4:
                        Qn, Qnb = thr.next()
                        p4 = prot.next()
                        S.op("pe", lambda e: e.matmul(C.ps[p4][:, 0:128], Pb[:], L2[:], start=True, stop=True), reads=[Pbb, L2b], writes=[C.psb[p4]])
                        S.op("dve", lambda e: e.tensor_tensor(out=Qn[:], in0=C.ps[p4][:, 0:128], in1=Qb[:], op=ALU.add), reads=[C.psb[p4], Qbb], writes=[Qnb])
                        Qb, Qbb = Qn, Qnb
                    Pb, Pbb = Pn, Pnb
                    Lb, Lbb, Ub, Ubb = L2, L2b, U2, U2b
                pq = prot.next()
                S.op("pe", lambda e: e.matmul(C.ps[pq][:, 0:128], kT[:, ts_], qT[:, ts_], start=True, stop=True), reads=[kTb, qTb], writes=[C.psb[pq]])
                S.op("dve", lambda e: e.tensor_tensor(out=aqT[:, t, :], in0=C.ps[pq][:, 0:128], in1=e2[:], op=ALU.mult), reads=[C.psb[pq], e2b], writes=[intb[t]])
                vb_, vbb_ = thr.next(); kg_, kgb_ = thr.next()
                S.op("act", lambda e: e.activation(out=vb_[:], in_=vtok[:, t, :], func=AF.Copy, scale=BETA[:, t, hh]),
                     reads=[tokb[t], gb_], writes=[vbb_])
                S.op("act", lambda e: e.activation(out=kg_[:], in_=ktok[:, t, :], func=AF.Copy, scale=BEG[:, t, hh]),
                     reads=[tokb[t], gb_], writes=[kgb_])
                S.op("act", lambda e: e.activation(out=kd0[:, t, :], in_=ktok[:, t, :], func=AF.Copy, scale=DC0[:, t, hh]),
                     reads=[tokb[t], gb_], writes=[intb[t]])
                pu = prot.next(); pw = prot.next()
                S.op("pe", lambda e: e.matmul(C.ps[pu][:, 0:128], Pb[:], vb_[:], start=True, stop=True), reads=[Pbb, vbb_], writes=[C.psb[pu]])
                S.op("pe", lambda e: e.matmul(C.ps[pw][:, 0:128], kg_[:], Pb[:], start=True, stop=True), reads=[Pbb, kgb_], writes=[C.psb[pw]])
                S.op("act", lambda e: e.activation(out=U_[:, t, :], in_=C.ps[pu][:, 0:128], func=AF.Copy), reads=[C.psb[pu]], writes=[intb[t]])
                S.op("act", lambda e: e.activation(out=wT[:, t, :], in_=C.ps[pw][:, 0:128], func=AF.Copy), reads=[C.psb[pw]], writes=[intb[t]])
            S.op("dve", lambda e: e.memset(Sf[:], 0.0), writes=[Sfb])
            S.op("dve", lambda e: e.memset(Sb[0][:], 0.0), writes=[Sbb[0]])
            si = 0
            for t in range(NT):
                for hf in range(2):
                    cur, curb = Sb[si % 2], Sbb[si % 2]
                    nxt, nxtb = Sb[(si + 1) % 2], Sbb[(si + 1) % 2]
                    si += 1
                    pws = prot.next(); po = prot.next(); psu = prot.next()
                    S.op("pe", lambda e: e.matmul(C.ps[pws][:, 0:128], wT[:, t, :], cur[:], start=True, stop=True), reads=[intb[t], curb], writes=[C.psb[pws]])
                    vn0, vn0b = tfr.next()
                    vn, vnb = thr.next()
                    S.op("dve", lambda e: e.tensor_tensor(out=vn0[:], in0=U_[:, t, :], in1=C.ps[pws][:, 0:128], op=ALU.subtract),
                         reads=[intb[t], C.psb[pws]], writes=[vn0b])
                    S.op("dve", lambda e: e.tensor_scalar(out=vn[:], in0=vn0[:], scalar1=half[:, hf:hf + 1], scalar2=None, op0=ALU.mult),
                         reads=[vn0b, halfb], writes=[vnb])
                    S.op("pe", lambda e: e.matmul(C.ps[po][:, 0:128], qgT[:, t, :], cur[:], start=True, stop=False), reads=[intb[t], curb], writes=[C.psb[po]])
                    S.op("pe", lambda e: e.matmul(C.ps[po][:, 0:128], aqT[:, t, :], vn[:], start=False, stop=True), reads=[intb[t], vnb], writes=[C.psb[po]])
                    rows = slice(hf * 64, (hf + 1) * 64)
                    S.op("act", lambda e: e.activation(out=otok(rows, t), in_=C.ps[po][rows, 0:128], func=AF.Copy), reads=[C.psb[po]], writes=[otokb[t]])
                    kd = kd0
                    S.op("pe", lambda e: e.matmul(C.ps[psu][:, 0:128], kd[:, t, :], vn[:], start=True, stop=True), reads=[intb[t], vnb], writes=[C.psb[psu]])
                    S.op("dve", lambda e: e.scalar_tensor_tensor(out=Sf[:], in0=Sf[:], scalar=egl[:, t, hf:hf + 1], in1=C.ps[psu][:, 0:128],
                                                                 op0=ALU.mult, op1=ALU.add), reads=[Sfb, eglb[t], C.psb[psu]], writes=[Sfb])
                    S.op("act", lambda e: e.activation(out=nxt[:], in_=Sf[:], func=AF.Copy), reads=[Sfb], writes=[nxtb])
            for t in range(NT):
                pz = prot.next()
                for k in range(KC):
                    S.op("pe", lambda e: e.matmul(C.ps[pz][:, 0:128], C.xnT[:, k, t * 128:(t + 1) * 128], wh[:, k, 384:512],
                                                  start=(k == 0), stop=(k == KC - 1)), reads=[C.xnTb[t], whb], writes=[C.psb[pz]])
                z_, zb_ = tfr.next(); o2, o2b = tfr.next(); ss_, ssb_ = tfr.next()
                S.op("act", lambda e: e.activation(out=z_[:], in_=C.ps[pz][:, 0:128], func=AF.Silu), reads=[C.psb[pz]], writes=[zb_])
                S.op("act", lambda e: e.activation(out=o2[:], in_=otok(slice(0, 128), t), func=AF.Square, accum_out=ss_[:, 0:1]),
                     reads=[otokb[t]], writes=[o2b, ssb_])
                small_rstd(C, None, None, ss_[:, 0:1], ssb_, 128.0)
                S.op("dve", lambda e: e.scalar_tensor_tensor(out=o2[:], in0=otok(slice(0, 128), t), scalar=ss_[:, 0:1], in1=og_[:], op0=ALU.mult, op1=ALU.mult),
                     reads=[otokb[t], ssb_, ogb], writes=[o2b])
                ob, obb = thr.next()
                S.op("pool", lambda e: e.tensor_tensor(out=ob[:], in0=o2[:], in1=z_[:], op=ALU.mult), reads=[o2b, zb_], writes=[obb])
                pt_ = prot.next()
                ptv = C.ps[pt_][:].bitcast(BF16)
                S.op("pe", lambda e: e.transpose(ptv[:, 0:128], ob[:], C.identh[:]), reads=[obb, C.identhb], writes=[C.psb[pt_]])
                S.op("act", lambda e: e.activation(out=OGT[:, t * 128:(t + 1) * 128], in_=ptv[:, 0:128], func=AF.Copy),
                     reads=[C.psb[pt_]], writes=[OGTb[t]])
            ws_ = h % 2
            S.dma("pool", wo[ws_][:], dr["gdn_w_out"][j][h * 128:(h + 1) * 128, :], writes=[wob[ws_]])
            for t in range(NT):
                for dh in range(2):
                    py = prot.next()
                    S.op("pe", lambda e: e.matmul(C.ps[py][:], OGT[:, t * 128:(t + 1) * 128], wo[ws_][:, dh * 512:(dh + 1) * 512],
                                                  start=True, stop=True), reads=[OGTb[t], wob[ws_]], writes=[C.psb[py]])
                    Xs = C.X[:, t, dh * 512:(dh + 1) * 512]
                    S.op("dve", lambda e: e.tensor_tensor(out=Xs, in0=C.ps[py][:], in1=Xs, op=ALU.add), reads=[C.psb[py], C.Xb[t]], writes=[C.Xb[t]])
        S.barrier()


def nsa_layer(C, l):
    S, nc, dr = C.S, C.nc, C.dr
    jl = l // 4
    C.psrot = Rot([0, 1])
    W = dr["nsa_w_in"][jl]
    with contextlib.ExitStack() as st:
        sb = lambda n, sh, dt: C.sb("nsa%d_%s" % (l, n), sh, dt, st)
        norm_phase(C, dr["norm_mix"][l:l + 1, :], st)
        prot = Rot([0, 1, 2])
        orot = Rot([3, 4, 5, 6])
        OT = sb("OT", (128, 4, T), BF16); OTb = [Buf() for _ in range(NT)]
        kcmpT = sb("kcmpT", (64, 2, 128), BF16); vcmp = sb("vcmp", (128, 2, 64), BF16); cmpb = Buf()
        GATE = sb("GATE", (128, NT, 48), F32); gateb = Buf()
        mask01 = sb("mask01", (128, 128), BF16); maskw = sb("maskw", (128, 128), BF16); mkb = Buf()
        S.op("dve", lambda e: e.tensor_scalar(out=mask01[:], in0=C.maskneg[:], scalar1=-1.0, scalar2=None, op0=ALU.is_ge),
             reads=[C.masknegb], writes=[mkb])
        S.dma("pool", maskw[:], dr["c_maskw"], writes=[mkb])
        kg = sb("kg", (128, 3, 64), F32); kgb = Buf()
        for i in range(3):
            S.dma("sp", kg[:, i, :], dr["nsa_k_norm"][jl, i:i + 1, :].partition_broadcast(128), writes=[kgb])
        tf = [sb("tf%d" % i, (128, 512), F32) for i in range(2)]
        tfr = Rot([(tf[i], Buf()) for i in range(2)])
        tfs = [sb("tfs%d" % i, (128, 128), F32) for i in range(4)]
        tfsr = Rot([(tfs[i], Buf()) for i in range(4)])
        th = [sb("th%d" % i, (128, 512), BF16) for i in range(2)]
        thr = Rot([(th[i], Buf()) for i in range(2)])
        ths = [sb("ths%d" % i, (128, 128), BF16) for i in range(3)]
        thsr = Rot([(ths[i], Buf()) for i in range(3)])
        sm = [sb("sm%d" % i, (128, 16), F32) for i in range(4)]
        smr = Rot([(sm[i], Buf()) for i in range(4)])

        with contextlib.ExitStack() as sa:
            sba = lambda n, sh, dt: C.sb("nsaA%d_%s" % (l, n), sh, dt, sa)
            wgl = sba("wgl", (128, KC, 48), BF16); wglb = Buf()
            S.dma("pool", wgl[:], W[:, 1792:1840].rearrange("(k p) n -> p k n", p=128), writes=[wglb])
            wA = sba("wA", (128, KC, 256), BF16); wAb = Buf()
            S.dma("pool", wA[:], W[:, 1024:1280].rearrange("(k p) n -> p k n", p=128), writes=[wAb])
            w1 = [sba("w1_%d" % c, (64, 32, 256), BF16) for c in range(2)]; w1b = Buf()
            w2 = [sba("w2_%d" % c, (128, 2, 64), BF16) for c in range(2)]
            posT = sba("posT", (64, 2, 32), BF16)
            for c in range(2):
                S.dma("pool", w1[c][:], dr["nsa_cmp_w1"][jl, c].rearrange("(l d) f -> d l f", d=64), writes=[w1b])
                S.dma("pool", w2[c][:], dr["nsa_cmp_w2"][jl, c].rearrange("(a p) n -> p a n", p=128), writes=[w1b])
                S.dma("pool", posT[:, c, :], dr["nsa_cmp_pos"][jl, c].rearrange("l d -> d l"), writes=[w1b], slow=True)
            xcT = sba("xcT", (64, 4, T), BF16); xcTb = Buf()
            for t in range(NT):
                pi = prot.next()
                for k in range(KC):
                    S.op("pe", lambda e: e.matmul(C.ps[pi][:, 0:48], C.xnT[:, k, t * 128:(t + 1) * 128], wgl[:, k, :],
                                                  start=(k == 0), stop=(k == KC - 1)), reads=[C.xnTb[t], wglb], writes=[C.psb[pi]])
                S.op("act", lambda e: e.activation(out=GATE[:, t, :], in_=C.ps[pi][:, 0:48], func=AF.Sigmoid), reads=[C.psb[pi]], writes=[gateb])
                pi = prot.next()
                for k in range(KC):
                    S.op("pe", lambda e: e.matmul(C.ps[pi][:, 0:256], C.xnT[:, k, t * 128:(t + 1) * 128], wA[:, k, :],
                                                  start=(k == 0), stop=(k == KC - 1)), reads=[C.xnTb[t], wAb], writes=[C.psb[pi]])
                xb_, xbb_ = thr.next()
                S.op("act", lambda e: e.activation(out=xb_[:, 0:256], in_=C.ps[pi][:, 0:256], func=AF.Copy), reads=[C.psb[pi]], writes=[xbb_])
                pt_ = prot.next()
                ptv = C.ps[pt_][:].bitcast(BF16)
                for a in range(4):
                    S.op("pe", lambda e: e.transpose(ptv[0:64, a * 128:(a + 1) * 128], xb_[:, a * 64:(a + 1) * 64], C.identh[:]),
                         reads=[xbb_, C.identhb], writes=[C.psb[pt_]])
                S.op("act", lambda e: e.activation(out=xcT[:, :, t * 128:(t + 1) * 128],
                                                   in_=ptv[0:64, 0:512].rearrange("p (a b) -> p a b", a=4), func=AF.Copy),
                     reads=[C.psb[pt_]], writes=[xcTb])
            for c in range(2):
                pb_, pbb_ = smr.next()
                for fc in range(2):
                    pi = prot.next()
                    for l_ in range(32):
                        S.op("pe", lambda e: e.matmul(C.ps[pi][:, 0:1], w1[c][:, l_, fc * 128:(fc + 1) * 128], posT[:, c, l_:l_ + 1],
                                                      start=(l_ == 0), stop=(l_ == 31)), reads=[w1b], writes=[C.psb[pi]])
                    S.op("act", lambda e: e.activation(out=pb_[:, fc:fc + 1], in_=C.ps[pi][:, 0:1], func=AF.Copy), reads=[C.psb[pi]], writes=[pbb_])
                for g in range(2):
                    src = c * 2 + g
                    hT, hTb = thr.next()
                    for fc in range(2):
                        pi = prot.next()
                        for l_ in range(32):
                            S.op("pe", lambda e: e.matmul(C.ps[pi][:, 0:127], w1[c][:, l_, fc * 128:(fc + 1) * 128],
                                                          xcT[:, src, l_:l_ + 16 * 126 + 1:16], start=(l_ == 0), stop=(l_ == 31)),
                                 reads=[w1b, xcTb], writes=[C.psb[pi]])
                        S.op("act", lambda e: e.activation(out=hT[:, fc * 128:fc * 128 + 127], in_=C.ps[pi][:, 0:127], func=AF.Silu,
                                                           bias=pb_[:, fc:fc + 1], scale=1.0), reads=[C.psb[pi], pbb_], writes=[hTb])
                    pi = prot.next()
                    for fc in range(2):
                        S.op("pe", lambda e: e.matmul(C.ps[pi][0:127, 0:64], hT[:, fc * 128:fc * 128 + 127], w2[c][:, fc, :],
                                                      start=(fc == 0), stop=(fc == 1)), reads=[hTb, w1b], writes=[C.psb[pi]])
                    if c == 1:
                        S.op("act", lambda e: e.activation(out=vcmp[0:127, g, :], in_=C.ps[pi][0:127, 0:64], func=AF.Copy),
                             reads=[C.psb[pi]], writes=[cmpb])
                    else:
                        kc_, kcb_ = tfsr.next(); ss_, ssb_ = smr.next()
                        S.op("dve", lambda e: e.memset(kc_[:, 0:128], 0.0), writes=[kcb_])
                        S.op("act", lambda e: e.activation(out=kc_[0:127, 0:64], in_=C.ps[pi][0:127, 0:64], func=AF.Copy), reads=[C.psb[pi]], writes=[kcb_])
                        S.op("act", lambda e: e.activation(out=kc_[:, 64:128], in_=kc_[:, 0:64], func=AF.Square, accum_out=ss_[:, 0:1]),
                             reads=[kcb_], writes=[kcb_, ssb_])
                        small_rstd(C, None, None, ss_[:, 0:1], ssb_, 64.0)
                        kb_, kbb_ = thsr.next()
                        S.op("dve", lambda e: e.scalar_tensor_tensor(out=kb_[:, 0:64], in0=kc_[:, 0:64], scalar=ss_[:, 0:1], in1=kg[:, 0, :],
                                                                     op0=ALU.mult, op1=ALU.mult), reads=[kcb_, ssb_, kgb], writes=[kbb_])
                        pt_ = prot.next()
                        ptv = C.ps[pt_][:].bitcast(BF16)
                        S.op("pe", lambda e: e.transpose(ptv[0:64, 0:128], kb_[:, 0:64], C.identh[:]), reads=[kbb_, C.identhb], writes=[C.psb[pt_]])
                        S.op("act", lambda e: e.activation(out=kcmpT[:, g, :], in_=ptv[0:64, 0:128], func=AF.Copy), reads=[C.psb[pt_]], writes=[cmpb])
            S.barrier()
        keep = sb("keep", (128, 512), BF16); addc = sb("addc", (128, 512), BF16); dcbase = sb("dcbase", (128, 127), F32)
        ovl = sb("ovl", (128, 32), BF16); e64 = sb("e64", (64, T), BF16); cB = Buf()
        S.dma("pool", keep[:], dr["c_keep"], writes=[cB]); S.dma("pool", addc[:], dr["c_add"], writes=[cB])
        S.dma("sp", dcbase[:], dr["c_dcbase"], writes=[cB])
        S.dma("pool", ovl[:], dr["c_overlap"], writes=[cB]); S.dma("pool", e64[:], dr["c_e64"], writes=[cB])
        S.op("dve", lambda e: e.memset(vcmp[127:128, :, :], 0.0) if False else e.memset(sm[0][:, 0:1], 0.0), writes=[smr.items[0][1]])
        gvq = sb("gvq", (128, 512), F32); gvk = sb("gvk", (128, 128), F32); gvb = Buf()
        for p_ in range(8):
            S.dma("sp", gvq[:, p_ * 64:(p_ + 1) * 64], dr["nsa_q_norm"][jl:jl + 1, :].partition_broadcast(128), writes=[gvb])
        S.op("dve", lambda e: e.tensor_scalar(out=gvq[:], in0=gvq[:], scalar1=0.125, scalar2=None, op0=ALU.mult), reads=[gvb], writes=[gvb])
        S.op("dve", lambda e: e.tensor_copy(out=gvk[:, 0:64], in_=kg[:, 1, :]), reads=[kgb], writes=[gvb])
        S.op("dve", lambda e: e.tensor_copy(out=gvk[:, 64:128], in_=kg[:, 2, :]), reads=[kgb], writes=[gvb])
        eb = sb("eb", (128, 16, 16), F32); ebb = Buf()
        slopes = [2.0 ** (-8.0 * (i + 1) / 16.0) for i in range(16)]
        for hd in range(16):
            S.op("dve", lambda e: e.tensor_scalar(out=eb[:, hd, :], in0=C.relpos[:], scalar1=-64.0, scalar2=float(np.float32(slopes[hd])),
                                                  op0=ALU.add, op1=ALU.mult),
                 reads=[C.relposb], writes=[ebb])
        wq = sb("wq", (128, KC, 512), BF16); wkv = sb("wkv", (128, KC, 256), BF16); wqb = Buf()
        qTg = sb("qTg", (64, 8, T), BF16); qTb = [Buf() for _ in range(NT)]
        ksT = sb("ksT", (64, T), BF16); kwT = sb("kwT", (64, T), BF16); kTb = [Buf() for _ in range(NT)]
        vsx = sb("vsx", (128, NT, 66), BF16); vwx = sb("vwx", (128, NT, 66), BF16); vxb = [Buf() for _ in range(NT)]
        S.op("dve", lambda e: e.memset(vsx[:, :, 64:66], 1.0), writes=vxb)
        S.op("dve", lambda e: e.memset(vwx[:, :, 64:66], 1.0), writes=vxb)
        selb64 = sb("selb64", (64, 128), BF16); selbb = Buf()
        S.op("dve", lambda e: e.memset(selb64[:], 0.0), writes=[selbb])
        ocm = sb("ocm", (128, 8, 64), F32); ocmb = Buf()
        dm = sb("dm", (128, 127), F32); dmb = Buf()
        PT = [sb("PT%d" % i, (128, 128), BF16) for i in range(5)]
        PTr = Rot([(PT[i], Buf()) for i in range(5)])
        opair = [sb("opair%d" % i, (128, 128), BF16) for i in range(2)]; opb = [Buf() for _ in range(2)]
        oh = [sb("oh%d" % i, (128, 64), F32) for i in range(2)]; ohb = [Buf() for _ in range(2)]
        wos = [sb("wo%d" % i, (128, D), BF16) for i in range(2)]
        wor = Rot([(wos[i], Buf()) for i in range(2)])
        for g in range(2):
            S.dma("pool", wq[:], W[:, g * 512:(g + 1) * 512].rearrange("(k p) n -> p k n", p=128), writes=[wqb])
            for a, col in enumerate((1280, 1536, 1408, 1664)):
                S.dma("pool", wkv[:, :, a * 64:(a + 1) * 64], W[:, col + g * 64:col + (g + 1) * 64].rearrange("(k p) n -> p k n", p=128),
                      writes=[wqb])
            for t in range(NT):
                pi = prot.next()
                for k in range(KC):
                    S.op("pe", lambda e: e.matmul(C.ps[pi][:], C.xnT[:, k, t * 128:(t + 1) * 128], wq[:, k, :],
                                                  start=(k == 0), stop=(k == KC - 1)), reads=[C.xnTb[t], wqb], writes=[C.psb[pi]])
                qf, qfb = tfr.next(); q2, q2b = tfr.next(); ss_, ssb_ = smr.next()
                S.op("act", lambda e: e.activation(out=qf[:], in_=C.ps[pi][:], func=AF.Copy), reads=[C.psb[pi]], writes=[qfb])
                S.op("pool", lambda e: e.tensor_tensor(out=q2[:], in0=qf[:], in1=qf[:], op=ALU.mult), reads=[qfb], writes=[q2b])
                S.op("dve", lambda e: e.tensor_reduce(out=ss_[:, 0:8], in_=q2[:].rearrange("p (g d) -> p g d", g=8), axis=AX.X, op=ALU.add),
                     reads=[q2b], writes=[ssb_])
                small_rstd(C, None, None, ss_[:, 0:8], ssb_, 64.0)
                S.op("dve", lambda e: e.tensor_tensor(out=q2[:].rearrange("p (g d) -> p g d", g=8), in0=qf[:].rearrange("p (g d) -> p g d", g=8),
                                                      in1=ss_[:, 0:8].unsqueeze(2).to_broadcast([128, 8, 64]), op=ALU.mult),
                     reads=[qfb, ssb_], writes=[q2b])
                qb_, qbb_ = thr.next()
                S.op("pool", lambda e: e.tensor_tensor(out=qb_[:], in0=q2[:], in1=gvq[:], op=ALU.mult), reads=[q2b, gvb], writes=[qbb_])
                for half_ in range(2):
                    pt_ = prot.next()
                    ptv = C.ps[pt_][:].bitcast(BF16)
                    for a in range(4):
                        hh_ = half_ * 4 + a
                        S.op("pe", lambda e: e.transpose(ptv[0:64, a * 128:(a + 1) * 128], qb_[:, hh_ * 64:(hh_ + 1) * 64], C.identh[:]),
                             reads=[qbb_, C.identhb], writes=[C.psb[pt_]])
                    S.op("act", lambda e: e.activation(out=qTg[:, half_ * 4:(half_ + 1) * 4, t * 128:(t + 1) * 128],
                                                       in_=ptv[0:64, 0:512].rearrange("p (a b) -> p a b", a=4), func=AF.Copy),
                         reads=[C.psb[pt_]], writes=[qTb[t]])
                pi = prot.next()
                for k in range(KC):
                    S.op("pe", lambda e: e.matmul(C.ps[pi][:, 0:256], C.xnT[:, k, t * 128:(t + 1) * 128], wkv[:, k, :],
                                                  start=(k == 0), stop=(k == KC - 1)), reads=[C.xnTb[t], wqb], writes=[C.psb[pi]])
                kf, kfb = tfsr.next(); k2, k2b = tfsr.next(); ss_, ssb_ = smr.next()
                S.op("act", lambda e: e.activation(out=kf[:, 0:128], in_=C.ps[pi][:, 0:128], func=AF.Copy), reads=[C.psb[pi]], writes=[kfb])
                S.op("act", lambda e: e.activation(out=vsx[:, t, 0:64], in_=C.ps[pi][:, 128:192], func=AF.Copy), reads=[C.psb[pi]], writes=[vxb[t]])
                S.op("act", lambda e: e.activation(out=vwx[:, t, 0:64], in_=C.ps[pi][:, 192:256], func=AF.Copy), reads=[C.psb[pi]], writes=[vxb[t]])
                S.op("pool", lambda e: e.tensor_tensor(out=k2[:, 0:128], in0=kf[:, 0:128], in1=kf[:, 0:128], op=ALU.mult), reads=[kfb], writes=[k2b])
                S.op("dve", lambda e: e.tensor_reduce(out=ss_[:, 0:2], in_=k2[:, 0:128].rearrange("p (g d) -> p g d", g=2), axis=AX.X, op=ALU.add),
                     reads=[k2b], writes=[ssb_])
                small_rstd(C, None, None, ss_[:, 0:2], ssb_, 64.0)
                S.op("dve", lambda e: e.tensor_tensor(out=k2[:, 0:128].rearrange("p (g d) -> p g d", g=2),
                                                      in0=kf[:, 0:128].rearrange("p (g d) -> p g d", g=2),
                                                      in1=ss_[:, 0:2].unsqueeze(2).to_broadcast([128, 2, 64]), op=ALU.mult),
                     reads=[kfb, ssb_], writes=[k2b])
                kb_, kbb_ = thsr.next()
                S.op("pool", lambda e: e.tensor_tensor(out=kb_[:, 0:128], in0=k2[:, 0:128], in1=gvk[:], op=ALU.mult), reads=[k2b, gvb], writes=[kbb_])
                pt_ = prot.next()
                ptv = C.ps[pt_][:].bitcast(BF16)
                for a in range(2):
                    S.op("pe", lambda e: e.transpose(ptv[0:64, a * 128:(a + 1) * 128], kb_[:, a * 64:(a + 1) * 64], C.identh[:]),
                         reads=[kbb_, C.identhb], writes=[C.psb[pt_]])
                S.op("act", lambda e: e.activation(out=ksT[:, t * 128:(t + 1) * 128], in_=ptv[0:64, 0:128], func=AF.Copy), reads=[C.psb[pt_]], writes=[kTb[t]])
                S.op("dve", lambda e: e.tensor_copy(out=kwT[:, t * 128:(t + 1) * 128], in_=ptv[0:64, 128:256]), reads=[C.psb[pt_]], writes=[kTb[t]])
            for jq in range(NT):
                qs_ = slice(jq * 128, (jq + 1) * 128)
                d0, d0b = tfsr.next(); d1, d1b = tfsr.next()
                S.op("dve", lambda e: e.tensor_scalar(out=d0[:, 0:127], in0=dcbase[:], scalar1=float(128 * jq), scalar2=None, op0=ALU.add),
                     reads=[cB], writes=[d0b])
                S.op("dve", lambda e: e.tensor_scalar(out=d1[:, 0:127], in0=d0[:, 0:127], scalar1=0.0, scalar2=None, op0=ALU.is_lt),
                     reads=[d0b], writes=[d1b])
                S.op("dve", lambda e: e.scalar_tensor_tensor(out=dm[:], in0=d1[:, 0:127], scalar=1e9, in1=d0[:, 0:127], op0=ALU.mult, op1=ALU.add),
                     reads=[d0b, d1b], writes=[dmb])
                for p_ in range(8):
                    hd = g * 8 + p_
                    pi = prot.next()
                    S.op("pe", lambda e: e.matmul(C.ps[pi][:, 0:127], qTg[:, p_, qs_], kcmpT[:, g, 0:127], start=True, stop=True),
                         reads=[qTb[jq], cmpb], writes=[C.psb[pi]])
                    sc, scb = tfsr.next(); ss_, ssb_ = smr.next()
                    S.op("dve", lambda e: e.scalar_tensor_tensor(out=sc[:, 0:127], in0=dm[:], scalar=-float(np.float32(slopes[hd])), in1=C.ps[pi][:, 0:127],
                                                                 op0=ALU.mult, op1=ALU.add), reads=[dmb, C.psb[pi]], writes=[scb])
                    S.op("act", lambda e: e.activation(out=sc[:, 0:127], in_=sc[:, 0:127], func=AF.Exp, accum_out=ss_[:, 0:1]),
                         reads=[scb], writes=[scb, ssb_])
                    S.op("dve", lambda e: e.tensor_scalar(out=ss_[:, 0:1], in0=ss_[:, 0:1], scalar1=1e-30, scalar2=None, op0=ALU.max),
                         reads=[ssb_], writes=[ssb_])
                    S.op("dve", lambda e: e.reciprocal(out=ss_[:, 0:1], in_=ss_[:, 0:1]), reads=[ssb_], writes=[ssb_])
                    pb_, pbb_ = thsr.next()
                    S.op("dve", lambda e: e.memset(pb_[:, 127:128], 0.0), writes=[pbb_])
                    S.op("dve", lambda e: e.tensor_scalar(out=pb_[:, 0:127], in0=sc[:, 0:127], scalar1=ss_[:, 0:1], scalar2=None, op0=ALU.mult),
                         reads=[scb, ssb_], writes=[pbb_])
                    pt_ = prot.next()
                    ptv = C.ps[pt_][:].bitcast(BF16)
                    S.op("pe", lambda e: e.transpose(ptv[:, 0:128], pb_[:, 0:128], C.identh[:]), reads=[pbb_, C.identhb], writes=[C.psb[pt_]])
                    pT, pTb = PTr.next()
                    S.op("act", lambda e: e.activation(out=pT[:], in_=ptv[:, 0:128], func=AF.Copy), reads=[C.psb[pt_]], writes=[pTb])
                    po = prot.next()
                    S.op("pe", lambda e: e.matmul(C.ps[po][:, 0:64], pT[0:127, :], vcmp[0:127, g, :], start=True, stop=True),
                         reads=[pTb, cmpb], writes=[C.psb[po]])
                    S.op("pe", lambda e: e.matmul(C.ps[7][:, 0:32], pT[0:127, :], ovl[0:127, :], start=(p_ == 0), stop=(p_ == 7)),
                         reads=[pTb, cB], writes=[C.psb[7]])
                    S.op("dve", lambda e: e.tensor_scalar(out=ocm[:, p_, :], in0=C.ps[po][:, 0:64], scalar1=GATE[:, jq, hd * 3:hd * 3 + 1], scalar2=None,
                                                          op0=ALU.mult), reads=[C.psb[po], gateb], writes=[ocmb])
                scr, scrb = tfsr.next(); v8, v8b = smr.next()
                S.op("dve", lambda e: e.tensor_tensor(out=scr[:, 0:32], in0=C.ps[7][:, 0:32], in1=keep[:, jq * 32:(jq + 1) * 32], op=ALU.mult),
                     reads=[C.psb[7], cB], writes=[scrb])
                S.op("dve", lambda e: e.tensor_tensor(out=scr[:, 0:32], in0=scr[:, 0:32], in1=addc[:, jq * 32:(jq + 1) * 32], op=ALU.add),
                     reads=[scrb, cB], writes=[scrb])
                S.op("dve", lambda e: e.max(out=v8[:, 0:8], in_=scr[:, 0:32]), reads=[scrb], writes=[v8b])
                S.op("dve", lambda e: e.tensor_scalar(out=scr[:, 0:32], in0=scr[:, 0:32], scalar1=v8[:, 7:8], scalar2=None, op0=ALU.is_ge),
                     reads=[scrb, v8b], writes=[scrb])
                sbq, sbqb = thsr.next()
                S.op("dve", lambda e: e.tensor_scalar(out=sbq[:, 0:32], in0=scr[:, 0:32], scalar1=-1.0, scalar2=30000.0, op0=ALU.add, op1=ALU.mult),
                     reads=[scrb], writes=[sbqb])
                pt_ = prot.next()
                ptv = C.ps[pt_][:].bitcast(BF16)
                S.op("pe", lambda e: e.transpose(ptv[0:32, 0:128], sbq[:, 0:32], C.identh[:]), reads=[sbqb, C.identhb], writes=[C.psb[pt_]])
                S.op("act", lambda e: e.activation(out=selb64[0:32, :], in_=ptv[0:32, 0:128], func=AF.Copy), reads=[C.psb[pt_]], writes=[selbb])
                for p_ in range(8):
                    hd = g * 8 + p_
                    u = p_ % 2
                    posel = orot.next(); powin = orot.next()
                    for i in range(jq + 1):
                        ks_ = slice(i * 128, (i + 1) * 128)
                        pS = prot.next()
                        S.op("pe", lambda e: e.matmul(C.ps[pS][:, 0:128], ksT[:, ks_], qTg[:, p_, qs_], start=True, stop=False),
                             reads=[kTb[i], qTb[jq]], writes=[C.psb[pS]])
                        S.op("pe", lambda e: e.matmul(C.ps[pS][:, 0:128], e64[:, ks_], selb64[:], start=False, stop=True),
                             reads=[cB, selbb], writes=[C.psb[pS]])
                        P, Pb = PTr.next()
                        S.op("act", lambda e: e.activation(out=P[:], in_=C.ps[pS][:, 0:128], func=AF.Exp, bias=eb[:, hd, jq - i:jq - i + 1], scale=1.0),
                             reads=[C.psb[pS], ebb], writes=[Pb])
                        if i == jq:
                            S.op("pool", lambda e: e.tensor_tensor(out=P[:], in0=P[:], in1=mask01[:], op=ALU.mult), reads=[Pb, mkb], writes=[Pb])
                        S.op("pe", lambda e: e.matmul(C.ps[posel][:, 0:65], P[:], vsx[:, i, 0:65], start=(i == 0), stop=(i == jq)),
                             reads=[Pb, vxb[i]], writes=[C.psb[posel]])
                    i0 = max(0, jq - 4)
                    for i in range(i0, jq + 1):
                        ks_ = slice(i * 128, (i + 1) * 128)
                        pS = prot.next()
                        S.op("pe", lambda e: e.matmul(C.ps[pS][:, 0:128], kwT[:, ks_], qTg[:, p_, qs_], start=True, stop=True),
                             reads=[kTb[i], qTb[jq]], writes=[C.psb[pS]])
                        P, Pb = PTr.next()
                        S.op("act", lambda e: e.activation(out=P[:], in_=C.ps[pS][:, 0:128], func=AF.Exp, bias=eb[:, hd, jq - i:jq - i + 1], scale=1.0),
                             reads=[C.psb[pS], ebb], writes=[Pb])
                        if i == jq:
                            S.op("pool", lambda e: e.tensor_tensor(out=P[:], in0=P[:], in1=mask01[:], op=ALU.mult), reads=[Pb, mkb], writes=[Pb])
                        elif i == jq - 4:
                            S.op("pool", lambda e: e.tensor_tensor(out=P[:], in0=P[:], in1=maskw[:], op=ALU.mult), reads=[Pb, mkb], writes=[Pb])
                        S.op("pe", lambda e: e.matmul(C.ps[powin][:, 0:65], P[:], vwx[:, i, 0:65], start=(i == i0), stop=(i == jq)),
                             reads=[Pb, vxb[i]], writes=[C.psb[powin]])
                    fs, fsb = smr.next()
                    S.op("dve", lambda e: e.reciprocal(out=fs[:, 0:1], in_=C.ps[posel][:, 64:65]), reads=[C.psb[posel]], writes=[fsb])
                    S.op("dve", lambda e: e.reciprocal(out=fs[:, 1:2], in_=C.ps[powin][:, 64:65]), reads=[C.psb[powin]], writes=[fsb])
                    S.op("dve", lambda e: e.tensor_tensor(out=fs[:, 0:2], in0=fs[:, 0:2], in1=GATE[:, jq, hd * 3 + 1:hd * 3 + 3], op=ALU.mult),
                         reads=[fsb, gateb], writes=[fsb])
                    S.op("dve", lambda e: e.scalar_tensor_tensor(out=oh[u][:], in0=C.ps[posel][:, 0:64], scalar=fs[:, 0:1], in1=ocm[:, p_, :],
                                                                 op0=ALU.mult, op1=ALU.add), reads=[C.psb[posel], fsb, ocmb], writes=[ohb[u]])
                    pr_ = (p_ // 2) % 2
                    S.op("dve", lambda e: e.scalar_tensor_tensor(out=opair[pr_][:, u * 64:(u + 1) * 64], in0=C.ps[powin][:, 0:64], scalar=fs[:, 1:2],
                                                                 in1=oh[u][:], op0=ALU.mult, op1=ALU.add),
                         reads=[C.psb[powin], fsb, ohb[u]], writes=[opb[pr_]])
                    if u == 1:
                        pt_ = prot.next()
                        ptv = C.ps[pt_][:].bitcast(BF16)
                        S.op("pe", lambda e: e.transpose(ptv[:, 0:128], opair[pr_][:], C.identh[:]), reads=[opb[pr_], C.identhb], writes=[C.psb[pt_]])
                        S.op("act", lambda e: e.activation(out=OT[:, p_ // 2, qs_], in_=ptv[:, 0:128], func=AF.Copy),
                             reads=[C.psb[pt_]], writes=[OTb[jq]])
            for a in range(4):
                wo_, wob_ = wor.next()
                row0 = (g * 4 + a) * 128
                S.dma("pool", wo_[:], dr["nsa_w_out"][jl][row0:row0 + 128, :], writes=[wob_])
                for t in range(NT):
                    for dh in range(2):
                        py = prot.next()
                        S.op("pe", lambda e: e.matmul(C.ps[py][:], OT[:, a, t * 128:(t + 1) * 128], wo_[:, dh * 512:(dh + 1) * 512],
                                                      start=True, stop=True), reads=[OTb[t], wob_], writes=[C.psb[py]])
                        Xs = C.X[:, t, dh * 512:(dh + 1) * 512]
                        S.op("dve", lambda e: e.tensor_tensor(out=Xs, in0=C.ps[py][:], in1=Xs, op=ALU.add),
                             reads=[C.psb[py], C.Xb[t]], writes=[C.Xb[t]])
        S.barrier()
_NC_CACHE = {}


def run(inputs, nlayers=4, trace=False):
    if nlayers not in _NC_CACHE:
        _NC_CACHE[nlayers] = build(nlayers)
    nc = _NC_CACHE[nlayers]
    consts = host_consts()
    x = np.asarray(inputs["x"], dtype=np.float32)
    shared = {}
    for name in nc._in_names:
        if name == "x":
            continue
        if name in consts:
            shared[name] = consts[name]
        elif "@" in name:
            base, l = name.split("@")
            shared[name.replace("@", "_L")] = np.ascontiguousarray(np.asarray(inputs[base], dtype=np.float32)[int(l)])
        else:
            shared[name] = np.ascontiguousarray(np.asarray(inputs[name], dtype=np.float32))
    in_maps = []
    for c in range(8):
        m = {"x": np.ascontiguousarray(x[c])}
        m.update(shared)
        in_maps.append(m)
    res = run_bass_kernel_spmd(nc, in_maps, core_ids=list(range(8)), trace=trace)
    out = np.stack([np.asarray(r["out"]) for r in res.results], axis=0).astype(np.float32)
    return out, res


def kernel(**inputs):
    out, _ = run(inputs, 4)
    return out
```

```python
import contextlib
import math
import numpy as np
import concourse.bass as bass
import concourse.mybir as mybir
from concourse.bass_utils import run_bass_kernel_spmd

F32 = mybir.dt.float32
BF16 = mybir.dt.bfloat16
AF = mybir.ActivationFunctionType
ALU = mybir.AluOpType
AX = mybir.AxisListType

T = 2048
D = 1024
NT = 16
KC = 8
EPS = 1e-6
NEXP = 16
DEXP = 512


class Buf:
    __slots__ = ("name", "w", "r", "excl")

    def __init__(self, name="", excl=False):
        self.name = name
        self.excl = excl
        self.w = None
        self.r = None


class _Rec:
    def __init__(self):
        self.call = None

    def __getattr__(self, name):
        def f(*args, **kwargs):
            self.call = (name, args, kwargs)
            return self
        return f


class _Op:
    __slots__ = ("idx", "eng", "call", "deps", "dur", "is_dma", "slow", "start", "tok", "nsucc", "tcls")


def _free_size(ap):
    sh = ap.shape
    n = 1
    for d in sh[1:]:
        n *= int(d)
    return n


class Sched:
    WINDOW = 48

    def __init__(self, nc, es, ndma=28):
        self.nc = nc
        self.engs = {"pe": nc.tensor, "dve": nc.vector, "act": nc.scalar, "pool": nc.gpsimd, "sp": nc.sync}
        self.sem = {k: es.enter_context(nc.semaphore("c_" + k)) for k in self.engs}
        self.cnt = {k: 0 for k in self.engs}
        self.dsem = [es.enter_context(nc.semaphore("d%d" % i)) for i in range(ndma)]
        self.dcnt = [0] * ndma
        self.dnext = 0
        self.dnext_sw = 0
        self.waited = {k: {} for k in self.engs}
        self.ninst = 0
        self.muted = False
        self.ops = []
        self.seg = 0
        import os
        self.reorder = os.environ.get("KREORDER", "1") == "1"

    def _record(self, e, call, reads, writes, is_dma=False, slow=False):
        op = _Op()
        op.idx = len(self.ops)
        op.eng = e
        op.call = call
        op.is_dma = is_dma
        op.slow = slow
        deps = set()
        seg = self.seg
        xr = [b for b in reads if b.excl and b not in writes]
        if xr:
            writes = writes + xr
            reads = [b for b in reads if not b.excl]
        for b in reads:
            if b.w is not None and b.w[0] == seg:
                deps.add(b.w[1])
        for b in writes:
            if b.w is not None and b.w[0] == seg:
                deps.add(b.w[1])
            if b.r and b.r[0] == seg:
                deps.update(b.r[1])
        op.deps = deps
        for b in reads:
            if not b.r or b.r[0] != seg:
                b.r = (seg, [])
            b.r[1].append(op.idx)
        for b in writes:
            b.w = (seg, op.idx)
            b.r = (seg, [])
        name, args, kwargs = call
        if is_dma:
            out = kwargs["out"]
            nbytes = _free_size(out) * int(out.shape[0]) * 4
            op.dur = 2.0 + nbytes / 150e3
        elif e == "pe":
            if name == "transpose":
                op.dur = 0.12
            else:
                op.dur = 0.06 + _free_size(args[2]) / 2400.0
        else:
            o = kwargs.get("out", args[0] if args else None)
            op.dur = 0.22 + (_free_size(o) / 1000.0 if o is not None else 0.1)
        op.tcls = None
        if e == "act" and name == "activation":
            f = kwargs.get("func")
            if f in (AF.Exp, AF.Ln):
                op.tcls = "E"
            elif f in (AF.Copy, AF.Square, AF.Identity):
                op.tcls = None
            else:
                op.tcls = str(f)
        self.ops.append(op)

    def op(self, e, fn, reads=(), writes=()):
        if self.muted:
            return
        r = _Rec()
        fn(r)
        self._record(e, r.call, list(reads), list(writes))

    def dma(self, q, out, in_, reads=(), writes=(), slow=False):
        if self.muted:
            return
        self._record(q, ("dma_start", (), {"out": out, "in_": in_}), list(reads), list(writes), is_dma=True, slow=slow)

    def _schedule(self):
        import bisect
        ops = self.ops
        n = len(ops)
        if not self.reorder:
            for i, op in enumerate(ops):
                op.start = float(i)
            return
        succ = [[] for _ in range(n)]
        indeg = [0] * n
        for op in ops:
            indeg[op.idx] = len(op.deps)
            for d in op.deps:
                succ[d].append(op.idx)
        ready_t = [0.0] * n
        finish = [0.0] * n
        avail = {k: [] for k in self.engs}
        for op in ops:
            if indeg[op.idx] == 0:
                avail[op.eng].append(op.idx)
        free = {k: 0.0 for k in self.engs}
        dmafree = {k: 0.0 for k in self.engs}
        placed = 0
        act_cls = getattr(self, "_act_cls", None)
        W = self.WINDOW
        maxdma = getattr(self, 'MAXDMA', 10**9)
        while placed < n:
            best = None
            for e, lst in avail.items():
                if not lst:
                    continue
                fe = free[e]
                for i in lst[:W]:
                    st_ = ready_t[i]
                    if st_ < fe:
                        st_ = fe
                    if e == "act":
                        tc = ops[i].tcls
                        if tc is not None and tc != act_cls:
                            st_ += 1.3
                    key = (st_, i)
                    if best is None or key < best[0]:
                        best = (key, e, i)
            (st_, i), e, _ = best
            op = ops[i]
            if e == "act" and op.tcls is not None:
                act_cls = op.tcls
            avail[e].remove(i)
            op.start = st_
            if op.is_dma:
                free[e] = st_ + (1.0 if e == "pool" else 0.1)
                t0 = max(st_, dmafree[e])
                fin = t0 + op.dur
                dmafree[e] = t0 + (op.dur - 2.0)
            else:
                fin = st_ + op.dur
                free[e] = fin
            finish[i] = fin
            placed += 1
            for s_ in succ[i]:
                lat = fin + (0.0 if (ops[s_].eng == e and not op.is_dma) else 0.25)
                if lat > ready_t[s_]:
                    ready_t[s_] = lat
                indeg[s_] -= 1
                if indeg[s_] == 0:
                    bisect.insort(avail[ops[s_].eng], s_)

    def _wait(self, e, tok):
        kind, key, val = tok
        if kind == "c" and key == e and e == "pe":
            return
        sk = (kind, key)
        if self.waited[e].get(sk, 0) >= val:
            return
        sem = self.sem[key] if kind == "c" else self.dsem[key]
        self.engs[e].wait_ge(sem, val)
        self.waited[e][sk] = val

    def flush(self):
        if not self.ops:
            self.seg += 1
            return
        self._schedule()
        ops = self.ops
        order = sorted(ops, key=lambda o: (o.start, o.idx))
        for op in order:
            e = op.eng
            name, args, kwargs = op.call
            if op.is_dma:
                half = len(self.dsem) // 2
                if e == "pool":
                    i = half + self.dnext_sw
                    self.dnext_sw = (self.dnext_sw + 1) % (len(self.dsem) - half)
                else:
                    i = self.dnext
                    self.dnext = (self.dnext + 1) % half
                if self.dcnt[i] > 0:
                    self._wait(e, ("d", i, self.dcnt[i]))
            for d in sorted(op.deps):
                self._wait(e, ops[d].tok)
            if op.is_dma:
                self.dcnt[i] += 16
                kw = dict(kwargs)
                if op.slow:
                    kw["allow_slow_non_contiguous"] = True
                self.engs[e].dma_start(**kw).then_inc(self.dsem[i], 16)
                op.tok = ("d", i, self.dcnt[i])
            else:
                inst = getattr(self.engs[e], name)(*args, **kwargs)
                self.cnt[e] += 1
                inst.then_inc(self.sem[e], 1)
                op.tok = ("c", e, self.cnt[e])
            self.ninst += 1
        self.ops = []
        self.seg += 1

    def barrier(self, engines=("pe", "dve", "act", "pool", "sp")):
        if self.muted:
            return
        self.flush()
        for e in engines:
            for k in self.engs:
                if k != e and self.cnt[k] > 0:
                    self._wait(e, ("c", k, self.cnt[k]))
            for i, c in enumerate(self.dcnt):
                if c > 0:
                    self._wait(e, ("d", i, c))


class Rot:
    def __init__(self, items):
        self.items = items
        self.i = 0

    def next(self):
        it = self.items[self.i]
        self.i = (self.i + 1) % len(self.items)
        return it


SHAPES = {
    "x": (T, D), "norm_mix": (4, D), "norm_ffn": (4, D),
    "da_w_in": (1, D, 3072), "da_q_norm": (1, 64), "da_k_norm": (1, 64),
    "da_lambda_q1": (1, 64), "da_lambda_k1": (1, 64), "da_lambda_q2": (1, 64), "da_lambda_k2": (1, 64),
    "da_subln": (1, 128), "da_w_out": (1, 1024, D),
    "nsa_w_in": (1, D, 1840), "nsa_q_norm": (1, 64), "nsa_k_norm": (1, 3, 64), "nsa_cmp_pos": (1, 2, 32, 64),
    "nsa_cmp_w1": (1, 2, 2048, 256), "nsa_cmp_w2": (1, 2, 256, 64), "nsa_w_out": (1, 1024, D),
    "gdn_w_in": (1, D, 4112), "gdn_conv_w": (1, 4, 3072), "gdn_a_log": (1, 8), "gdn_dt_bias": (1, 8),
    "gdn_out_norm": (1, 128), "gdn_w_out": (1, 1024, D),
    "lru_w_in": (1, D, 2048), "lru_conv_w": (1, 4, 1024), "lru_conv_b": (1, 1024), "lru_w_a": (1, 4, 256, 256),
    "lru_b_a": (1, 1024), "lru_w_x": (1, 4, 256, 256), "lru_b_x": (1, 1024), "lru_lambda": (1, 1024),
    "lru_w_out": (1, 1024, D),
    "moe_w_group": (4, D, 4), "moe_b_group": (4, 4), "moe_w_router": (4, D, 16), "moe_b_router": (4, 16),
    "moe_w_gate": (4, 16, D, DEXP), "moe_w_up": (4, 16, D, DEXP), "moe_w_down": (4, 16, DEXP, D),
    "c_ident": (128, 128), "c_maskneg": (128, 128), "c_relpos": (128, 16),
    "c_e64": (64, 2048), "c_keep": (128, 512), "c_add": (128, 512), "c_dcbase": (128, 127), "c_overlap": (128, 32),
    "c_maskw": (128, 128),
    "c_mup": (128, 128), "c_mfull": (128, 128), "c_mbu": (128, 128), "c_mbls": (128, 128), "c_half": (128, 2),
}


def host_consts():
    p = np.arange(128)
    c = {}
    c["c_ident"] = np.eye(128, dtype=np.float32)
    c["c_maskneg"] = np.where(p[:, None] > p[None, :], -30000.0, 0.0).astype(np.float32)
    c["c_relpos"] = (p[:, None] - 128.0 * np.arange(16)[None, :]).astype(np.float32)
    same = (p[:, None] // 64) == (p[None, :] // 64)
    c["c_mup"] = (same & (p[:, None] <= p[None, :])).astype(np.float32)
    c["c_mfull"] = same.astype(np.float32)
    c["c_mbu"] = np.where(same & (p[:, None] <= p[None, :]), 0.0, -1e4).astype(np.float32)
    c["c_mbls"] = np.where(same & (p[None, :] < p[:, None]), 0.0, -1e4).astype(np.float32)
    c["c_half"] = np.stack([(p < 64), (p >= 64)], axis=1).astype(np.float32)
    key = np.arange(2048)
    e64 = np.zeros((64, 2048), np.float32)
    e64[key // 64, key] = 1.0
    c["c_e64"] = e64
    tpos = (np.arange(16)[None, :, None] * 128 + p[:, None, None])
    blk = np.arange(32)[None, None, :]
    qblk = tpos // 64
    forced = (blk == 0) | (blk == qblk)
    future = blk > qblk
    c["c_keep"] = (~forced & ~future).astype(np.float32).reshape(128, 512)
    c["c_add"] = (1e4 * forced - 1.0 * (future & ~forced)).astype(np.float32).reshape(128, 512)
    n = np.arange(127)
    c["c_dcbase"] = (p[:, None] - 16.0 * n[None, :] - 31.0).astype(np.float32)
    cs = n * 16
    ss0 = np.arange(32) * 64
    ov = ((cs[:, None] < ss0[None, :] + 64) & (cs[:, None] + 32 > ss0[None, :])).astype(np.float32)
    c["c_overlap"] = np.concatenate([ov, np.zeros((1, 32), np.float32)], axis=0)
    c["c_maskw"] = (p[:, None] > p[None, :]).astype(np.float32)
    return c


class Ctx:
    pass


class StopBuild(Exception):
    pass


def ckpt(C, n):
    import os
    if int(os.environ.get("KSTOP", "0")) == n:
        C.S.barrier()
        C.S.muted = True


def build(nlayers=4, only_layer=None):
    nc = bass.Bass("TRN2", target_bir_lowering=False)
    class LazyDram(dict):
        def __missing__(self, name):
            base = name.split("@")[0]
            shape = SHAPES[base] if "@" not in name else SHAPES[base][1:]
            self[name] = nc.dram_tensor(name.replace("@", "_L"), list(shape), F32, kind="ExternalInput").ap()
            return self[name]

    dr = LazyDram()
    x_d = dr["x"]
    out_d = nc.dram_tensor("out", [T, D], F32, kind="ExternalOutput").ap()

    with contextlib.ExitStack() as es:
        S = Sched(nc, es)
        C = Ctx()
        C.nc, C.S, C.dr = nc, S, dr

        uid = [0]

        def sb(name, shape, dt, stack=es):
            uid[0] += 1
            return stack.enter_context(nc.sbuf_tensor("%s_u%d" % (name, uid[0]), list(shape), dt))

        C.sb = sb
        C.X = sb("X", (128, NT, D), F32)
        C.Xb = [Buf("X%d" % i) for i in range(NT)]
        C.xnT = sb("xnT", (128, KC, T), BF16)
        C.xnTb = [Buf("xnT%d" % i) for i in range(NT)]
        C.ident = sb("ident", (128, 128), F32); C.identb = Buf("ident")
        C.identh = sb("identh", (128, 128), BF16); C.identhb = Buf("identh")
        C.maskneg = sb("maskneg", (128, 128), BF16); C.masknegb = Buf("maskneg")
        C.relpos = sb("relpos", (128, 16), F32); C.relposb = Buf("relpos")
        C.ps = [es.enter_context(nc.psum_tensor("ps%d" % i, [128, 512], F32)) for i in range(8)]
        C.psb = [Buf("ps%d" % i, excl=True) for i in range(8)]

        S.dma("sp", C.ident[:], dr["c_ident"], writes=[C.identb])
        S.dma("pool", C.identh[:], dr["c_ident"], writes=[C.identhb])
        S.dma("pool", C.maskneg[:], dr["c_maskneg"], writes=[C.masknegb])
        S.dma("sp", C.relpos[:], dr["c_relpos"], writes=[C.relposb])
        xv = x_d.rearrange("(t p) d -> p t d", p=128)
        for i in range(0, NT, 2):
            S.dma("sp", C.X[:, i:i + 2, :], xv[:, i:i + 2, :], writes=C.Xb[i:i + 2])

        for l in range(nlayers):
          try:
            m = l % 4
            if only_layer is not None and l != only_layer:
                continue
            import os
            parts = os.environ.get("KPARTS", "mix,moe")
            if m == 0 and "mix" in parts:
                da_layer(C, l)
            if m == 3 and "mix" in parts:
                lru_layer(C, l)
            if m == 2 and "mix" in parts:
                S.barrier()
                keep_r = S.reorder
                S.reorder = keep_r and os.environ.get("KREORDER_GDN", "1") == "1"
                keep_w = S.WINDOW
                S.WINDOW = int(os.environ.get("KWINDOW_GDN", str(keep_w)))
                gdn_layer(C, l)
                S.barrier()
                S.reorder = keep_r
                S.WINDOW = keep_w
            if m == 1 and "mix" in parts:
                nsa_layer(C, l)
            if "moe" in parts:
                moe_layer(C, l)
          except StopBuild:
            break

        S.muted = False
        ov = out_d.rearrange("(t p) d -> p t d", p=128)
        outb = Buf("out")
        for i in range(0, NT, 2):
            S.dma("sp", ov[:, i:i + 2, :], C.X[:, i:i + 2, :], reads=C.Xb[i:i + 2], writes=[outb])
        S.barrier()
    nc._in_names = list(dr.keys())
    print('inputs', nc._in_names)
    print("instructions:", S.ninst, "counts", S.cnt)
    return nc


def small_rstd(C, tmp, tmpb, ss, ssb, n_over, shape_p=128):
    S = C.S
    S.op("dve", lambda e: e.tensor_scalar(out=ss, in0=ss, scalar1=1.0 / n_over, scalar2=EPS,
                                          op0=ALU.mult, op1=ALU.add), reads=[ssb], writes=[ssb])
    S.op("act", lambda e: e.activation(out=ss, in_=ss, func=AF.Ln), reads=[ssb], writes=[ssb])
    S.op("act", lambda e: e.activation(out=ss, in_=ss, func=AF.Exp, scale=-0.5), reads=[ssb], writes=[ssb])


def norm_phase(C, gain_row_ap, st, router=None, keep=False):
    S, nc = C.S, C.nc
    outer_st = st
    if not keep:
        st = contextlib.ExitStack()
    C.gain = C.sb("gain", (128, D), F32, st); C.gainb = Buf("gain")
    C.junk = C.sb("junk", (128, D), BF16, st); C.junkb = Buf("junk")
    S.dma("sp", C.gain[:], gain_row_ap.partition_broadcast(128), writes=[C.gainb])
    xn32 = [C.sb("xn32_%d" % i, (128, D), F32, st) for i in range(2)]
    xn32b = [Buf() for _ in range(2)]
    ssq = [C.sb("ssq_%d" % i, (128, 1), F32, st) for i in range(2)]
    ssqb = [Buf() for _ in range(2)]
    if router is not None:
        xT32 = [C.sb("xT32_%d" % i, (128, KC, 128), BF16, st) for i in range(2)]
        xT32b = [Buf() for _ in range(2)]
    for t in range(NT):
        s = t % 2
        Xt = C.X[:, t, :]
        S.op("act", lambda e: e.activation(out=C.junk[:], in_=Xt, func=AF.Square, accum_out=ssq[s][:]),
             reads=[C.Xb[t]], writes=[C.junkb, ssqb[s]])
        small_rstd(C, None, None, ssq[s][:], ssqb[s], float(D))
        S.op("dve", lambda e: e.scalar_tensor_tensor(out=xn32[s][:], in0=Xt, scalar=ssq[s][:, 0:1], in1=C.gain[:],
                                                     op0=ALU.mult, op1=ALU.mult),
             reads=[C.Xb[t], ssqb[s], C.gainb], writes=[xn32b[s]])
        for b in range(2):
            pi = C.psrot.next()
            ps, psb = C.ps[pi], C.psb[pi]
            for k4 in range(4):
                k = b * 4 + k4
                S.op("pe", lambda e: e.transpose(ps[:, k4 * 128:(k4 + 1) * 128], xn32[s][:, k * 128:(k + 1) * 128],
                                                 C.ident[:]),
                     reads=[xn32b[s], C.identb], writes=[psb])
            psv = ps[:].rearrange("p (a b) -> p a b", a=4)
            S.op("act", lambda e: e.activation(out=C.xnT[:, b * 4:(b + 1) * 4, t * 128:(t + 1) * 128], in_=psv,
                                               func=AF.Copy),
                 reads=[psb], writes=[C.xnTb[t]])
            if router is not None:
                S.op("dve", lambda e: e.tensor_tensor(out=xT32[s][:, b * 4:(b + 1) * 4, :], in0=psv,
                                                      in1=C.xnT[:, b * 4:(b + 1) * 4, t * 128:(t + 1) * 128], op=ALU.subtract),
                     reads=[psb, C.xnTb[t]], writes=[xT32b[s]])
        if router is not None:
            router(t, xT32[s], xT32b[s])
    if not keep:
        S.barrier()
        st.close()


def moe_layer(C, l):
    S, nc, dr = C.S, C.nc, C.dr
    C.psrot = Rot(list(range(8)))
    with contextlib.ExitStack() as st:
        sb = lambda n, sh, dt: C.sb("moe%d_%s" % (l, n), sh, dt, st)
        wr = sb("wr", (128, KC, 20), F32); wrb = Buf()
        S.dma("sp", wr[:, :, 0:4], dr["moe_w_group"][l].rearrange("(k p) n -> p k n", p=128), writes=[wrb])
        S.dma("sp", wr[:, :, 4:20], dr["moe_w_router"][l].rearrange("(k p) n -> p k n", p=128), writes=[wrb])
        wrh = sb("wrh", (128, KC, 20), BF16); wrl = sb("wrl", (128, KC, 20), BF16)
        S.op("dve", lambda e: e.tensor_copy(out=wrh[:], in_=wr[:]), reads=[wrb], writes=[wrb])
        S.op("dve", lambda e: e.tensor_tensor(out=wrl[:], in0=wr[:], in1=wrh[:], op=ALU.subtract), reads=[wrb], writes=[wrb])
        rb = sb("rb", (128, 20), F32); rbb = Buf()
        S.dma("sp", rb[:, 0:4], dr["moe_b_group"][l:l + 1, :].partition_broadcast(128), writes=[rbb])
        S.dma("sp", rb[:, 4:20], dr["moe_b_router"][l:l + 1, :].partition_broadcast(128), writes=[rbb])
        comb = sb("comb", (128, NT, 16), F32); combb = [Buf() for _ in range(NT)]
        L = [sb("L%d" % i, (128, 20), F32) for i in range(2)]
        sm = [sb("sm%d" % i, (128, 64), F32) for i in range(2)]
        Lb = [Buf() for _ in range(2)]

        wg = [sb("wg%d" % i, (128, KC, DEXP), BF16) for i in range(2)]
        wu = [sb("wu%d" % i, (128, KC, DEXP), BF16) for i in range(2)]
        wd = [sb("wd%d" % i, (128, 4, D), BF16) for i in range(2)]
        wgb = [Buf() for _ in range(2)]; wub = [Buf() for _ in range(2)]; wdb = [Buf() for _ in range(2)]

        def load_expert(e):
            s = e % 2
            S.dma("pool", wg[s][:], dr["moe_w_gate@%d" % l][e].rearrange("(k p) f -> p k f", p=128), writes=[wgb[s]])
            S.dma("pool", wu[s][:], dr["moe_w_up@%d" % l][e].rearrange("(k p) f -> p k f", p=128), writes=[wub[s]])
            S.dma("pool", wd[s][:], dr["moe_w_down@%d" % l][e].rearrange("(k p) f -> p k f", p=128), writes=[wdb[s]])

        load_expert(0)
        load_expert(1)

        def router(t, xT, xTb):
            s = t % 2
            pi = C.psrot.next()
            ps, psb = C.ps[pi], C.psb[pi]
            for k in range(KC):
                xh = C.xnT[:, k, t * 128:(t + 1) * 128]
                S.op("pe", lambda e: e.matmul(ps[:, 0:20], xh, wrh[:, k, :], start=(k == 0), stop=False),
                     reads=[C.xnTb[t], wrb], writes=[psb])
                S.op("pe", lambda e: e.matmul(ps[:, 0:20], xT[:, k, :], wrh[:, k, :], start=False, stop=False),
                     reads=[xTb, wrb], writes=[psb])
                S.op("pe", lambda e: e.matmul(ps[:, 0:20], xh, wrl[:, k, :], start=False, stop=(k == KC - 1)),
                     reads=[C.xnTb[t], wrb], writes=[psb])
            Lt, m = L[s], sm[s]
            S.op("dve", lambda e: e.tensor_tensor(out=Lt[:], in0=ps[:, 0:20], in1=rb[:], op=ALU.add),
                 reads=[psb, rbb], writes=[Lb[s]])
            B = [Lb[s]]
            S.op("dve", lambda e: e.tensor_reduce(out=m[:, 0:1], in_=Lt[:, 0:4], axis=AX.X, op=ALU.max, negate=True),
                 reads=B, writes=B)
            S.op("act", lambda e: e.activation(out=m[:, 2:6], in_=Lt[:, 0:4], func=AF.Exp, bias=m[:, 0:1], scale=1.0,
                                               accum_out=m[:, 1:2]), reads=B, writes=B)
            S.op("dve", lambda e: e.tensor_scalar(out=m[:, 6:10], in0=Lt[:, 0:4], scalar1=m[:, 0:1], scalar2=0.0,
                                                  op0=ALU.add, op1=ALU.is_ge), reads=B, writes=B)
            S.op("dve", lambda e: e.tensor_scalar(out=m[:, 6:10], in0=m[:, 6:10], scalar1=-1.0, scalar2=1e30,
                                                  op0=ALU.add, op1=ALU.mult), reads=B, writes=B)
            S.op("dve", lambda e: e.tensor_tensor(
                out=m[:, 10:26].rearrange("p (g j) -> p g j", g=4),
                in0=Lt[:, 4:20].rearrange("p (g j) -> p g j", g=4),
                in1=m[:, 6:10].unsqueeze(2).to_broadcast([128, 4, 4]), op=ALU.add), reads=B, writes=B)
            S.op("dve", lambda e: e.max(out=m[:, 26:34], in_=m[:, 10:26]), reads=B, writes=B)
            S.op("dve", lambda e: e.tensor_scalar(out=m[:, 34:35], in0=m[:, 26:27], scalar1=-1.0, scalar2=None,
                                                  op0=ALU.mult), reads=B, writes=B)
            S.op("act", lambda e: e.activation(out=m[:, 40:56], in_=m[:, 10:26], func=AF.Exp, bias=m[:, 34:35],
                                               scale=1.0), reads=B, writes=B)
            S.op("dve", lambda e: e.tensor_scalar(out=m[:, 10:26], in0=m[:, 10:26], scalar1=m[:, 27:28], scalar2=None,
                                                  op0=ALU.is_ge), reads=B, writes=B)
            S.op("dve", lambda e: e.tensor_tensor(out=m[:, 40:56], in0=m[:, 40:56], in1=m[:, 10:26], op=ALU.mult),
                 reads=B, writes=B)
            S.op("dve", lambda e: e.tensor_reduce(out=m[:, 35:36], in_=m[:, 40:56], axis=AX.X, op=ALU.add),
                 reads=B, writes=B)
            S.op("dve", lambda e: e.tensor_tensor(out=m[:, 36:37], in0=m[:, 35:36], in1=m[:, 1:2], op=ALU.mult),
                 reads=B, writes=B)
            S.op("dve", lambda e: e.reciprocal(out=m[:, 36:37], in_=m[:, 36:37]), reads=B, writes=B)
            S.op("dve", lambda e: e.tensor_scalar(out=comb[:, t, :], in0=m[:, 40:56], scalar1=m[:, 36:37], scalar2=None,
                                                  op0=ALU.mult), reads=B, writes=[combb[t]])

        norm_phase(C, dr["norm_ffn"][l:l + 1, :], st, router=router, keep=True)
        ckpt(C, 11)

        sg = [sb("sg%d" % i, (128, 512), BF16) for i in range(3)]
        sgr = Rot([(sg[i], Buf()) for i in range(3)])
        hT = [sb("hT%d" % i, (128, 4, 512), BF16) for i in range(2)]
        hTb = [[Buf() for _ in range(4)] for _ in range(2)]
        hi = 0
        for e_ in range(NEXP):
            s = e_ % 2
            for tb in range(4):
                hs = hi % 2
                hi += 1
                for fc in range(4):
                    pg = C.psrot.next(); pu = C.psrot.next()
                    for k in range(KC):
                        S.op("pe", lambda e: e.matmul(C.ps[pg][:], wg[s][:, k, fc * 128:(fc + 1) * 128],
                                                      C.xnT[:, k, tb * 512:(tb + 1) * 512],
                                                      start=(k == 0), stop=(k == KC - 1)),
                             reads=[wgb[s]] + C.xnTb[tb * 4:tb * 4 + 4], writes=[C.psb[pg]])
                    for k in range(KC):
                        S.op("pe", lambda e: e.matmul(C.ps[pu][:], wu[s][:, k, fc * 128:(fc + 1) * 128],
                                                      C.xnT[:, k, tb * 512:(tb + 1) * 512],
                                                      start=(k == 0), stop=(k == KC - 1)),
                             reads=[wub[s]] + C.xnTb[tb * 4:tb * 4 + 4], writes=[C.psb[pu]])
                    sgt, sgb = sgr.next()
                    S.op("act", lambda e: e.activation(out=sgt[:], in_=C.ps[pg][:], func=AF.Silu),
                         reads=[C.psb[pg]], writes=[sgb])
                    S.op("dve", lambda e: e.tensor_tensor(out=hT[hs][:, fc, :], in0=C.ps[pu][:], in1=sgt[:], op=ALU.mult),
                         reads=[C.psb[pu], sgb], writes=[hTb[hs][fc]])
                for tt in range(4):
                    t = tb * 4 + tt
                    for dh in range(2):
                        py = C.psrot.next()
                        for fc in range(4):
                            S.op("pe", lambda e: e.matmul(C.ps[py][:], hT[hs][:, fc, tt * 128:(tt + 1) * 128],
                                                          wd[s][:, fc, dh * 512:(dh + 1) * 512],
                                                          start=(fc == 0), stop=(fc == 3)),
                                 reads=[hTb[hs][fc], wdb[s]], writes=[C.psb[py]])
                        Xs = C.X[:, t, dh * 512:(dh + 1) * 512]
                        S.op("dve", lambda e: e.scalar_tensor_tensor(out=Xs, in0=C.ps[py][:], scalar=comb[:, t, e_:e_ + 1],
                                                                     in1=Xs, op0=ALU.mult, op1=ALU.add),
                             reads=[C.psb[py], combb[t], C.Xb[t]], writes=[C.Xb[t]])
            if e_ + 2 < NEXP:
                load_expert(e_ + 2)
            if e_ == 0:
                ckpt(C, 12)
        S.barrier()


def da_layer(C, l):
    S, nc, dr = C.S, C.nc, C.dr
    j = l // 4
    lam_init = 0.8 - 0.6 * math.exp(-0.3 * l)
    H = 8
    C.psrot = Rot([0, 1])
    with contextlib.ExitStack() as st:
        sb = lambda n, sh, dt: C.sb("da%d_%s" % (l, n), sh, dt, st)
        norm_phase(C, dr["norm_mix"][l:l + 1, :], st, keep=True)
        ckpt(C, 1)
        gv = sb("gv", (128, 256), F32); gvb = Buf()
        for c in range(2):
            S.dma("sp", gv[:, c * 64:(c + 1) * 64], dr["da_q_norm"][j:j + 1, :].partition_broadcast(128), writes=[gvb])
            S.dma("sp", gv[:, 128 + c * 64:128 + (c + 1) * 64], dr["da_k_norm"][j:j + 1, :].partition_broadcast(128),
                  writes=[gvb])
        S.op("dve", lambda e: e.tensor_scalar(out=gv[:, 0:128], in0=gv[:, 0:128], scalar1=0.125, scalar2=None, op0=ALU.mult),
             reads=[gvb], writes=[gvb])
        lm = sb("lm", (128, 4, 64), F32); lmb = Buf()
        for i, n in enumerate(("da_lambda_q1", "da_lambda_k1", "da_lambda_q2", "da_lambda_k2")):
            S.dma("sp", lm[:, i, :], dr[n][j:j + 1, :].partition_broadcast(128), writes=[lmb])
        lv = sb("lv", (128, 8), F32); lvb = Buf()
        S.op("dve", lambda e: e.tensor_tensor(out=lm[:, 0, :], in0=lm[:, 0, :], in1=lm[:, 1, :], op=ALU.mult),
             reads=[lmb], writes=[lmb])
        S.op("dve", lambda e: e.tensor_tensor(out=lm[:, 2, :], in0=lm[:, 2, :], in1=lm[:, 3, :], op=ALU.mult),
             reads=[lmb], writes=[lmb])
        S.op("dve", lambda e: e.tensor_reduce(out=lv[:, 0:1], in_=lm[:, 0, :], axis=AX.X, op=ALU.add), reads=[lmb], writes=[lvb])
        S.op("dve", lambda e: e.tensor_reduce(out=lv[:, 1:2], in_=lm[:, 2, :], axis=AX.X, op=ALU.add), reads=[lmb], writes=[lvb])
        S.op("act", lambda e: e.activation(out=lv[:, 2:4], in_=lv[:, 0:2], func=AF.Exp), reads=[lvb], writes=[lvb])
        S.op("dve", lambda e: e.tensor_tensor(out=lv[:, 4:5], in0=lv[:, 3:4], in1=lv[:, 2:3], op=ALU.subtract),
             reads=[lvb], writes=[lvb])
        S.op("dve", lambda e: e.tensor_scalar(out=lv[:, 4:5], in0=lv[:, 4:5], scalar1=-lam_init, scalar2=None, op0=ALU.add),
             reads=[lvb], writes=[lvb])
        sl = sb("sl", (128, 128), F32); slb = Buf()
        S.dma("sp", sl[:], dr["da_subln"][j:j + 1, :].partition_broadcast(128), writes=[slb])
        S.op("dve", lambda e: e.tensor_scalar(out=sl[:], in0=sl[:], scalar1=1.0 - lam_init, scalar2=None, op0=ALU.mult),
             reads=[slb], writes=[slb])
        eb = sb("eb", (128, H, 16), F32); ebb = Buf()
        for h in range(H):
            slope = 2.0 ** (-(h + 1))
            S.op("dve", lambda e: e.tensor_scalar(out=eb[:, h, :], in0=C.relpos[:], scalar1=slope, scalar2=None, op0=ALU.mult),
                 reads=[C.relposb], writes=[ebb])
        wo = sb("wo", (128, H, D), BF16); wob = Buf()
        S.dma("pool", wo[:], dr["da_w_out"][j].rearrange("(h e) d -> e h d", e=128), writes=[wob])
        ONT = sb("ONT", (128, H, T), BF16); ONTb = [[Buf() for _ in range(NT)] for _ in range(H)]
        wh = [sb("wh%d" % i, (128, KC, 384), BF16) for i in range(2)]; whb = [Buf() for _ in range(2)]
        QK = sb("QK", (64, 4, T), BF16)
        qTb = [Buf() for _ in range(NT)]; kTb = qTb
        mask01 = sb("mask01", (128, 128), BF16); mask01b = Buf()
        S.op("dve", lambda e: e.tensor_scalar(out=mask01[:], in0=C.maskneg[:], scalar1=-1.0, scalar2=None, op0=ALU.is_ge),
             reads=[C.masknegb], writes=[mask01b])
        vx = sb("vx", (128, NT, 130), BF16); vxb = [Buf() for _ in range(NT)]
        onesb = Buf()
        S.op("dve", lambda e: e.memset(vx[:, :, 128:130], 1.0), writes=vxb)
        qk = [sb("qk%d" % i, (128, 256), F32) for i in range(2)]; qkb = [Buf() for _ in range(2)]
        sq = [sb("sq%d" % i, (128, 256), F32) for i in range(2)]; sqb = [Buf() for _ in range(2)]
        qkn = [sb("qkn%d" % i, (128, 256), BF16) for i in range(2)]; qknb = [Buf() for _ in range(2)]
        ss4 = [sb("ss4%d" % i, (128, 4), F32) for i in range(2)]; ss4b = [Buf() for _ in range(2)]
        PT = [sb("PT%d" % i, (128, 128), BF16) for i in range(4)]
        PTr = Rot([(PT[i], Buf()) for i in range(4)])
        oc = [sb("oc%d" % i, (128, 2, 128), F32) for i in range(2)]; ocb = [Buf() for _ in range(2)]
        rs = [sb("rs%d" % i, (128, 4), F32) for i in range(2)]; rsb = [Buf() for _ in range(2)]
        od = [sb("od%d" % i, (128, 128), F32) for i in range(2)]; odb = [Buf() for _ in range(2)]
        on = [sb("on%d" % i, (128, 128), BF16) for i in range(2)]; onb = [Buf() for _ in range(2)]
        C.junk = sb("dajunk", (128, 128), BF16); C.junkb = Buf()

        def load_head(h):
            s = h % 2
            for part in range(3):
                S.dma("pool", wh[s][:, :, part * 128:(part + 1) * 128],
                      dr["da_w_in"][j][:, part * 1024 + h * 128: part * 1024 + (h + 1) * 128].rearrange("(k p) n -> p k n", p=128),
                      writes=[whb[s]])

        load_head(0)
        ckpt(C, 2)
        srot = Rot([4, 5, 6, 7])
        orot = Rot([2, 3])
        for h in range(H):
            s = h % 2
            if h + 1 < H:
                load_head(h + 1)
            for t in range(NT):
                u = t % 2
                pj = srot.next()
                ps, psb = C.ps[pj], C.psb[pj]
                for k in range(KC):
                    S.op("pe", lambda e: e.matmul(ps[:, 0:384], C.xnT[:, k, t * 128:(t + 1) * 128], wh[s][:, k, :],
                                                  start=(k == 0), stop=(k == KC - 1)),
                         reads=[C.xnTb[t], whb[s]], writes=[psb])
                S.op("act", lambda e: e.activation(out=qk[u][:], in_=ps[:, 0:256], func=AF.Copy), reads=[psb], writes=[qkb[u]])
                S.op("act", lambda e: e.activation(out=vx[:, t, 0:128], in_=ps[:, 256:384], func=AF.Copy),
                     reads=[psb], writes=[vxb[t]])
                S.op("pool", lambda e: e.tensor_tensor(out=sq[u][:], in0=qk[u][:], in1=qk[u][:], op=ALU.mult),
                     reads=[qkb[u]], writes=[sqb[u]])
                S.op("dve", lambda e: e.tensor_reduce(out=ss4[u][:], in_=sq[u][:].rearrange("p (g d) -> p g d", g=4),
                                                      axis=AX.X, op=ALU.add), reads=[sqb[u]], writes=[ss4b[u]])
                small_rstd(C, None, None, ss4[u][:], ss4b[u], 64.0)
                S.op("dve", lambda e: e.tensor_tensor(out=sq[u][:].rearrange("p (g d) -> p g d", g=4),
                                                      in0=qk[u][:].rearrange("p (g d) -> p g d", g=4),
                                                      in1=ss4[u][:].unsqueeze(2).to_broadcast([128, 4, 64]), op=ALU.mult),
                     reads=[qkb[u], ss4b[u]], writes=[sqb[u]])
                S.op("pool", lambda e: e.tensor_tensor(out=qkn[u][:], in0=sq[u][:], in1=gv[:], op=ALU.mult),
                     reads=[sqb[u], gvb], writes=[qknb[u]])
                pt, ptb = C.ps[1], C.psb[1]
                ptv = pt[:].bitcast(BF16)
                for jj in range(4):
                    S.op("pe", lambda e: e.transpose(ptv[0:64, jj * 128:(jj + 1) * 128], qkn[u][:, jj * 64:(jj + 1) * 64], C.identh[:]),
                         reads=[qknb[u], C.identhb], writes=[ptb])
                S.op("act", lambda e: e.activation(out=QK[:, :, t * 128:(t + 1) * 128],
                                                   in_=ptv[0:64, 0:512].rearrange("p (a b) -> p a b", a=4), func=AF.Copy),
                     reads=[ptb], writes=[qTb[t]])
            ckpt(C, 3)
            for jq in range(NT):
                if jq == 1:
                    ckpt(C, 4)
                u = jq % 2
                for c in range(2):
                    po = orot.next()
                    for i in range(jq + 1):
                        pS = srot.next()
                        diag = (i == jq)
                        S.op("pe", lambda e: e.matmul(C.ps[pS][:, 0:128], QK[:, 2 + c, i * 128:(i + 1) * 128],
                                                      QK[:, c, jq * 128:(jq + 1) * 128],
                                                      start=True, stop=True),
                             reads=[kTb[i], qTb[jq]], writes=[C.psb[pS]])
                        P, Pb = PTr.next()
                        S.op("act", lambda e: e.activation(out=P[:], in_=C.ps[pS][:, 0:128], func=AF.Exp,
                                                           bias=eb[:, h, jq - i:jq - i + 1], scale=1.0),
                             reads=[C.psb[pS], ebb], writes=[Pb])
                        if diag:
                            S.op("pool", lambda e: e.tensor_tensor(out=P[:], in0=P[:], in1=mask01[:], op=ALU.mult),
                                 reads=[Pb, mask01b], writes=[Pb])
                        S.op("pe", lambda e: e.matmul(C.ps[po][:, 0:129], P[:], vx[:, i, 0:129], start=(i == 0), stop=diag),
                             reads=[Pb, vxb[i]], writes=[C.psb[po]])
                    S.op("dve", lambda e: e.reciprocal(out=rs[u][:, c:c + 1], in_=C.ps[po][:, 128:129]),
                         reads=[C.psb[po]], writes=[rsb[u]])
                    S.op("dve", lambda e: e.tensor_scalar(out=oc[u][:, c, :], in0=C.ps[po][:, 0:128], scalar1=rs[u][:, c:c + 1],
                                                          scalar2=None, op0=ALU.mult),
                         reads=[C.psb[po], rsb[u]], writes=[ocb[u]])
                S.op("dve", lambda e: e.scalar_tensor_tensor(out=od[u][:], in0=oc[u][:, 1, :], scalar=lv[:, 4:5], in1=oc[u][:, 0, :],
                                                             op0=ALU.mult, op1=ALU.add),
                     reads=[ocb[u], lvb], writes=[odb[u]])
                S.op("act", lambda e: e.activation(out=C.junk[:, 0:128], in_=od[u][:], func=AF.Square, accum_out=rs[u][:, 2:3]),
                     reads=[odb[u]], writes=[C.junkb, rsb[u]])
                small_rstd(C, None, None, rs[u][:, 2:3], rsb[u], 128.0)
                S.op("dve", lambda e: e.scalar_tensor_tensor(out=on[u][:], in0=od[u][:], scalar=rs[u][:, 2:3], in1=sl[:],
                                                             op0=ALU.mult, op1=ALU.mult),
                     reads=[odb[u], rsb[u], slb], writes=[onb[u]])
                pt, ptb = C.ps[1], C.psb[1]
                ptv = pt[:].bitcast(BF16)
                S.op("pe", lambda e: e.transpose(ptv[:, 256:384], on[u][:], C.identh[:]), reads=[onb[u], C.identhb], writes=[ptb])
                S.op("act", lambda e: e.activation(out=ONT[:, h, jq * 128:(jq + 1) * 128], in_=ptv[:, 256:384], func=AF.Copy),
                     reads=[ptb], writes=[ONTb[h][jq]])
            ckpt(C, 5)
        yrot = Rot([4, 5, 6, 7])
        for t in range(NT):
            for dh in range(2):
                py = yrot.next()
                for h in range(H):
                    S.op("pe", lambda e: e.matmul(C.ps[py][:], ONT[:, h, t * 128:(t + 1) * 128], wo[:, h, dh * 512:(dh + 1) * 512],
                                                  start=(h == 0), stop=(h == H - 1)),
                         reads=[ONTb[h][t], wob], writes=[C.psb[py]])
                Xs = C.X[:, t, dh * 512:(dh + 1) * 512]
                S.op("dve", lambda e: e.tensor_tensor(out=Xs, in0=C.ps[py][:], in1=Xs, op=ALU.add),
                     reads=[C.psb[py], C.Xb[t]], writes=[C.Xb[t]])
        S.barrier()


def lru_layer(C, l):
    S, nc, dr = C.S, C.nc, C.dr
    j = l // 4
    C.psrot = Rot([0, 1])
    with contextlib.ExitStack() as st:
        sb = lambda n, sh, dt: C.sb("lru%d_%s" % (l, n), sh, dt, st)
        norm_phase(C, dr["norm_mix"][l:l + 1, :], st, keep=True)
        pv = sb("pv", (128, 10, 8), F32); pvb = Buf()
        for jj in range(4):
            S.dma("sp", pv[:, jj, :], dr["lru_conv_w"][j, jj].rearrange("(c p) -> p c", p=128), writes=[pvb], slow=True)
        for idx, n in ((4, "lru_conv_b"), (5, "lru_b_a"), (6, "lru_b_x"), (7, "lru_lambda")):
            S.dma("sp", pv[:, idx, :], dr[n][j].rearrange("(c p) -> p c", p=128), writes=[pvb], slow=True)
        S.op("act", lambda e: e.activation(out=pv[:, 8, :], in_=pv[:, 7, :], func=AF.Exp, scale=-1.0), reads=[pvb], writes=[pvb])
        S.op("act", lambda e: e.activation(out=pv[:, 8, :], in_=pv[:, 8, :], func=AF.Ln, bias=1.0, scale=1.0), reads=[pvb], writes=[pvb])
        S.op("dve", lambda e: e.tensor_scalar(out=pv[:, 7, :], in0=pv[:, 8, :], scalar1=-8.0, scalar2=None, op0=ALU.mult),
             reads=[pvb], writes=[pvb])
        wa = sb("wa", (128, 4, 2, 256), BF16); wx = sb("wx", (128, 4, 2, 256), BF16); wab = Buf()
        S.dma("pool", wa[:], dr["lru_w_a"][j].rearrange("n (h p) d -> p n h d", p=128), writes=[wab])
        S.dma("pool", wx[:], dr["lru_w_x"][j].rearrange("n (h p) d -> p n h d", p=128), writes=[wab])
        win = sb("win", (128, KC, 512), BF16); winb = Buf()
        wout = [sb("wout%d" % i, (128, D), BF16) for i in range(2)]; woutb = [Buf() for _ in range(2)]
        rec = sb("rec", (128, 2, T), F32); recb = [Buf() for _ in range(2)]
        xr = sb("xr", (128, 2, T), F32); xrb = [Buf() for _ in range(2)]
        xrh = sb("xrh", (128, 2, T), BF16); xrhb = [Buf() for _ in range(2)]
        hg = [sb("hg%d" % i, (128, T), BF16) for i in range(2)]; hgb = [Buf() for _ in range(2)]
        hcar = sb("hcar", (128, 1), F32); hcarb = Buf()
        tmp = [sb("tmp%d" % i, (128, 512), F32) for i in range(10)]
        tmpr = Rot([(tmp[i], Buf()) for i in range(10)])
        prot = Rot([2, 3, 4, 5, 6, 7])
        TB = 4
        for n in range(4):
            S.dma("pool", win[:, :, 0:256], dr["lru_w_in"][j][:, n * 256:(n + 1) * 256].rearrange("(k p) c -> p k c", p=128),
                  writes=[winb])
            S.dma("pool", win[:, :, 256:512], dr["lru_w_in"][j][:, 1024 + n * 256:1024 + (n + 1) * 256].rearrange("(k p) c -> p k c", p=128),
                  writes=[winb])
            for h2 in range(2):
                cc = 2 * n + h2
                for tb in range(TB):
                    pr = prot.next()
                    for k in range(KC):
                        S.op("pe", lambda e: e.matmul(C.ps[pr][:], win[:, k, 256 + h2 * 128:256 + (h2 + 1) * 128],
                                                      C.xnT[:, k, tb * 512:(tb + 1) * 512], start=(k == 0), stop=(k == KC - 1)),
                             reads=[winb] + C.xnTb[tb * 4:tb * 4 + 4], writes=[C.psb[pr]])
                    S.op("act", lambda e: e.activation(out=rec[:, h2, tb * 512:(tb + 1) * 512], in_=C.ps[pr][:], func=AF.Copy),
                         reads=[C.psb[pr]], writes=[recb[h2]])
                S.op("dve", lambda e: e.tensor_scalar(out=xr[:, h2, :], in0=rec[:, h2, :], scalar1=pv[:, 3, cc:cc + 1],
                                                      scalar2=pv[:, 4, cc:cc + 1], op0=ALU.mult, op1=ALU.add),
                     reads=[recb[h2], pvb], writes=[xrb[h2]])
                for sh in (1, 2, 3):
                    S.op("dve", lambda e: e.scalar_tensor_tensor(out=xr[:, h2, sh:T], in0=rec[:, h2, 0:T - sh],
                                                                 scalar=pv[:, 3 - sh, cc:cc + 1], in1=xr[:, h2, sh:T],
                                                                 op0=ALU.mult, op1=ALU.add),
                         reads=[recb[h2], pvb, xrb[h2]], writes=[xrb[h2]])
                S.op("pool", lambda e: e.tensor_copy(out=xrh[:, h2, :], in_=xr[:, h2, :]), reads=[xrb[h2]], writes=[xrhb[h2]])
            for h2 in range(2):
                cc = 2 * n + h2
                ws = cc % 2
                S.dma("pool", wout[ws][:], dr["lru_w_out"][j][cc * 128:(cc + 1) * 128, :], writes=[woutb[ws]])
                for tb in range(TB):
                    sl_ = slice(tb * 512, (tb + 1) * 512)
                    pa = prot.next(); px = prot.next(); pg = prot.next()
                    for ic in range(2):
                        S.op("pe", lambda e: e.matmul(C.ps[pa][:], wa[:, n, ic, h2 * 128:(h2 + 1) * 128], xrh[:, ic, sl_],
                                                      start=(ic == 0), stop=(ic == 1)),
                             reads=[wab, xrhb[ic]], writes=[C.psb[pa]])
                    for ic in range(2):
                        S.op("pe", lambda e: e.matmul(C.ps[px][:], wx[:, n, ic, h2 * 128:(h2 + 1) * 128], xrh[:, ic, sl_],
                                                      start=(ic == 0), stop=(ic == 1)),
                             reads=[wab, xrhb[ic]], writes=[C.psb[px]])
                    for k in range(KC):
                        S.op("pe", lambda e: e.matmul(C.ps[pg][:], win[:, k, h2 * 128:(h2 + 1) * 128],
                                                      C.xnT[:, k, sl_], start=(k == 0), stop=(k == KC - 1)),
                             reads=[winb] + C.xnTb[tb * 4:tb * 4 + 4], writes=[C.psb[pg]])
                    r_, rb_ = tmpr.next(); i_, ib_ = tmpr.next(); a_, ab_ = tmpr.next(); t_, tb_ = tmpr.next()
                    g_, gb_ = tmpr.next(); u_, ub_ = tmpr.next()
                    S.op("act", lambda e: e.activation(out=r_[:], in_=C.ps[pa][:], func=AF.Sigmoid, bias=pv[:, 5, cc:cc + 1], scale=1.0),
                         reads=[C.psb[pa], pvb], writes=[rb_])
                    S.op("act", lambda e: e.activation(out=i_[:], in_=C.ps[px][:], func=AF.Sigmoid, bias=pv[:, 6, cc:cc + 1], scale=1.0),
                         reads=[C.psb[px], pvb], writes=[ib_])
                    S.op("act", lambda e: e.activation(out=a_[:], in_=r_[:], func=AF.Exp, scale=pv[:, 7, cc:cc + 1]),
                         reads=[rb_, pvb], writes=[ab_])
                    S.op("pool", lambda e: e.tensor_tensor(out=t_[:], in0=a_[:], in1=a_[:], op=ALU.mult), reads=[ab_], writes=[tb_])
                    S.op("dve", lambda e: e.tensor_scalar(out=t_[:], in0=t_[:], scalar1=-1.0, scalar2=1.0, op0=ALU.mult, op1=ALU.add),
                         reads=[tb_], writes=[tb_])
                    S.op("act", lambda e: e.activation(out=t_[:], in_=t_[:], func=AF.Sqrt), reads=[tb_], writes=[tb_])
                    S.op("pool", lambda e: e.tensor_tensor(out=i_[:], in0=i_[:], in1=xr[:, h2, sl_], op=ALU.mult),
                         reads=[ib_, xrb[h2]], writes=[ib_])
                    S.op("pool", lambda e: e.tensor_tensor(out=t_[:], in0=t_[:], in1=i_[:], op=ALU.mult), reads=[tb_, ib_], writes=[tb_])
                    if tb == 0:
                        S.op("dve", lambda e: e.tensor_tensor_scan(out=r_[:], data0=a_[:], data1=t_[:], initial=0.0,
                                                                   op0=ALU.mult, op1=ALU.add),
                             reads=[ab_, tb_], writes=[rb_])
                    else:
                        S.op("dve", lambda e: e.tensor_tensor_scan(out=r_[:], data0=a_[:], data1=t_[:], initial=hcar[:, 0:1],
                                                                   op0=ALU.mult, op1=ALU.add),
                             reads=[ab_, tb_, hcarb], writes=[rb_])
                    S.op("dve", lambda e: e.tensor_copy(out=hcar[:, 0:1], in_=r_[:, 511:512]), reads=[rb_], writes=[hcarb])
                    S.op("act", lambda e: e.activation(out=g_[:], in_=C.ps[pg][:], func=AF.Copy), reads=[C.psb[pg]], writes=[gb_])
                    S.op("pool", lambda e: e.tensor_tensor(out=u_[:], in0=g_[:], in1=g_[:], op=ALU.mult), reads=[gb_], writes=[ub_])
                    S.op("dve", lambda e: e.tensor_scalar(out=u_[:], in0=u_[:], scalar1=0.044715, scalar2=1.0, op0=ALU.mult, op1=ALU.add),
                         reads=[ub_], writes=[ub_])
                    S.op("pool", lambda e: e.tensor_tensor(out=u_[:], in0=u_[:], in1=g_[:], op=ALU.mult), reads=[ub_, gb_], writes=[ub_])
                    S.op("act", lambda e: e.activation(out=u_[:], in_=u_[:], func=AF.Sigmoid, scale=1.5957691216057308),
                         reads=[ub_], writes=[ub_])
                    S.op("pool", lambda e: e.tensor_tensor(out=u_[:], in0=u_[:], in1=g_[:], op=ALU.mult), reads=[ub_, gb_], writes=[ub_])
                    S.op("dve", lambda e: e.tensor_tensor(out=hg[ws][:, sl_], in0=u_[:], in1=r_[:], op=ALU.mult),
                         reads=[ub_, rb_], writes=[hgb[ws]])
                for t in range(NT):
                    for dh in range(2):
                        py = prot.next()
                        S.op("pe", lambda e: e.matmul(C.ps[py][:], hg[ws][:, t * 128:(t + 1) * 128], wout[ws][:, dh * 512:(dh + 1) * 512],
                                                      start=True, stop=True),
                             reads=[hgb[ws], woutb[ws]], writes=[C.psb[py]])
                        Xs = C.X[:, t, dh * 512:(dh + 1) * 512]
                        S.op("dve", lambda e: e.tensor_tensor(out=Xs, in0=C.ps[py][:], in1=Xs, op=ALU.add),
                             reads=[C.psb[py], C.Xb[t]], writes=[C.Xb[t]])
        S.barrier()


def gdn_layer(C, l):
    S, nc, dr = C.S, C.nc, C.dr
    j = l // 4
    H = 8
    C.psrot = Rot([0, 1])
    with contextlib.ExitStack() as st:
        sb = lambda n, sh, dt: C.sb("gdn%d_%s" % (l, n), sh, dt, st)
        norm_phase(C, dr["norm_mix"][l:l + 1, :], st)
        prot = Rot(list(range(8)))
        W = dr["gdn_w_in"][j]
        def cload(name, dt):
            t_ = sb(name, (128, 128), dt); b_ = Buf()
            S.dma("pool", t_[:], dr["c_" + name], writes=[b_])
            return t_, b_
        mup, mupb = cload("mup", BF16)
        mfull, mfullb = cload("mfull", BF16)
        mbu, mbub = cload("mbu", F32)
        mbls, mblsb = cload("mbls", F32)
        half = sb("half", (128, 2), F32); halfb = Buf()
        S.dma("sp", half[:], dr["c_half"], writes=[halfb])
        ones = sb("ones", (128, 128), BF16); onesb = Buf()
        S.op("dve", lambda e: e.memset(ones[:], 1.0), writes=[onesb])
        cw = sb("cw", (128, 4, 24), F32); cwb = Buf()
        for jj in range(4):
            S.dma("sp", cw[:, jj, :], dr["gdn_conv_w"][j, jj].rearrange("(c p) -> p c", p=128), writes=[cwb], slow=True)
        hp = sb("hp", (128, 3, 8), F32); hpb = Buf()
        S.dma("sp", hp[:, 0, :], dr["gdn_a_log"][j:j + 1, :].partition_broadcast(128), writes=[hpb])
        S.dma("sp", hp[:, 1, :], dr["gdn_dt_bias"][j:j + 1, :].partition_broadcast(128), writes=[hpb])
        S.op("act", lambda e: e.activation(out=hp[:, 0, :], in_=hp[:, 0, :], func=AF.Exp), reads=[hpb], writes=[hpb])
        S.op("dve", lambda e: e.tensor_scalar(out=hp[:, 0, :], in0=hp[:, 0, :], scalar1=-1.0, scalar2=None, op0=ALU.mult),
             reads=[hpb], writes=[hpb])
        og_ = sb("ogain", (128, 128), F32); ogb = Buf()
        S.dma("sp", og_[:], dr["gdn_out_norm"][j:j + 1, :].partition_broadcast(128), writes=[ogb])
        wba = sb("wba", (128, KC, 16), BF16); wbab = Buf()
        S.dma("pool", wba[:], W[:, 4096:4112].rearrange("(k p) n -> p k n", p=128), writes=[wbab])
        BETA = sb("BETA", (128, NT, 8), F32); G = sb("G", (128, NT, 8), F32); gb_ = Buf()
        for t in range(NT):
            pi = prot.next()
            for k in range(KC):
                S.op("pe", lambda e: e.matmul(C.ps[pi][:, 0:16], C.xnT[:, k, t * 128:(t + 1) * 128], wba[:, k, :],
                                              start=(k == 0), stop=(k == KC - 1)),
                     reads=[C.xnTb[t], wbab], writes=[C.psb[pi]])
            S.op("act", lambda e: e.activation(out=BETA[:, t, :], in_=C.ps[pi][:, 0:8], func=AF.Sigmoid), reads=[C.psb[pi]], writes=[gb_])
            S.op("dve", lambda e: e.tensor_tensor(out=G[:, t, :], in0=C.ps[pi][:, 8:16], in1=hp[:, 1, :], op=ALU.add),
                 reads=[C.psb[pi], hpb], writes=[gb_])
        S.op("act", lambda e: e.activation(out=G[:], in_=G[:], func=AF.Exp), reads=[gb_], writes=[gb_])
        S.op("act", lambda e: e.activation(out=G[:], in_=G[:], func=AF.Ln, bias=1.0, scale=1.0), reads=[gb_], writes=[gb_])
        S.op("dve", lambda e: e.tensor_tensor(out=G[:], in0=G[:], in1=hp[:, 0, :].unsqueeze(1).to_broadcast([128, NT, 8]), op=ALU.mult),
             reads=[gb_, hpb], writes=[gb_])
        Gh = sb("Gh", (128, NT, 8), BF16); Gl = sb("Gl", (128, NT, 8), BF16)
        S.op("dve", lambda e: e.tensor_copy(out=Gh[:], in_=G[:]), reads=[gb_], writes=[gb_])
        S.op("dve", lambda e: e.tensor_tensor(out=Gl[:], in0=G[:], in1=Gh[:], op=ALU.subtract), reads=[gb_], writes=[gb_])
        GC = sb("GC", (128, NT, 8), F32); GL = sb("GL", (128, NT, 8), F32)
        for t in range(NT):
            pi = prot.next()
            S.op("pe", lambda e: e.matmul(C.ps[pi][:, 0:8], mup[:], Gh[:, t, :], start=True, stop=False), reads=[mupb, gb_], writes=[C.psb[pi]])
            S.op("pe", lambda e: e.matmul(C.ps[pi][:, 0:8], mup[:], Gl[:, t, :], start=False, stop=True), reads=[mupb, gb_], writes=[C.psb[pi]])
            S.op("act", lambda e: e.activation(out=GC[:, t, :], in_=C.ps[pi][:, 0:8], func=AF.Copy), reads=[C.psb[pi]], writes=[gb_])
            pi = prot.next()
            S.op("pe", lambda e: e.matmul(C.ps[pi][:, 0:8], mfull[:], Gh[:, t, :], start=True, stop=False), reads=[mfullb, gb_], writes=[C.psb[pi]])
            S.op("pe", lambda e: e.matmul(C.ps[pi][:, 0:8], mfull[:], Gl[:, t, :], start=False, stop=True), reads=[mfullb, gb_], writes=[C.psb[pi]])
            S.op("act", lambda e: e.activation(out=GL[:, t, :], in_=C.ps[pi][:, 0:8], func=AF.Copy), reads=[C.psb[pi]], writes=[gb_])
        NGC = sb("NGC", (128, NT, 8), F32); BEG = sb("BEG", (128, NT, 8), F32)
        DC0 = sb("DC0", (128, NT, 8), F32)
        S.op("dve", lambda e: e.tensor_scalar(out=NGC[:], in0=GC[:], scalar1=-1.0, scalar2=None, op0=ALU.mult), reads=[gb_], writes=[gb_])
        S.op("act", lambda e: e.activation(out=BEG[:], in_=GC[:], func=AF.Exp), reads=[gb_], writes=[gb_])
        S.op("dve", lambda e: e.tensor_tensor(out=BEG[:], in0=BEG[:], in1=BETA[:], op=ALU.mult), reads=[gb_], writes=[gb_])
        S.op("dve", lambda e: e.tensor_tensor(out=DC0[:], in0=GL[:], in1=GC[:], op=ALU.subtract), reads=[gb_], writes=[gb_])
        S.op("act", lambda e: e.activation(out=DC0[:], in_=DC0[:], func=AF.Exp), reads=[gb_], writes=[gb_])
        wh = sb("wh", (128, KC, 512), BF16); whb = Buf()
        pre = sb("pre", (128, T), F32); preb = Buf()
        post = sb("post", (128, T), F32); postb = Buf()
        sqs = [sb("sqb%d" % i, (128, 512), BF16) for i in range(2)]
        sqr = Rot([(sqs[i], Buf()) for i in range(2)])
        qT = sb("qT", (128, T), BF16); kT = sb("kT", (128, T), BF16); vT = sb("vT", (128, T), BF16)
        qTb = Buf(); kTb = Buf(); vTb = Buf()
        ktok = sb("ktok", (128, NT, 128), BF16); vtok = sb("vtok", (128, NT, 128), BF16); tokb = [Buf() for _ in range(NT)]
        U_ = sb("U", (128, NT, 128), F32); wT = sb("wT", (128, NT, 128), BF16); qgT = sb("qgT", (128, NT, 128), BF16)
        aqT = sb("aqT", (128, NT, 128), BF16); kd0 = sb("kd0", (128, NT, 128), BF16)
        intb = [Buf() for _ in range(NT)]
        egl = sb("egl", (128, NT, 2), F32); eglb = [Buf() for _ in range(NT)]
        otokb = [preb for _ in range(NT)]
        otok = lambda rows, t: pre[rows, t * 128:(t + 1) * 128]
        OGT = sb("OGT", (128, T), BF16); OGTb = [Buf() for _ in range(NT)]
        wo = [sb("wo%d" % i, (128, D), BF16) for i in range(2)]; wob = [Buf() for _ in range(2)]
        Sf = sb("Sf", (128, 128), F32); Sfb = Buf()
        Sb = [sb("Sb%d" % i, (128, 128), BF16) for i in range(2)]; Sbb = [Buf() for _ in range(2)]
        NTMP = 8
        tf = [sb("tf%d" % i, (128, 128), F32) for i in range(NTMP)]
        tfr = Rot([(tf[i], Buf()) for i in range(NTMP)])
        th = [sb("th%d" % i, (128, 128), BF16) for i in range(20)]
        thr = Rot([(th[i], Buf()) for i in range(20)])
        bigf = [sb("bigf%d" % i, (128, 512), F32) for i in range(2)]
        bigr = Rot([(bigf[i], Buf()) for i in range(2)])

        for h in range(H):
            for part in range(4):
                col = part * 1024 + h * 128
                S.dma("pool", wh[:, :, part * 128:(part + 1) * 128], W[:, col:col + 128].rearrange("(k p) n -> p k n", p=128),
                      writes=[whb])
            for part, (dst, dstb) in enumerate(((qT, qTb), (kT, kTb), (vT, vTb))):
                cc = part * 8 + h
                for tb in range(4):
                    pi = prot.next()
                    for k in range(KC):
                        S.op("pe", lambda e: e.matmul(C.ps[pi][:], wh[:, k, part * 128:(part + 1) * 128], C.xnT[:, k, tb * 512:(tb + 1) * 512],
                                                      start=(k == 0), stop=(k == KC - 1)),
                             reads=[whb] + C.xnTb[tb * 4:tb * 4 + 4], writes=[C.psb[pi]])
                    S.op("act", lambda e: e.activation(out=pre[:, tb * 512:(tb + 1) * 512], in_=C.ps[pi][:], func=AF.Copy),
                         reads=[C.psb[pi]], writes=[preb])
                S.op("dve", lambda e: e.tensor_scalar(out=post[:], in0=pre[:], scalar1=cw[:, 3, cc:cc + 1], scalar2=None, op0=ALU.mult),
                     reads=[preb, cwb], writes=[postb])
                for sh in (1, 2, 3):
                    S.op("dve", lambda e: e.scalar_tensor_tensor(out=post[:, sh:T], in0=pre[:, 0:T - sh], scalar=cw[:, 3 - sh, cc:cc + 1],
                                                                 in1=post[:, sh:T], op0=ALU.mult, op1=ALU.add),
                         reads=[preb, cwb, postb], writes=[postb])
                if part == 2:
                    S.op("act", lambda e: e.activation(out=dst[:], in_=post[:], func=AF.Silu), reads=[postb], writes=[dstb])
                else:
                    S.op("act", lambda e: e.activation(out=post[:], in_=post[:], func=AF.Silu), reads=[postb], writes=[postb])
                    for tb in range(4):
                        sl_ = slice(tb * 512, (tb + 1) * 512)
                        pi = prot.next()
                        sq_, sqbb = sqr.next()
                        S.op("pool", lambda e: e.tensor_tensor(out=sq_[:], in0=post[:, sl_], in1=post[:, sl_], op=ALU.mult), reads=[postb], writes=[sqbb])
                        S.op("pe", lambda e: e.matmul(C.ps[pi][:], ones[:], sq_[:], start=True, stop=True),
                             reads=[onesb, sqbb], writes=[C.psb[pi]])
                        r_, rb_ = bigr.next()
                        S.op("dve", lambda e: e.tensor_scalar(out=r_[:], in0=C.ps[pi][:], scalar1=EPS, scalar2=None, op0=ALU.add),
                             reads=[C.psb[pi]], writes=[rb_])
                        S.op("act", lambda e: e.activation(out=r_[:], in_=r_[:], func=AF.Ln), reads=[rb_], writes=[rb_])
                        S.op("act", lambda e: e.activation(out=r_[:], in_=r_[:], func=AF.Exp, scale=-0.5), reads=[rb_], writes=[rb_])
                        qs = (128.0 ** -0.5) if part == 0 else 1.0
                        S.op("dve", lambda e: e.scalar_tensor_tensor(out=dst[:, sl_], in0=post[:, sl_], scalar=qs, in1=r_[:],
                                                                     op0=ALU.mult, op1=ALU.mult),
                             reads=[postb, rb_], writes=[dstb])
            for t in range(NT):
                pi = prot.next()
                pv_ = C.ps[pi][:].bitcast(BF16)
                S.op("pe", lambda e: e.transpose(pv_[:, 0:128], kT[:, t * 128:(t + 1) * 128], C.identh[:]), reads=[kTb, C.identhb], writes=[C.psb[pi]])
                S.op("pe", lambda e: e.transpose(pv_[:, 128:256], vT[:, t * 128:(t + 1) * 128], C.identh[:]), reads=[vTb, C.identhb], writes=[C.psb[pi]])
                S.op("act", lambda e: e.activation(out=ktok[:, t, :], in_=pv_[:, 0:128], func=AF.Copy), reads=[C.psb[pi]], writes=[tokb[t]])
                S.op("dve", lambda e: e.tensor_copy(out=vtok[:, t, :], in_=pv_[:, 128:256]), reads=[C.psb[pi]], writes=[tokb[t]])
            for t in range(NT):
                ts_ = slice(t * 128, (t + 1) * 128)
                hh = slice(h, h + 1)
                gh_, ghb_ = thr.next(); gl_, glb_ = thr.next()
                S.op("pool", lambda e: e.tensor_copy(out=gh_[:], in_=Gh[:, t, hh].to_broadcast([128, 128])), reads=[gb_], writes=[ghb_])
                S.op("pool", lambda e: e.tensor_copy(out=gl_[:], in_=Gl[:, t, hh].to_broadcast([128, 128])), reads=[gb_], writes=[glb_])
                pbc = prot.next()
                S.op("pe", lambda e: e.matmul(C.ps[pbc][:, 0:128], gh_[:], mup[:], start=True, stop=False), reads=[ghb_, mupb], writes=[C.psb[pbc]])
                S.op("pe", lambda e: e.matmul(C.ps[pbc][:, 0:128], gl_[:], mup[:], start=False, stop=True), reads=[glb_, mupb], writes=[C.psb[pbc]])
                pgl = prot.next()
                S.op("pe", lambda e: e.matmul(C.ps[pgl][:, 0:128], gh_[:], mfull[:], start=True, stop=False), reads=[ghb_, mfullb], writes=[C.psb[pgl]])
                S.op("pe", lambda e: e.matmul(C.ps[pgl][:, 0:128], gl_[:], mfull[:], start=False, stop=True), reads=[glb_, mfullb], writes=[C.psb[pgl]])
                S.op("act", lambda e: e.activation(out=egl[:, t, 0:1], in_=C.ps[pgl][:, 0:1], func=AF.Exp), reads=[C.psb[pgl]], writes=[eglb[t]])
                S.op("act", lambda e: e.activation(out=egl[:, t, 1:2], in_=C.ps[pgl][:, 64:65], func=AF.Exp), reads=[C.psb[pgl]], writes=[eglb[t]])
                e2, e2b = tfr.next(); e1, e1b = tfr.next(); eg, egb = tfr.next()
                S.op("dve", lambda e: e.tensor_tensor(out=e2[:], in0=C.ps[pbc][:, 0:128], in1=mbu[:], op=ALU.add), reads=[C.psb[pbc], mbub], writes=[e2b])
                S.op("act", lambda e: e.activation(out=e2[:], in_=e2[:], func=AF.Exp, bias=NGC[:, t, hh], scale=1.0), reads=[e2b, gb_], writes=[e2b])
                S.op("dve", lambda e: e.scalar_tensor_tensor(out=e1[:], in0=C.ps[pbc][:, 0:128], scalar=-1.0, in1=mbls[:], op0=ALU.mult, op1=ALU.add),
                     reads=[C.psb[pbc], mblsb], writes=[e1b])
                S.op("act", lambda e: e.activation(out=e1[:], in_=e1[:], func=AF.Exp, bias=GC[:, t, hh], scale=1.0), reads=[e1b, gb_], writes=[e1b])
                S.op("act", lambda e: e.activation(out=eg[:], in_=C.ps[pbc][:, 0:128], func=AF.Exp), reads=[C.psb[pbc]], writes=[egb])
                S.op("pool", lambda e: e.tensor_tensor(out=qgT[:, t, :], in0=qT[:, ts_], in1=eg[:], op=ALU.mult), reads=[qTb, egb], writes=[intb[t]])
                pk = prot.next()
                S.op("pe", lambda e: e.matmul(C.ps[pk][:, 0:128], kT[:, ts_], kT[:, ts_], start=True, stop=True), reads=[kTb], writes=[C.psb[pk]])
                Lb, Lbb = thr.next(); Ub, Ubb = thr.next(); Pb, Pbb = thr.next(); Qb, Qbb = thr.next()
                S.op("dve", lambda e: e.scalar_tensor_tensor(out=Lb[:], in0=C.ps[pk][:, 0:128], scalar=BETA[:, t, hh], in1=e1[:],
                                                             op0=ALU.mult, op1=ALU.mult), reads=[C.psb[pk], gb_, e1b], writes=[Lbb])
                pt_ = prot.next()
                ptv = C.ps[pt_][:].bitcast(BF16)
                S.op("pe", lambda e: e.transpose(ptv[:, 0:128], Lb[:], C.identh[:]), reads=[Lbb, C.identhb], writes=[C.psb[pt_]])
                S.op("act", lambda e: e.activation(out=Ub[:], in_=ptv[:, 0:128], func=AF.Copy), reads=[C.psb[pt_]], writes=[Ubb])
                S.op("pool", lambda e: e.tensor_tensor(out=Qb[:], in0=C.identh[:], in1=Lb[:], op=ALU.subtract), reads=[C.identhb, Lbb], writes=[Qbb])
                S.op("pool", lambda e: e.tensor_tensor(out=Pb[:], in0=C.identh[:], in1=Ub[:], op=ALU.subtract), reads=[C.identhb, Ubb], writes=[Pbb])
                for step in range(5):
                    L2, L2b = thr.next(); U2, U2b = thr.next()
                    p1 = prot.next(); p2 = prot.next()
                    S.op("pe", lambda e: e.matmul(C.ps[p1][:, 0:128], Ub[:], Lb[:], start=True, stop=True), reads=[Ubb, Lbb], writes=[C.psb[p1]])
                    S.op("pe", lambda e: e.matmul(C.ps[p2][:, 0:128], Lb[:], Ub[:], start=True, stop=True), reads=[Ubb, Lbb], writes=[C.psb[p2]])
                    S.op("act", lambda e: e.activation(out=L2[:], in_=C.ps[p1][:, 0:128], func=AF.Copy), reads=[C.psb[p1]], writes=[L2b])
                    S.op("dve", lambda e: e.tensor_copy(out=U2[:], in_=C.ps[p2][:, 0:128]), reads=[C.psb[p2]], writes=[U2b])
                    Pn, Pnb = thr.next()
                    p3 = prot.next()
                    S.op("pe", lambda e: e.matmul(C.ps[p3][:, 0:128], Qb[:], U2[:], start=True, stop=True), reads=[Qbb, U2b], writes=[C.psb[p3]])
                    S.op("dve", lambda e: e.tensor_tensor(out=Pn[:], in0=C.ps[p3][:, 0:128], in1=Pb[:], op=ALU.add), reads=[C.psb[p3], Pbb], writes=[Pnb])
                    if step < 4:
                        Qn, Qnb = thr.next()
                        p4 = prot.next()
                        S.op("pe", lambda e: e.matmul(C.ps[p4][:, 0:128], Pb[:], L2[:], start=True, stop=True), reads=[Pbb, L2b], writes=[C.psb[p4]])
                        S.op("dve", lambda e: e.tensor_tensor(out=Qn[:], in0=C.ps[p4][:, 0:128], in1=Qb[:], op=ALU.add), reads=[C.psb[p4], Qbb], writes=[Qnb])
                        Qb, Qbb = Qn, Qnb
                    Pb, Pbb = Pn, Pnb
                    Lb, Lbb, Ub, Ubb = L2, L2b, U2, U2b
                pq = prot.next()
                S.op("pe", lambda e: e.matmul(C.ps[pq][:, 0:128], kT[:, ts_], qT[:, ts_], start=True, stop=True), reads=[kTb, qTb], writes=[C.psb[pq]])
                S.op("dve", lambda e: e.tensor_tensor(out=aqT[:, t, :], in0=C.ps[pq][:, 0:128], in1=e2[:], op=ALU.mult), reads=[C.psb[pq], e2b], writes=[intb[t]])
                vb_, vbb_ = thr.next(); kg_, kgb_ = thr.next()
                S.op("act", lambda e: e.activation(out=vb_[:], in_=vtok[:, t, :], func=AF.Copy, scale=BETA[:, t, hh]),
                     reads=[tokb[t], gb_], writes=[vbb_])
                S.op("act", lambda e: e.activation(out=kg_[:], in_=ktok[:, t, :], func=AF.Copy, scale=BEG[:, t, hh]),
                     reads=[tokb[t], gb_], writes=[kgb_])
                S.op("act", lambda e: e.activation(out=kd0[:, t, :], in_=ktok[:, t, :], func=AF.Copy, scale=DC0[:, t, hh]),
                     reads=[tokb[t], gb_], writes=[intb[t]])
                pu = prot.next(); pw = prot.next()
                S.op("pe", lambda e: e.matmul(C.ps[pu][:, 0:128], Pb[:], vb_[:], start=True, stop=True), reads=[Pbb, vbb_], writes=[C.psb[pu]])
                S.op("pe", lambda e: e.matmul(C.ps[pw][:, 0:128], kg_[:], Pb[:], start=True, stop=True), reads=[Pbb, kgb_], writes=[C.psb[pw]])
                S.op("act", lambda e: e.activation(out=U_[:, t, :], in_=C.ps[pu][:, 0:128], func=AF.Copy), reads=[C.psb[pu]], writes=[intb[t]])
                S.op("act", lambda e: e.activation(out=wT[:, t, :], in_=C.ps[pw][:, 0:128], func=AF.Copy), reads=[C.psb[pw]], writes=[intb[t]])
            S.op("dve", lambda e: e.memset(Sf[:], 0.0), writes=[Sfb])
            S.op("dve", lambda e: e.memset(Sb[0][:], 0.0), writes=[Sbb[0]])
            si = 0
            for t in range(NT):
                for hf in range(2):
                    cur, curb = Sb[si % 2], Sbb[si % 2]
                    nxt, nxtb = Sb[(si + 1) % 2], Sbb[(si + 1) % 2]
                    si += 1
                    pws = prot.next(); po = prot.next(); psu = prot.next()
                    S.op("pe", lambda e: e.matmul(C.ps[pws][:, 0:128], wT[:, t, :], cur[:], start=True, stop=True), reads=[intb[t], curb], writes=[C.psb[pws]])
                    vn0, vn0b = tfr.next()
                    vn, vnb = thr.next()
                    S.op("dve", lambda e: e.tensor_tensor(out=vn0[:], in0=U_[:, t, :], in1=C.ps[pws][:, 0:128], op=ALU.subtract),
                         reads=[intb[t], C.psb[pws]], writes=[vn0b])
                    S.op("dve", lambda e: e.tensor_scalar(out=vn[:], in0=vn0[:], scalar1=half[:, hf:hf + 1], scalar2=None, op0=ALU.mult),
                         reads=[vn0b, halfb], writes=[vnb])
                    S.op("pe", lambda e: e.matmul(C.ps[po][:, 0:128], qgT[:, t, :], cur[:], start=True, stop=False), reads=[intb[t], curb], writes=[C.psb[po]])
                    S.op("pe", lambda e: e.matmul(C.ps[po][:, 0:128], aqT[:, t, :], vn[:], start=False, stop=True), reads=[intb[t], vnb], writes=[C.psb[po]])
                    rows = slice(hf * 64, (hf + 1) * 64)
                    S.op("act", lambda e: e.activation(out=otok(rows, t), in_=C.ps[po][rows, 0:128], func=AF.Copy), reads=[C.psb[po]], writes=[otokb[t]])
                    kd = kd0
                    S.op("pe", lambda e: e.matmul(C.ps[psu][:, 0:128], kd[:, t, :], vn[:], start=True, stop=True), reads=[intb[t], vnb], writes=[C.psb[psu]])
                    S.op("dve", lambda e: e.scalar_tensor_tensor(out=Sf[:], in0=Sf[:], scalar=egl[:, t, hf:hf + 1], in1=C.ps[psu][:, 0:128],
                                                                 op0=ALU.mult, op1=ALU.add), reads=[Sfb, eglb[t], C.psb[psu]], writes=[Sfb])
                    S.op("act", lambda e: e.activation(out=nxt[:], in_=Sf[:], func=AF.Copy), reads=[Sfb], writes=[nxtb])
            for t in range(NT):
                pz = prot.next()
                for k in range(KC):
                    S.op("pe", lambda e: e.matmul(C.ps[pz][:, 0:128], C.xnT[:, k, t * 128:(t + 1) * 128], wh[:, k, 384:512],
                                                  start=(k == 0), stop=(k == KC - 1)), reads=[C.xnTb[t], whb], writes=[C.psb[pz]])
                z_, zb_ = tfr.next(); o2, o2b = tfr.next(); ss_, ssb_ = tfr.next()
                S.op("act", lambda e: e.activation(out=z_[:], in_=C.ps[pz][:, 0:128], func=AF.Silu), reads=[C.psb[pz]], writes=[zb_])
                S.op("act", lambda e: e.activation(out=o2[:], in_=otok(slice(0, 128), t), func=AF.Square, accum_out=ss_[:, 0:1]),
                     reads=[otokb[t]], writes=[o2b, ssb_])
                small_rstd(C, None, None, ss_[:, 0:1], ssb_, 128.0)
                S.op("dve", lambda e: e.scalar_tensor_tensor(out=o2[:], in0=otok(slice(0, 128), t), scalar=ss_[:, 0:1], in1=og_[:], op0=ALU.mult, op1=ALU.mult),
                     reads=[otokb[t], ssb_, ogb], writes=[o2b])
                ob, obb = thr.next()
                S.op("pool", lambda e: e.tensor_tensor(out=ob[:], in0=o2[:], in1=z_[:], op=ALU.mult), reads=[o2b, zb_], writes=[obb])
                pt_ = prot.next()
                ptv = C.ps[pt_][:].bitcast(BF16)
                S.op("pe", lambda e: e.transpose(ptv[:, 0:128], ob[:], C.identh[:]), reads=[obb, C.identhb], writes=[C.psb[pt_]])
                S.op("act", lambda e: e.activation(out=OGT[:, t * 128:(t + 1) * 128], in_=ptv[:, 0:128], func=AF.Copy),
                     reads=[C.psb[pt_]], writes=[OGTb[t]])
            ws_ = h % 2
            S.dma("pool", wo[ws_][:], dr["gdn_w_out"][j][h * 128:(h + 1) * 128, :], writes=[wob[ws_]])
            for t in range(NT):
                for dh in range(2):
                    py = prot.next()
                    S.op("pe", lambda e: e.matmul(C.ps[py][:], OGT[:, t * 128:(t + 1) * 128], wo[ws_][:, dh * 512:(dh + 1) * 512],
                                                  start=True, stop=True), reads=[OGTb[t], wob[ws_]], writes=[C.psb[py]])
                    Xs = C.X[:, t, dh * 512:(dh + 1) * 512]
                    S.op("dve", lambda e: e.tensor_tensor(out=Xs, in0=C.ps[py][:], in1=Xs, op=ALU.add), reads=[C.psb[py], C.Xb[t]], writes=[C.Xb[t]])
        S.barrier()


def nsa_layer(C, l):
    S, nc, dr = C.S, C.nc, C.dr
    jl = l // 4
    C.psrot = Rot([0, 1])
    W = dr["nsa_w_in"][jl]
    with contextlib.ExitStack() as st:
        sb = lambda n, sh, dt: C.sb("nsa%d_%s" % (l, n), sh, dt, st)
        norm_phase(C, dr["norm_mix"][l:l + 1, :], st)
        prot = Rot([0, 1, 2])
        orot = Rot([3, 4, 5, 6])
        OT = sb("OT", (128, 4, T), BF16); OTb = [Buf() for _ in range(NT)]
        kcmpT = sb("kcmpT", (64, 2, 128), BF16); vcmp = sb("vcmp", (128, 2, 64), BF16); cmpb = Buf()
        GATE = sb("GATE", (128, NT, 48), F32); gateb = Buf()
        mask01 = sb("mask01", (128, 128), BF16); maskw = sb("maskw", (128, 128), BF16); mkb = Buf()
        S.op("dve", lambda e: e.tensor_scalar(out=mask01[:], in0=C.maskneg[:], scalar1=-1.0, scalar2=None, op0=ALU.is_ge),
             reads=[C.masknegb], writes=[mkb])
        S.dma("pool", maskw[:], dr["c_maskw"], writes=[mkb])
        kg = sb("kg", (128, 3, 64), F32); kgb = Buf()
        for i in range(3):
            S.dma("sp", kg[:, i, :], dr["nsa_k_norm"][jl, i:i + 1, :].partition_broadcast(128), writes=[kgb])
        tf = [sb("tf%d" % i, (128, 512), F32) for i in range(2)]
        tfr = Rot([(tf[i], Buf()) for i in range(2)])
        tfs = [sb("tfs%d" % i, (128, 128), F32) for i in range(4)]
        tfsr = Rot([(tfs[i], Buf()) for i in range(4)])
        th = [sb("th%d" % i, (128, 512), BF16) for i in range(2)]
        thr = Rot([(th[i], Buf()) for i in range(2)])
        ths = [sb("ths%d" % i, (128, 128), BF16) for i in range(3)]
        thsr = Rot([(ths[i], Buf()) for i in range(3)])
        sm = [sb("sm%d" % i, (128, 16), F32) for i in range(4)]
        smr = Rot([(sm[i], Buf()) for i in range(4)])

        with contextlib.ExitStack() as sa:
            sba = lambda n, sh, dt: C.sb("nsaA%d_%s" % (l, n), sh, dt, sa)
            wgl = sba("wgl", (128, KC, 48), BF16); wglb = Buf()
            S.dma("pool", wgl[:], W[:, 1792:1840].rearrange("(k p) n -> p k n", p=128), writes=[wglb])
            wA = sba("wA", (128, KC, 256), BF16); wAb = Buf()
            S.dma("pool", wA[:], W[:, 1024:1280].rearrange("(k p) n -> p k n", p=128), writes=[wAb])
            w1 = [sba("w1_%d" % c, (64, 32, 256), BF16) for c in range(2)]; w1b = Buf()
            w2 = [sba("w2_%d" % c, (128, 2, 64), BF16) for c in range(2)]
            posT = sba("posT", (64, 2, 32), BF16)
            for c in range(2):
                S.dma("pool", w1[c][:], dr["nsa_cmp_w1"][jl, c].rearrange("(l d) f -> d l f", d=64), writes=[w1b])
                S.dma("pool", w2[c][:], dr["nsa_cmp_w2"][jl, c].rearrange("(a p) n -> p a n", p=128), writes=[w1b])
                S.dma("pool", posT[:, c, :], dr["nsa_cmp_pos"][jl, c].rearrange("l d -> d l"), writes=[w1b], slow=True)
            xcT = sba("xcT", (64, 4, T), BF16); xcTb = Buf()
            for t in range(NT):
                pi = prot.next()
                for k in range(KC):
                    S.op("pe", lambda e: e.matmul(C.ps[pi][:, 0:48], C.xnT[:, k, t * 128:(t + 1) * 128], wgl[:, k, :],
                                                  start=(k == 0), stop=(k == KC - 1)), reads=[C.xnTb[t], wglb], writes=[C.psb[pi]])
                S.op("act", lambda e: e.activation(out=GATE[:, t, :], in_=C.ps[pi][:, 0:48], func=AF.Sigmoid), reads=[C.psb[pi]], writes=[gateb])
                pi = prot.next()
                for k in range(KC):
                    S.op("pe", lambda e: e.matmul(C.ps[pi][:, 0:256], C.xnT[:, k, t * 128:(t + 1) * 128], wA[:, k, :],
                                                  start=(k == 0), stop=(k == KC - 1)), reads=[C.xnTb[t], wAb], writes=[C.psb[pi]])
                xb_, xbb_ = thr.next()
                S.op("act", lambda e: e.activation(out=xb_[:, 0:256], in_=C.ps[pi][:, 0:256], func=AF.Copy), reads=[C.psb[pi]], writes=[xbb_])
                pt_ = prot.next()
                ptv = C.ps[pt_][:].bitcast(BF16)
                for a in range(4):
                    S.op("pe", lambda e: e.transpose(ptv[0:64, a * 128:(a + 1) * 128], xb_[:, a * 64:(a + 1) * 64], C.identh[:]),
                         reads=[xbb_, C.identhb], writes=[C.psb[pt_]])
                S.op("act", lambda e: e.activation(out=xcT[:, :, t * 128:(t + 1) * 128],
                                                   in_=ptv[0:64, 0:512].rearrange("p (a b) -> p a b", a=4), func=AF.Copy),
                     reads=[C.psb[pt_]], writes=[xcTb])
            for c in range(2):
                pb_, pbb_ = smr.next()
                for fc in range(2):
                    pi = prot.next()
                    for l_ in range(32):
                        S.op("pe", lambda e: e.matmul(C.ps[pi][:, 0:1], w1[c][:, l_, fc * 128:(fc + 1) * 128], posT[:, c, l_:l_ + 1],
                                                      start=(l_ == 0), stop=(l_ == 31)), reads=[w1b], writes=[C.psb[pi]])
                    S.op("act", lambda e: e.activation(out=pb_[:, fc:fc + 1], in_=C.ps[pi][:, 0:1], func=AF.Copy), reads=[C.psb[pi]], writes=[pbb_])
                for g in range(2):
                    src = c * 2 + g
                    hT, hTb = thr.next()
                    for fc in range(2):
                        pi = prot.next()
                        for l_ in range(32):
                            S.op("pe", lambda e: e.matmul(C.ps[pi][:, 0:127], w1[c][:, l_, fc * 128:(fc + 1) * 128],
                                                          xcT[:, src, l_:l_ + 16 * 126 + 1:16], start=(l_ == 0), stop=(l_ == 31)),
                                 reads=[w1b, xcTb], writes=[C.psb[pi]])
                        S.op("act", lambda e: e.activation(out=hT[:, fc * 128:fc * 128 + 127], in_=C.ps[pi][:, 0:127], func=AF.Silu,
                                                           bias=pb_[:, fc:fc + 1], scale=1.0), reads=[C.psb[pi], pbb_], writes=[hTb])
                    pi = prot.next()
                    for fc in range(2):
                        S.op("pe", lambda e: e.matmul(C.ps[pi][0:127, 0:64], hT[:, fc * 128:fc * 128 + 127], w2[c][:, fc, :],
                                                      start=(fc == 0), stop=(fc == 1)), reads=[hTb, w1b], writes=[C.psb[pi]])
                    if c == 1:
                        S.op("act", lambda e: e.activation(out=vcmp[0:127, g, :], in_=C.ps[pi][0:127, 0:64], func=AF.Copy),
                             reads=[C.psb[pi]], writes=[cmpb])
                    else:
                        kc_, kcb_ = tfsr.next(); ss_, ssb_ = smr.next()
                        S.op("dve", lambda e: e.memset(kc_[:, 0:128], 0.0), writes=[kcb_])
                        S.op("act", lambda e: e.activation(out=kc_[0:127, 0:64], in_=C.ps[pi][0:127, 0:64], func=AF.Copy), reads=[C.psb[pi]], writes=[kcb_])
                        S.op("act", lambda e: e.activation(out=kc_[:, 64:128], in_=kc_[:, 0:64], func=AF.Square, accum_out=ss_[:, 0:1]),
                             reads=[kcb_], writes=[kcb_, ssb_])
                        small_rstd(C, None, None, ss_[:, 0:1], ssb_, 64.0)
                        kb_, kbb_ = thsr.next()
                        S.op("dve", lambda e: e.scalar_tensor_tensor(out=kb_[:, 0:64], in0=kc_[:, 0:64], scalar=ss_[:, 0:1], in1=kg[:, 0, :],
                                                                     op0=ALU.mult, op1=ALU.mult), reads=[kcb_, ssb_, kgb], writes=[kbb_])
                        pt_ = prot.next()
                        ptv = C.ps[pt_][:].bitcast(BF16)
                        S.op("pe", lambda e: e.transpose(ptv[0:64, 0:128], kb_[:, 0:64], C.identh[:]), reads=[kbb_, C.identhb], writes=[C.psb[pt_]])
                        S.op("act", lambda e: e.activation(out=kcmpT[:, g, :], in_=ptv[0:64, 0:128], func=AF.Copy), reads=[C.psb[pt_]], writes=[cmpb])
            S.barrier()
        keep = sb("keep", (128, 512), BF16); addc = sb("addc", (128, 512), BF16); dcbase = sb("dcbase", (128, 127), F32)
        ovl = sb("ovl", (128, 32), BF16); e64 = sb("e64", (64, T), BF16); cB = Buf()
        S.dma("pool", keep[:], dr["c_keep"], writes=[cB]); S.dma("pool", addc[:], dr["c_add"], writes=[cB])
        S.dma("sp", dcbase[:], dr["c_dcbase"], writes=[cB])
        S.dma("pool", ovl[:], dr["c_overlap"], writes=[cB]); S.dma("pool", e64[:], dr["c_e64"], writes=[cB])
        S.op("dve", lambda e: e.memset(vcmp[127:128, :, :], 0.0) if False else e.memset(sm[0][:, 0:1], 0.0), writes=[smr.items[0][1]])
        gvq = sb("gvq", (128, 512), F32); gvk = sb("gvk", (128, 128), F32); gvb = Buf()
        for p_ in range(8):
            S.dma("sp", gvq[:, p_ * 64:(p_ + 1) * 64], dr["nsa_q_norm"][jl:jl + 1, :].partition_broadcast(128), writes=[gvb])
        S.op("dve", lambda e: e.tensor_scalar(out=gvq[:], in0=gvq[:], scalar1=0.125, scalar2=None, op0=ALU.mult), reads=[gvb], writes=[gvb])
        S.op("dve", lambda e: e.tensor_copy(out=gvk[:, 0:64], in_=kg[:, 1, :]), reads=[kgb], writes=[gvb])
        S.op("dve", lambda e: e.tensor_copy(out=gvk[:, 64:128], in_=kg[:, 2, :]), reads=[kgb], writes=[gvb])
        eb = sb("eb", (128, 16, 16), F32); ebb = Buf()
        slopes = [2.0 ** (-8.0 * (i + 1) / 16.0) for i in range(16)]
        for hd in range(16):
            S.op("dve", lambda e: e.tensor_scalar(out=eb[:, hd, :], in0=C.relpos[:], scalar1=-64.0, scalar2=float(np.float32(slopes[hd])),
                                                  op0=ALU.add, op1=ALU.mult),
                 reads=[C.relposb], writes=[ebb])
        wq = sb("wq", (128, KC, 512), BF16); wkv = sb("wkv", (128, KC, 256), BF16); wqb = Buf()
        qTg = sb("qTg", (64, 8, T), BF16); qTb = [Buf() for _ in range(NT)]
        ksT = sb("ksT", (64, T), BF16); kwT = sb("kwT", (64, T), BF16); kTb = [Buf() for _ in range(NT)]
        vsx = sb("vsx", (128, NT, 66), BF16); vwx = sb("vwx", (128, NT, 66), BF16); vxb = [Buf() for _ in range(NT)]
        S.op("dve", lambda e: e.memset(vsx[:, :, 64:66], 1.0), writes=vxb)
        S.op("dve", lambda e: e.memset(vwx[:, :, 64:66], 1.0), writes=vxb)
        selb64 = sb("selb64", (64, 128), BF16); selbb = Buf()
        S.op("dve", lambda e: e.memset(selb64[:], 0.0), writes=[selbb])
        ocm = sb("ocm", (128, 8, 64), F32); ocmb = Buf()
        dm = sb("dm", (128, 127), F32); dmb = Buf()
        PT = [sb("PT%d" % i, (128, 128), BF16) for i in range(5)]
        PTr = Rot([(PT[i], Buf()) for i in range(5)])
        opair = [sb("opair%d" % i, (128, 128), BF16) for i in range(2)]; opb = [Buf() for _ in range(2)]
        oh = [sb("oh%d" % i, (128, 64), F32) for i in range(2)]; ohb = [Buf() for _ in range(2)]
        wos = [sb("wo%d" % i, (128, D), BF16) for i in range(2)]
        wor = Rot([(wos[i], Buf()) for i in range(2)])
        for g in range(2):
            S.dma("pool", wq[:], W[:, g * 512:(g + 1) * 512].rearrange("(k p) n -> p k n", p=128), writes=[wqb])
            for a, col in enumerate((1280, 1536, 1408, 1664)):
                S.dma("pool", wkv[:, :, a * 64:(a + 1) * 64], W[:, col + g * 64:col + (g + 1) * 64].rearrange("(k p) n -> p k n", p=128),
                      writes=[wqb])
            for t in range(NT):
                pi = prot.next()
                for k in range(KC):
                    S.op("pe", lambda e: e.matmul(C.ps[pi][:], C.xnT[:, k, t * 128:(t + 1) * 128], wq[:, k, :],
                                                  start=(k == 0), stop=(k == KC - 1)), reads=[C.xnTb[t], wqb], writes=[C.psb[pi]])
                qf, qfb = tfr.next(); q2, q2b = tfr.next(); ss_, ssb_ = smr.next()
                S.op("act", lambda e: e.activation(out=qf[:], in_=C.ps[pi][:], func=AF.Copy), reads=[C.psb[pi]], writes=[qfb])
                S.op("pool", lambda e: e.tensor_tensor(out=q2[:], in0=qf[:], in1=qf[:], op=ALU.mult), reads=[qfb], writes=[q2b])
                S.op("dve", lambda e: e.tensor_reduce(out=ss_[:, 0:8], in_=q2[:].rearrange("p (g d) -> p g d", g=8), axis=AX.X, op=ALU.add),
                     reads=[q2b], writes=[ssb_])
                small_rstd(C, None, None, ss_[:, 0:8], ssb_, 64.0)
                S.op("dve", lambda e: e.tensor_tensor(out=q2[:].rearrange("p (g d) -> p g d", g=8), in0=qf[:].rearrange("p (g d) -> p g d", g=8),
                                                      in1=ss_[:, 0:8].unsqueeze(2).to_broadcast([128, 8, 64]), op=ALU.mult),
                     reads=[qfb, ssb_], writes=[q2b])
                qb_, qbb_ = thr.next()
                S.op("pool", lambda e: e.tensor_tensor(out=qb_[:], in0=q2[:], in1=gvq[:], op=ALU.mult), reads=[q2b, gvb], writes=[qbb_])
                for half_ in range(2):
                    pt_ = prot.next()
                    ptv = C.ps[pt_][:].bitcast(BF16)
                    for a in range(4):
                        hh_ = half_ * 4 + a
                        S.op("pe", lambda e: e.transpose(ptv[0:64, a * 128:(a + 1) * 128], qb_[:, hh_ * 64:(hh_ + 1) * 64], C.identh[:]),
                             reads=[qbb_, C.identhb], writes=[C.psb[pt_]])
                    S.op("act", lambda e: e.activation(out=qTg[:, half_ * 4:(half_ + 1) * 4, t * 128:(t + 1) * 128],
                                                       in_=ptv[0:64, 0:512].rearrange("p (a b) -> p a b", a=4), func=AF.Copy),
                         reads=[C.psb[pt_]], writes=[qTb[t]])
                pi = prot.next()
                for k in range(KC):
                    S.op("pe", lambda e: e.matmul(C.ps[pi][:, 0:256], C.xnT[:, k, t * 128:(t + 1) * 128], wkv[:, k, :],
                                                  start=(k == 0), stop=(k == KC - 1)), reads=[C.xnTb[t], wqb], writes=[C.psb[pi]])
                kf, kfb = tfsr.next(); k2, k2b = tfsr.next(); ss_, ssb_ = smr.next()
                S.op("act", lambda e: e.activation(out=kf[:, 0:128], in_=C.ps[pi][:, 0:128], func=AF.Copy), reads=[C.psb[pi]], writes=[kfb])
                S.op("act", lambda e: e.activation(out=vsx[:, t, 0:64], in_=C.ps[pi][:, 128:192], func=AF.Copy), reads=[C.psb[pi]], writes=[vxb[t]])
                S.op("act", lambda e: e.activation(out=vwx[:, t, 0:64], in_=C.ps[pi][:, 192:256], func=AF.Copy), reads=[C.psb[pi]], writes=[vxb[t]])
                S.op("pool", lambda e: e.tensor_tensor(out=k2[:, 0:128], in0=kf[:, 0:128], in1=kf[:, 0:128], op=ALU.mult), reads=[kfb], writes=[k2b])
                S.op("dve", lambda e: e.tensor_reduce(out=ss_[:, 0:2], in_=k2[:, 0:128].rearrange("p (g d) -> p g d", g=2), axis=AX.X, op=ALU.add),
                     reads=[k2b], writes=[ssb_])
                small_rstd(C, None, None, ss_[:, 0:2], ssb_, 64.0)
                S.op("dve", lambda e: e.tensor_tensor(out=k2[:, 0:128].rearrange("p (g d) -> p g d", g=2),
                                                      in0=kf[:, 0:128].rearrange("p (g d) -> p g d", g=2),
                                                      in1=ss_[:, 0:2].unsqueeze(2).to_broadcast([128, 2, 64]), op=ALU.mult),
                     reads=[kfb, ssb_], writes=[k2b])
                kb_, kbb_ = thsr.next()
                S.op("pool", lambda e: e.tensor_tensor(out=kb_[:, 0:128], in0=k2[:, 0:128], in1=gvk[:], op=ALU.mult), reads=[k2b, gvb], writes=[kbb_])
                pt_ = prot.next()
                ptv = C.ps[pt_][:].bitcast(BF16)
                for a in range(2):
                    S.op("pe", lambda e: e.transpose(ptv[0:64, a * 128:(a + 1) * 128], kb_[:, a * 64:(a + 1) * 64], C.identh[:]),
                         reads=[kbb_, C.identhb], writes=[C.psb[pt_]])
                S.op("act", lambda e: e.activation(out=ksT[:, t * 128:(t + 1) * 128], in_=ptv[0:64, 0:128], func=AF.Copy), reads=[C.psb[pt_]], writes=[kTb[t]])
                S.op("dve", lambda e: e.tensor_copy(out=kwT[:, t * 128:(t + 1) * 128], in_=ptv[0:64, 128:256]), reads=[C.psb[pt_]], writes=[kTb[t]])
            for jq in range(NT):
                qs_ = slice(jq * 128, (jq + 1) * 128)
                d0, d0b = tfsr.next(); d1, d1b = tfsr.next()
                S.op("dve", lambda e: e.tensor_scalar(out=d0[:, 0:127], in0=dcbase[:], scalar1=float(128 * jq), scalar2=None, op0=ALU.add),
                     reads=[cB], writes=[d0b])
                S.op("dve", lambda e: e.tensor_scalar(out=d1[:, 0:127], in0=d0[:, 0:127], scalar1=0.0, scalar2=None, op0=ALU.is_lt),
                     reads=[d0b], writes=[d1b])
                S.op("dve", lambda e: e.scalar_tensor_tensor(out=dm[:], in0=d1[:, 0:127], scalar=1e9, in1=d0[:, 0:127], op0=ALU.mult, op1=ALU.add),
                     reads=[d0b, d1b], writes=[dmb])
                for p_ in range(8):
                    hd = g * 8 + p_
                    pi = prot.next()
                    S.op("pe", lambda e: e.matmul(C.ps[pi][:, 0:127], qTg[:, p_, qs_], kcmpT[:, g, 0:127], start=True, stop=True),
                         reads=[qTb[jq], cmpb], writes=[C.psb[pi]])
                    sc, scb = tfsr.next(); ss_, ssb_ = smr.next()
                    S.op("dve", lambda e: e.scalar_tensor_tensor(out=sc[:, 0:127], in0=dm[:], scalar=-float(np.float32(slopes[hd])), in1=C.ps[pi][:, 0:127],
                                                                 op0=ALU.mult, op1=ALU.add), reads=[dmb, C.psb[pi]], writes=[scb])
                    S.op("act", lambda e: e.activation(out=sc[:, 0:127], in_=sc[:, 0:127], func=AF.Exp, accum_out=ss_[:, 0:1]),
                         reads=[scb], writes=[scb, ssb_])
                    S.op("dve", lambda e: e.tensor_scalar(out=ss_[:, 0:1], in0=ss_[:, 0:1], scalar1=1e-30, scalar2=None, op0=ALU.max),
                         reads=[ssb_], writes=[ssb_])
                    S.op("dve", lambda e: e.reciprocal(out=ss_[:, 0:1], in_=ss_[:, 0:1]), reads=[ssb_], writes=[ssb_])
                    pb_, pbb_ = thsr.next()
                    S.op("dve", lambda e: e.memset(pb_[:, 127:128], 0.0), writes=[pbb_])
                    S.op("dve", lambda e: e.tensor_scalar(out=pb_[:, 0:127], in0=sc[:, 0:127], scalar1=ss_[:, 0:1], scalar2=None, op0=ALU.mult),
                         reads=[scb, ssb_], writes=[pbb_])
                    pt_ = prot.next()
                    ptv = C.ps[pt_][:].bitcast(BF16)
                    S.op("pe", lambda e: e.transpose(ptv[:, 0:128], pb_[:, 0:128], C.identh[:]), reads=[pbb_, C.identhb], writes=[C.psb[pt_]])
                    pT, pTb = PTr.next()
                    S.op("act", lambda e: e.activation(out=pT[:], in_=ptv[:, 0:128], func=AF.Copy), reads=[C.psb[pt_]], writes=[pTb])
                    po = prot.next()
                    S.op("pe", lambda e: e.matmul(C.ps[po][:, 0:64], pT[0:127, :], vcmp[0:127, g, :], start=True, stop=True),
                         reads=[pTb, cmpb], writes=[C.psb[po]])
                    S.op("pe", lambda e: e.matmul(C.ps[7][:, 0:32], pT[0:127, :], ovl[0:127, :], start=(p_ == 0), stop=(p_ == 7)),
                         reads=[pTb, cB], writes=[C.psb[7]])
                    S.op("dve", lambda e: e.tensor_scalar(out=ocm[:, p_, :], in0=C.ps[po][:, 0:64], scalar1=GATE[:, jq, hd * 3:hd * 3 + 1], scalar2=None,
                                                          op0=ALU.mult), reads=[C.psb[po], gateb], writes=[ocmb])
                scr, scrb = tfsr.next(); v8, v8b = smr.next()
                S.op("dve", lambda e: e.tensor_tensor(out=scr[:, 0:32], in0=C.ps[7][:, 0:32], in1=keep[:, jq * 32:(jq + 1) * 32], op=ALU.mult),
                     reads=[C.psb[7], cB], writes=[scrb])
                S.op("dve", lambda e: e.tensor_tensor(out=scr[:, 0:32], in0=scr[:, 0:32], in1=addc[:, jq * 32:(jq + 1) * 32], op=ALU.add),
                     reads=[scrb, cB], writes=[scrb])
                S.op("dve", lambda e: e.max(out=v8[:, 0:8], in_=scr[:, 0:32]), reads=[scrb], writes=[v8b])
                S.op("dve", lambda e: e.tensor_scalar(out=scr[:, 0:32], in0=scr[:, 0:32], scalar1=v8[:, 7:8], scalar2=None, op0=ALU.is_ge),
                     reads=[scrb, v8b], writes=[scrb])
                sbq, sbqb = thsr.next()
                S.op("dve", lambda e: e.tensor_scalar(out=sbq[:, 0:32], in0=scr[:, 0:32], scalar1=-1.0, scalar2=30000.0, op0=ALU.add, op1=ALU.mult),
                     reads=[scrb], writes=[sbqb])
                pt_ = prot.next()
                ptv = C.ps[pt_][:].bitcast(BF16)
                S.op("pe", lambda e: e.transpose(ptv[0:32, 0:128], sbq[:, 0:32], C.identh[:]), reads=[sbqb, C.identhb], writes=[C.psb[pt_]])
                S.op("act", lambda e: e.activation(out=selb64[0:32, :], in_=ptv[0:32, 0:128], func=AF.Copy), reads=[C.psb[pt_]], writes=[selbb])
                for p_ in range(8):
                    hd = g * 8 + p_
                    u = p_ % 2
                    posel = orot.next(); powin = orot.next()
                    for i in range(jq + 1):
                        ks_ = slice(i * 128, (i + 1) * 128)
                        pS = prot.next()
                        S.op("pe", lambda e: e.matmul(C.ps[pS][:, 0:128], ksT[:, ks_], qTg[:, p_, qs_], start=True, stop=False),
                             reads=[kTb[i], qTb[jq]], writes=[C.psb[pS]])
                        S.op("pe", lambda e: e.matmul(C.ps[pS][:, 0:128], e64[:, ks_], selb64[:], start=False, stop=True),
                             reads=[cB, selbb], writes=[C.psb[pS]])
                        P, Pb = PTr.next()
                        S.op("act", lambda e: e.activation(out=P[:], in_=C.ps[pS][:, 0:128], func=AF.Exp, bias=eb[:, hd, jq - i:jq - i + 1], scale=1.0),
                             reads=[C.psb[pS], ebb], writes=[Pb])
                        if i == jq:
                            S.op("pool", lambda e: e.tensor_tensor(out=P[:], in0=P[:], in1=mask01[:], op=ALU.mult), reads=[Pb, mkb], writes=[Pb])
                        S.op("pe", lambda e: e.matmul(C.ps[posel][:, 0:65], P[:], vsx[:, i, 0:65], start=(i == 0), stop=(i == jq)),
                             reads=[Pb, vxb[i]], writes=[C.psb[posel]])
                    i0 = max(0, jq - 4)
                    for i in range(i0, jq + 1):
                        ks_ = slice(i * 128, (i + 1) * 128)
                        pS = prot.next()
                        S.op("pe", lambda e: e.matmul(C.ps[pS][:, 0:128], kwT[:, ks_], qTg[:, p_, qs_], start=True, stop=True),
                             reads=[kTb[i], qTb[jq]], writes=[C.psb[pS]])
                        P, Pb = PTr.next()
                        S.op("act", lambda e: e.activation(out=P[:], in_=C.ps[pS][:, 0:128], func=AF.Exp, bias=eb[:, hd, jq - i:jq - i + 1], scale=1.0),
                             reads=[C.psb[pS], ebb], writes=[Pb])
                        if i == jq:
                            S.op("pool", lambda e: e.tensor_tensor(out=P[:], in0=P[:], in1=mask01[:], op=ALU.mult), reads=[Pb, mkb], writes=[Pb])
                        elif i == jq - 4:
                            S.op("pool", lambda e: e.tensor_tensor(out=P[:], in0=P[:], in1=maskw[:], op=ALU.mult), reads=[Pb, mkb], writes=[Pb])
                        S.op("pe", lambda e: e.matmul(C.ps[powin][:, 0:65], P[:], vwx[:, i, 0:65], start=(i == i0), stop=(i == jq)),
                             reads=[Pb, vxb[i]], writes=[C.psb[powin]])
                    fs, fsb = smr.next()
                    S.op("dve", lambda e: e.reciprocal(out=fs[:, 0:1], in_=C.ps[posel][:, 64:65]), reads=[C.psb[posel]], writes=[fsb])
                    S.op("dve", lambda e: e.reciprocal(out=fs[:, 1:2], in_=C.ps[powin][:, 64:65]), reads=[C.psb[powin]], writes=[fsb])
                    S.op("dve", lambda e: e.tensor_tensor(out=fs[:, 0:2], in0=fs[:, 0:2], in1=GATE[:, jq, hd * 3 + 1:hd * 3 + 3], op=ALU.mult),
                         reads=[fsb, gateb], writes=[fsb])
                    S.op("dve", lambda e: e.scalar_tensor_tensor(out=oh[u][:], in0=C.ps[posel][:, 0:64], scalar=fs[:, 0:1], in1=ocm[:, p_, :],
                                                                 op0=ALU.mult, op1=ALU.add), reads=[C.psb[posel], fsb, ocmb], writes=[ohb[u]])
                    pr_ = (p_ // 2) % 2
                    S.op("dve", lambda e: e.scalar_tensor_tensor(out=opair[pr_][:, u * 64:(u + 1) * 64], in0=C.ps[powin][:, 0:64], scalar=fs[:, 1:2],
                                                                 in1=oh[u][:], op0=ALU.mult, op1=ALU.add),
                         reads=[C.psb[powin], fsb, ohb[u]], writes=[opb[pr_]])
                    if u == 1:
                        pt_ = prot.next()
                        ptv = C.ps[pt_][:].bitcast(BF16)
                        S.op("pe", lambda e: e.transpose(ptv[:, 0:128], opair[pr_][:], C.identh[:]), reads=[opb[pr_], C.identhb], writes=[C.psb[pt_]])
                        S.op("act", lambda e: e.activation(out=OT[:, p_ // 2, qs_], in_=ptv[:, 0:128], func=AF.Copy),
                             reads=[C.psb[pt_]], writes=[OTb[jq]])
            for a in range(4):
                wo_, wob_ = wor.next()
                row0 = (g * 4 + a) * 128
                S.dma("pool", wo_[:], dr["nsa_w_out"][jl][row0:row0 + 128, :], writes=[wob_])
                for t in range(NT):
                    for dh in range(2):
                        py = prot.next()
                        S.op("pe", lambda e: e.matmul(C.ps[py][:], OT[:, a, t * 128:(t + 1) * 128], wo_[:, dh * 512:(dh + 1) * 512],
                                                      start=True, stop=True), reads=[OTb[t], wob_], writes=[C.psb[py]])
                        Xs = C.X[:, t, dh * 512:(dh + 1) * 512]
                        S.op("dve", lambda e: e.tensor_tensor(out=Xs, in0=C.ps[py][:], in1=Xs, op=ALU.add),
                             reads=[C.psb[py], C.Xb[t]], writes=[C.Xb[t]])
        S.barrier()
_NC_CACHE = {}


def run(inputs, nlayers=4, trace=False):
    if nlayers not in _NC_CACHE:
        _NC_CACHE[nlayers] = build(nlayers)
    nc = _NC_CACHE[nlayers]
    consts = host_consts()
    x = np.asarray(inputs["x"], dtype=np.float32)
    shared = {}
    for name in nc._in_names:
        if name == "x":
            continue
        if name in consts:
            shared[name] = consts[name]
        elif "@" in name:
            base, l = name.split("@")
            shared[name.replace("@", "_L")] = np.ascontiguousarray(np.asarray(inputs[base], dtype=np.float32)[int(l)])
        else:
            shared[name] = np.ascontiguousarray(np.asarray(inputs[name], dtype=np.float32))
    in_maps = []
    for c in range(8):
        m = {"x": np.ascontiguousarray(x[c])}
        m.update(shared)
        in_maps.append(m)
    res = run_bass_kernel_spmd(nc, in_maps, core_ids=list(range(8)), trace=trace)
    out = np.stack([np.asarray(r["out"]) for r in res.results], axis=0).astype(np.float32)
    return out, res


def kernel(**inputs):
    out, _ = run(inputs, 4)
    return out
```
